# Optimizing a Trainium2 kernel written in Bass

```python
import jax, jax.numpy as jnp
from jax import lax
import numpy as np

D_MODEL = 1024
BATCH = 4
SEQ = 8192
DEPTH = 1

D_MIX = D_MODEL
ATTN_HEADS = 8
HEAD_DIM = 64
D_ATTN = ATTN_HEADS * HEAD_DIM
CONV_GROUPS = 8
D_CONV = D_MIX - D_ATTN
CONV_WIDTH = 3
IDX_HEADS = 8
IDX_DIM = 64
MAX_TOPK = 256
Q_BLOCK = 128
PEER_HEADS = 8
PEER_KEYS = 128
PEER_EXPERTS = PEER_KEYS * PEER_KEYS
PEER_HALF = 128
PEER_QDIM = 2 * PEER_HALF
PEER_TOPK = 16
EPS = 1e-6

SPLITS = [D_ATTN, D_ATTN, D_ATTN, D_CONV, D_CONV, D_CONV, IDX_HEADS * IDX_DIM, IDX_DIM, IDX_HEADS]
D_IN = sum(SPLITS)

kernel_name = "hybrid_dsa_shortconv_peer_block"


def rms_norm(x, w):
    xf = x.astype(jnp.float32)
    y = xf * lax.rsqrt(jnp.mean(xf * xf, axis=-1, keepdims=True) + EPS)
    return (y * w.astype(jnp.float32)).astype(x.dtype)


def _gather_rows(t, idx):
    return jax.vmap(lambda tb, ib: tb[ib])(t, idx)


def dsa_attention(q, k, v, iq, ik, iw):
    B, L = q.shape[0], q.shape[1]
    topk = min(MAX_TOPK, L // 4)
    n_blocks = L // Q_BLOCK
    s_pos = jnp.arange(L)
    att_scale = HEAD_DIM ** -0.5
    idx_scale = (IDX_DIM ** -0.5) * (IDX_HEADS ** -0.5)

    def block_fn(blk):
        start = blk * Q_BLOCK
        qb = lax.dynamic_slice_in_dim(q, start, Q_BLOCK, axis=1)
        iqb = lax.dynamic_slice_in_dim(iq, start, Q_BLOCK, axis=1)
        iwb = lax.dynamic_slice_in_dim(iw, start, Q_BLOCK, axis=1)
        t_pos = start + jnp.arange(Q_BLOCK)
        rel = jax.nn.relu(jnp.einsum('bqhd,bsd->bqhs', iqb, ik).astype(jnp.float32))
        score = jnp.einsum('bqhs,bqh->bqs', rel, iwb.astype(jnp.float32)) * idx_scale
        causal = s_pos[None, None, :] <= t_pos[None, :, None]
        score = jnp.where(causal, score, -jnp.inf)
        _, sel = lax.top_k(score, topk)
        k_sel = _gather_rows(k, sel)
        v_sel = _gather_rows(v, sel)
        logits = jnp.einsum('bqhd,bqkhd->bhqk', qb, k_sel).astype(jnp.float32) * att_scale
        valid = (sel <= t_pos[None, :, None])[:, None, :, :]
        logits = jnp.where(valid, logits, -jnp.inf)
        p = jax.nn.softmax(logits, axis=-1).astype(v.dtype)
        o = jnp.einsum('bhqk,bqkhd->bqhd', p, v_sel)
        return o.reshape(B, Q_BLOCK, ATTN_HEADS * HEAD_DIM)

    out = lax.map(block_fn, jnp.arange(n_blocks))
    return out.transpose(1, 0, 2, 3).reshape(B, L, ATTN_HEADS * HEAD_DIM)


def short_conv(b_gate, c_gate, xc, conv_w, conv_b):
    L = xc.shape[1]
    u = c_gate * xc
    up = jnp.pad(u, ((0, 0), (CONV_WIDTH - 1, 0), (0, 0)))
    y = conv_b
    for j in range(CONV_WIDTH):
        y = y + conv_w[j] * up[:, j:j + L]
    return b_gate * y


def peer_ffn(h, peer_wq, peer_subkeys, peer_u, peer_v):
    B, L, D = h.shape
    n_blocks = L // Q_BLOCK
    hb_all = h.reshape(B, n_blocks, Q_BLOCK, D).transpose(1, 0, 2, 3)

    def block_fn(hb):
        q = (hb @ peer_wq).reshape(B, Q_BLOCK, PEER_HEADS, 2, PEER_HALF)
        s = jnp.einsum('bqhpd,hpnd->bqhpn', q, peer_subkeys).astype(jnp.float32)
        vals, idx = lax.top_k(s, PEER_TOPK)
        combo = vals[..., 0, :, None] + vals[..., 1, None, :]
        ids = idx[..., 0, :, None] * PEER_KEYS + idx[..., 1, None, :]
        combo = combo.reshape(B, Q_BLOCK, PEER_HEADS, PEER_TOPK * PEER_TOPK)
        ids = ids.reshape(B, Q_BLOCK, PEER_HEADS, PEER_TOPK * PEER_TOPK)
        top_s, pos = lax.top_k(combo, PEER_TOPK)
        expert = jnp.take_along_axis(ids, pos, axis=-1)
        g = jax.nn.softmax(top_s, axis=-1).astype(hb.dtype)
        u_sel = peer_u[expert]
        a = jax.nn.gelu(jnp.einsum('bqhkd,bqd->bqhk', u_sel, hb))
        v_sel = peer_v[expert]
        return jnp.einsum('bqhk,bqhkd->bqd', g * a, v_sel)

    out = lax.map(block_fn, hb_all)
    return out.transpose(1, 0, 2, 3).reshape(B, L, D)


def setup_inputs(seed: int = 0) -> dict:
    key = jax.random.key(seed)
    ks = jax.random.split(key, 20)
    f32 = jnp.float32
    nrm = lambda k, shape, s: jax.random.normal(k, shape, f32) * s
    return {
        "x": nrm(ks[0], (BATCH, SEQ, D_MODEL), 1.0),
        "c": nrm(ks[1], (BATCH, D_MODEL), 1.0),
        "norm1_w": 1.0 + nrm(ks[2], (D_MODEL,), 0.02),
        "norm2_w": 1.0 + nrm(ks[3], (D_MODEL,), 0.02),
        "w_ada": nrm(ks[4], (D_MODEL, 6 * D_MODEL), D_MODEL ** -0.5),
        "b_ada": nrm(ks[5], (6 * D_MODEL,), 0.02),
        "w_in": nrm(ks[6], (D_MODEL, D_IN), D_MODEL ** -0.5),
        "q_norm_w": 1.0 + nrm(ks[7], (HEAD_DIM,), 0.02),
        "k_norm_w": 1.0 + nrm(ks[8], (HEAD_DIM,), 0.02),
        "conv_w": nrm(ks[9], (CONV_WIDTH, D_CONV), CONV_WIDTH ** -0.5),
        "conv_b": nrm(ks[10], (D_CONV,), 0.01),
        "attn_out_norm_w": 1.0 + nrm(ks[11], (D_ATTN,), 0.02),
        "conv_out_norm_w": 1.0 + nrm(ks[12], (D_CONV,), 0.02),
        "w_out": nrm(ks[13], (D_MIX, D_MODEL), D_MIX ** -0.5),
        "peer_wq": nrm(ks[14], (D_MODEL, PEER_HEADS * PEER_QDIM), D_MODEL ** -0.5),
        "peer_subkeys": nrm(ks[15], (PEER_HEADS, 2, PEER_KEYS, PEER_HALF), PEER_HALF ** -0.5),
        "peer_u": nrm(ks[16], (PEER_EXPERTS, D_MODEL), D_MODEL ** -0.5),
        "peer_v": nrm(ks[17], (PEER_EXPERTS, D_MODEL), PEER_HEADS ** -0.5),
    }


def reference(x, c, norm1_w, norm2_w, w_ada, b_ada, w_in, q_norm_w, k_norm_w,
              conv_w, conv_b, attn_out_norm_w, conv_out_norm_w, w_out,
              peer_wq, peer_subkeys, peer_u, peer_v):
    B, L, _ = x.shape
    mod = jax.nn.silu(c) @ w_ada + b_ada
    shift1, scale1, gate1, shift2, scale2, gate2 = [m[:, None, :] for m in jnp.split(mod, 6, axis=-1)]

    for _ in range(DEPTH):
        h = rms_norm(x, norm1_w) * (1.0 + scale1) + shift1
        proj = h @ w_in
        offs = np.cumsum(SPLITS)[:-1].tolist()
        q, k, v, cb, cc, cx, iq, ik, iw = jnp.split(proj, offs, axis=-1)
        q = rms_norm(q.reshape(B, L, ATTN_HEADS, HEAD_DIM), q_norm_w)
        k = rms_norm(k.reshape(B, L, ATTN_HEADS, HEAD_DIM), k_norm_w)
        v = v.reshape(B, L, ATTN_HEADS, HEAD_DIM)
        iq = iq.reshape(B, L, IDX_HEADS, IDX_DIM)
        y_attn = dsa_attention(q, k, v, iq, ik, iw)
        y_conv = short_conv(cb, cc, cx, conv_w, conv_b)
        y_mix = jnp.concatenate([rms_norm(y_attn, attn_out_norm_w),
                                 rms_norm(y_conv, conv_out_norm_w)], axis=-1)
        x = x + gate1 * (y_mix @ w_out)
        h2 = rms_norm(x, norm2_w) * (1.0 + scale2) + shift2
        x = x + gate2 * peer_ffn(h2, peer_wq, peer_subkeys, peer_u, peer_v)
    return x
```

```python
import numpy as np
import concourse.bass as bass
import concourse.mybir as mybir
from concourse.bass_utils import run_bass_kernel_spmd

F32 = mybir.dt.float32
BF16 = mybir.dt.bfloat16
ALU = mybir.AluOpType
AF = mybir.ActivationFunctionType
AX = mybir.AxisListType


class Tok:
    __slots__ = ("name", "w", "r")

    def __init__(self, name=""):
        self.name = name
        self.w = None
        self.r = []


def toks(name, n):
    return [Tok(f"{name}{i}") for i in range(n)]


class Ev:
    __slots__ = ("sem", "val", "eng", "know")

    def __init__(self, sem, val, eng, know):
        self.sem = sem
        self.val = val
        self.eng = eng
        self.know = know


class Sched:
    ENG = ("pe", "act", "dve", "pool", "sp")
    NDMA = 8

    def __init__(self, nc):
        self.nc = nc
        self.streams = {e: [] for e in self.ENG}
        self.sem = {e: nc.alloc_semaphore(f"s_{e}") for e in self.ENG}
        self.cnt = {e: 0 for e in self.ENG}
        self.know = {e: {} for e in self.ENG}
        self.dsem = {e: [nc.alloc_semaphore(f"d_{e}{i}") for i in range(self.NDMA)]
                     for e in ("sp", "act", "pool")}
        self.dcnt = {e: [0] * self.NDMA for e in self.dsem}
        self.dnext = {e: 0 for e in self.dsem}
        self.dlast = {e: [None] * self.NDMA for e in self.dsem}
        self.all_events = []
        self.last_ev = {}
        self.n_ops = 0

    def _need(self, eng, ev, waits):
        if ev is None:
            return
        k = self.know[eng]
        key = id(ev.sem)
        if k.get(key, 0) >= ev.val:
            return
        cur = waits.get(key)
        if cur is None or cur.val < ev.val:
            waits[key] = ev

    def _absorb(self, eng, ev):
        k = self.know[eng]
        for kk, vv in ev.know.items():
            if k.get(kk, 0) < vv:
                k[kk] = vv
        key = id(ev.sem)
        if k.get(key, 0) < ev.val:
            k[key] = ev.val

    def _deps(self, eng, reads, writes, same_eng_raw_only=True):
        waits = {}
        for t in reads:
            self._need(eng, t.w, waits)
        for t in writes:
            if t.w is not None and not (t.w.eng == eng and eng == "pe"):
                self._need(eng, t.w, waits)
            for ev in t.r:
                if ev.eng == eng and eng == "pe":
                    continue
                self._need(eng, ev, waits)
        return waits

    def op(self, eng, fn, reads=(), writes=()):
        waits = self._deps(eng, reads, writes)
        wl = list(waits.values())
        for ev in wl:
            self._absorb(eng, ev)
        self.cnt[eng] += 1
        ev = Ev(self.sem[eng], self.cnt[eng], eng, dict(self.know[eng]))
        self.streams[eng].append(([(w.sem, w.val) for w in wl], fn, (self.sem[eng], 1)))
        for t in reads:
            t.r.append(ev)
        for t in writes:
            t.w = ev
            t.r = []
        self.last_ev[("c", eng)] = ev
        self.n_ops += 1
        return ev

    def dma(self, q, fn, reads=(), writes=()):
        waits = self._deps(q, reads, writes)
        i = self.dnext[q]
        self.dnext[q] = (i + 1) % self.NDMA
        prev = self.dlast[q][i]
        if prev is not None:
            self._need(q, prev, waits)
        wl = list(waits.values())
        for ev in wl:
            self._absorb(q, ev)
        self.dcnt[q][i] += 16
        sem = self.dsem[q][i]
        ev = Ev(sem, self.dcnt[q][i], "dma_" + q, dict(self.know[q]))
        self.dlast[q][i] = ev
        self.streams[q].append(([(w.sem, w.val) for w in wl], fn, (sem, 16)))
        for t in reads:
            t.r.append(ev)
        for t in writes:
            t.w = ev
            t.r = []
        self.last_ev[("d", q, i)] = ev
        self.n_ops += 1
        return ev

    def barrier(self):
        evs = list(self.last_ev.values())
        for eng in self.ENG:
            waits = {}
            for ev in evs:
                self._need(eng, ev, waits)
            wl = list(waits.values())
            if not wl:
                continue
            for ev in wl:
                self._absorb(eng, ev)
            self.streams[eng].append(([(w.sem, w.val) for w in wl], None, None))

    def final_wait(self, eng="sp"):
        evs = list(self.last_ev.values())
        waits = {}
        for ev in evs:
            self._need(eng, ev, waits)
        wl = list(waits.values())
        for ev in wl:
            self._absorb(eng, ev)
        self.streams[eng].append(([(w.sem, w.val) for w in wl], None, None))

    def emit(self):
        nc = self.nc
        streams = self.streams

        def run(engine, items):
            for waits, fn, inc in items:
                for sem, val in waits:
                    engine.wait_ge(sem, val)
                if fn is not None:
                    ins = fn(engine)
                    ins.then_inc(inc[0], inc[1])

        with nc.Block() as block:
            @block.tensor
            def _(e):
                run(e, streams["pe"])

            @block.scalar
            def _(e):
                run(e, streams["act"])

            @block.vector
            def _(e):
                run(e, streams["dve"])

            @block.gpsimd
            def _(e):
                run(e, streams["pool"])

            @block.sync
            def _(e):
                run(e, streams["sp"])


from contextlib import ExitStack

D = 1024
SEQ = 8192
NT = SEQ // 128
NB = 32
C_Q, C_K, C_V, C_CB, C_CC, C_CX, C_IQ, C_IK, C_IW = 0, 512, 1024, 1536, 2048, 2560, 3072, 3584, 3648
DIN = 3656
EPS = 1e-6
NEG = -30000.0
NIT = 17
TOPK = 256.0
U32 = mybir.dt.uint32


def nchunks(j):
    g = j // 2
    return 4 * g + 2 if j % 2 == 0 else 4 * g + 4


def own_blocks(half):
    res = []
    for g in range(16):
        res += [4 * g + (0 if half == 0 else 1), 4 * g + (3 if half == 0 else 2)]
    return res


class Ctx:
    pass


_UNIQ = [0]


def _alloc(cx, es, name, shape, dtype):
    _UNIQ[0] += 1
    t = es.enter_context(cx.nc.sbuf_tensor(f"{name}_{_UNIQ[0]}", list(shape), dtype))
    return t.ap()


def declare(nc, dbg_out=(), dbg_in=()):
    cx = Ctx()
    cx.nc = nc

    def inp(name, shape, dt=F32):
        return nc.dram_tensor(name, list(shape), dt, kind="ExternalInput").ap()

    def scr(name, shape, dt):
        kind = "Internal"
        if name in dbg_out:
            kind = "ExternalOutput"
        if name in dbg_in:
            kind = "ExternalInput"
        return nc.dram_tensor(name, list(shape), dt, kind=kind).ap()

    cx.xseq = inp("xseq", [SEQ, D])
    cx.xown = inp("xown", [NB * 128, D])
    cx.xhalo = inp("xhalo", [64, D])
    cx.cT = inp("cT", [128, 8])
    cx.w_ada = inp("w_ada", [D, 6 * D])
    cx.b_ada = inp("b_ada", [1, 6 * D])
    cx.norm1_w = inp("norm1_w", [1, D])
    cx.norm2_w = inp("norm2_w", [1, D])
    cx.w_in = inp("w_in", [D, DIN])
    cx.pp = inp("pp", [128, 32])
    cx.aonw = inp("aonw", [1, 512])
    cx.w_out = inp("w_out", [D, D])
    cx.peer_wq = inp("peer_wq", [D, 2048])
    cx.skT = inp("skT", [16, 128, 128])
    cx.UT = inp("UT", [D, 16384])
    cx.VH = inp("VH", [16384, D])
    cx.ident = inp("ident", [128, 128])
    cx.hmask = inp("hmask", [128, 64])
    cx.iota = inp("iota", [128, 128])
    cx.cmask = inp("cmask", [128, 2, 256])
    cx.out = nc.dram_tensor("out", [NB * 128, D], F32, kind="ExternalOutput").ap()
    cx.KT = scr("KT", [4, 128, SEQ], BF16)
    cx.VP = scr("VP", [NT, 128, 520], BF16)
    cx.IKT = scr("IKT", [64, SEQ], BF16)
    cx.QT = scr("QT", [NB, 128, 4, 128], BF16)
    cx.IQT = scr("IQT", [NB, 64, 8, 128], BF16)
    cx.IW = scr("IW", [NB, 128, 8], F32)
    cx.YCT = scr("YCT", [NB, 128, 4, 128], BF16)
    cx.MB = scr("MB", [NB, 128, SEQ], BF16)
    cx.X1 = scr("X1", [NB * 128, D], F32)
    cx.H2T = scr("H2T", [NB, 128, 8, 128], BF16)
    cx.MT = scr("MT", [NB, 128, 128, 128], BF16)
    cx.UTb = scr("UTb", [8, 128, 16384], BF16)
    cx.Vb = scr("Vb", [128, 128, D], BF16)
    return cx


def setup_persistent(cx):
    nc = cx.nc
    S = cx.S = Sched(nc)
    A = lambda name, shape, dt: nc.alloc_sbuf_tensor(name, list(shape), dt).ap()
    cx.identb = A("identb", [128, 128], BF16)
    cx.identf = A("identf", [128, 128], F32)
    cx.eps_t = A("eps_t", [128, 1], F32)
    cx.ppt = A("ppt", [128, 32], F32)
    cx.bd64 = A("bd64", [128, 128], BF16)
    cx.ones512 = A("ones512", [128, 128], BF16)
    cx.t_mod = Tok("modbc")
    cx.MODD = nc.dram_tensor("MODD", [128, 6 * D], F32).ap()
    cx.t_MODD = Tok("MODD")
    cx.t_const = Tok("const")
    cx.banks = [nc.alloc_psum_tensor(f"bank{i}", [128, 512], F32).ap() for i in range(8)]
    cx.t_bank = toks("bank", 8)
    cx.t_KT, cx.t_VP, cx.t_IKT = toks("KT", NT), toks("VP", NT), toks("IKT", NT)
    cx.t_QT, cx.t_IQT, cx.t_IW, cx.t_YCT = toks("QT", NB), toks("IQT", NB), toks("IW", NB), toks("YCT", NB)
    cx.t_MB, cx.t_X1, cx.t_H2T, cx.t_MT = toks("MB", NB), toks("X1", NB), toks("H2T", NB), toks("MT", NB)
    cx.t_UTb, cx.t_Vb = toks("UTb", 16), toks("Vb", 16)
    cx.t_outs = toks("out", NB)
    S.dma("sp", lambda e: e.dma_start(out=cx.identf, in_=cx.ident), writes=[cx.t_const])
    S.dma("pool", lambda e: e.dma_start(out=cx.identb, in_=cx.ident), writes=[cx.t_const])
    S.dma("sp", lambda e: e.dma_start(out=cx.ppt, in_=cx.pp), writes=[cx.t_const])
    S.op("dve", lambda e: e.memset(cx.eps_t, EPS), writes=[cx.t_const])
    S.op("dve", lambda e: e.memset(cx.bd64, 0.0), writes=[cx.t_const])
    S.op("dve", lambda e: e.memset(cx.bd64[0:64, 0:64], 1.0 / 64), writes=[cx.t_const])
    S.op("dve", lambda e: e.memset(cx.bd64[64:128, 64:128], 1.0 / 64), writes=[cx.t_const])
    S.op("dve", lambda e: e.memset(cx.ones512, 1.0 / 512), writes=[cx.t_const])


def load_mod(cx, es):
    cx.modbc = _alloc(cx, es, "modbc", [128, 6 * D], F32)
    cx.t_mod = Tok("modbc")
    for q in range(6):
        cx.S.dma("sp" if q % 2 == 0 else "act", (lambda q=q: (lambda e: e.dma_start(out=cx.modbc[:, q * D:(q + 1) * D], in_=cx.MODD[:, q * D:(q + 1) * D])))(),
                 reads=[cx.t_MODD], writes=[cx.t_mod])


def phase_end(cx):
    cx.S.barrier()
    cx.S.emit()
    cx.S.streams = {e: [] for e in Sched.ENG}


def phase_ada(cx):
    nc, S = cx.nc, cx.S
    with ExitStack() as es:
        cx.modbc = _alloc(cx, es, "modbc", [128, 6 * D], F32)
        ct = _alloc(cx, es, "ct", [128, 8], F32)
        sc = _alloc(cx, es, "sc", [128, 8], F32)
        scb = _alloc(cx, es, "scb", [128, 8, 128], F32)
        bbc = _alloc(cx, es, "bbc", [128, 6 * D], F32)
        n12 = _alloc(cx, es, "n12", [128, 2, D], F32)
        wa = [_alloc(cx, es, f"wa{i}", [128, 8, 512], F32) for i in range(2)]
        t_wa = toks("wa", 2)
        t_ct, t_sc, t_scb, t_bbc, t_n = Tok(), Tok(), Tok(), Tok(), Tok()
        S.dma("sp", lambda e: e.dma_start(out=ct, in_=cx.cT), writes=[t_ct])
        S.dma("sp", lambda e: e.dma_start(out=bbc, in_=cx.b_ada.partition_broadcast(128).rearrange("p a b -> p (a b)")),
              writes=[t_bbc])
        S.dma("act", lambda e: e.dma_start(out=n12[:, 0, :], in_=cx.norm1_w.partition_broadcast(128).rearrange("p a b -> p (a b)")),
              writes=[t_n])
        S.dma("act", lambda e: e.dma_start(out=n12[:, 1, :], in_=cx.norm2_w.partition_broadcast(128).rearrange("p a b -> p (a b)")),
              writes=[t_n])
        S.op("act", lambda e: e.activation(sc, ct, AF.Silu), reads=[t_ct], writes=[t_sc])
        S.op("dve", lambda e: e.tensor_copy(scb, sc.unsqueeze(2).to_broadcast([128, 8, 128])), reads=[t_sc], writes=[t_scb])
        wsrc = cx.w_ada.rearrange("(k p) n -> p k n", p=128)
        for cg in range(12):
            b = cg % 2
            q = "sp" if cg % 2 == 0 else "act"
            S.dma(q, (lambda cg=cg, b=b: (lambda e: e.dma_start(out=wa[b], in_=wsrc[:, :, cg * 512:(cg + 1) * 512])))(),
                  writes=[t_wa[b]])
            for k in range(8):
                S.op("pe", (lambda k=k, b=b: (lambda e: e.matmul(cx.banks[b], scb[:, k, :], wa[b][:, k, :],
                                                                  start=(k == 0), stop=(k == 7))))(),
                     reads=[t_scb, t_wa[b]], writes=[cx.t_bank[b]])
            S.op("dve", (lambda cg=cg, b=b: (lambda e: e.tensor_tensor(cx.modbc[:, cg * 512:(cg + 1) * 512], cx.banks[b],
                                                                        bbc[:, cg * 512:(cg + 1) * 512], ALU.add)))(),
                 reads=[cx.t_bank[b], t_bbc], writes=[cx.t_mod])
        for (o, n) in ((1, 0), (4, 1)):
            S.op("dve", (lambda o=o, n=n: (lambda e: e.scalar_tensor_tensor(
                cx.modbc[:, o * D:(o + 1) * D], cx.modbc[:, o * D:(o + 1) * D], 1.0, n12[:, n, :], ALU.add, ALU.mult)))(),
                 reads=[cx.t_mod, t_n], writes=[cx.t_mod])
        S.dma("sp", lambda e: e.dma_start(out=cx.MODD, in_=cx.modbc), reads=[cx.t_mod], writes=[cx.t_MODD])
        phase_end(cx)


class NormBufs:
    def __init__(self, cx, es, tag):
        self.junk = _alloc(cx, es, f"nj{tag}", [128, D], BF16)
        self.ss = _alloc(cx, es, f"nss{tag}", [128, 1], F32)
        self.rt = _alloc(cx, es, f"nrt{tag}", [128, 1], F32)
        self.rstd = _alloc(cx, es, f"nrs{tag}", [128, 1], F32)
        self.t1 = _alloc(cx, es, f"nt1{tag}", [128, D], F32)
        self.hb = _alloc(cx, es, f"nhb{tag}", [128, D], BF16)
        self.t_junk, self.t_ss, self.t_rt, self.t_rstd, self.t_t1, self.t_hb = Tok(), Tok(), Tok(), Tok(), Tok(), Tok()


def norm_mod(cx, nb, x, t_x, n, which):
    S = cx.S
    A = cx.modbc[:, (1 + 3 * which) * D:(2 + 3 * which) * D]
    Sh = cx.modbc[:, (3 * which) * D:(3 * which + 1) * D]
    S.op("act", lambda e: e.activation(nb.junk[:n], x[:n], AF.Square, accum_out=nb.ss[:n, 0:1]),
         reads=[t_x], writes=[nb.t_junk, nb.t_ss])
    S.op("act", lambda e: e.activation(nb.rt[:n], nb.ss[:n], AF.Sqrt, bias=cx.eps_t[:n], scale=1.0 / D),
         reads=[nb.t_ss, cx.t_const], writes=[nb.t_rt])
    S.op("dve", lambda e: e.reciprocal(nb.rstd[:n], nb.rt[:n]), reads=[nb.t_rt], writes=[nb.t_rstd])
    S.op("dve", lambda e: e.scalar_tensor_tensor(nb.t1[:n], x[:n], nb.rstd[:n, 0:1], A[:n], ALU.mult, ALU.mult),
         reads=[t_x, nb.t_rstd, cx.t_mod], writes=[nb.t_t1])
    S.op("pool", lambda e: e.tensor_tensor(nb.hb[:n], nb.t1[:n], Sh[:n], ALU.add),
         reads=[nb.t_t1, cx.t_mod], writes=[nb.t_hb])


def transpose8(cx, nb, n, bank_i, hT, t_hT, evac="act"):
    S = cx.S
    pb = cx.banks[bank_i].bitcast(BF16).rearrange("p (k t) -> p k t", k=8)
    for k in range(8):
        S.op("pe", (lambda k=k: (lambda e: e.transpose(pb[:, k, :n], nb.hb[:n, k * 128:(k + 1) * 128], cx.identb[:n, :n])))(),
             reads=[nb.t_hb, cx.t_const], writes=[cx.t_bank[bank_i]])
    if evac == "act":
        S.op("act", lambda e: e.activation(hT[:, :, :n], pb[:, :, :n], AF.Copy), reads=[cx.t_bank[bank_i]], writes=[t_hT])
    else:
        S.op("dve", lambda e: e.tensor_copy(hT[:, :, :n], pb[:, :, :n]), reads=[cx.t_bank[bank_i]], writes=[t_hT])


def load_win(cx, es):
    S = cx.S
    winb = _alloc(cx, es, "winb", [128, 8, DIN], BF16)
    t_w = Tok("winb")
    src = cx.w_in.rearrange("(k p) n -> p k n", p=128)
    for k in range(8):
        S.dma("pool", (lambda k=k: (lambda e: e.dma_start(out=winb[:, k, :], in_=src[:, k, :])))(), writes=[t_w])
    return winb, t_w


def qk_norm(cx, pk_i, w_col, out3, t_out, sq, t_sq, rk, t_rk, pms_i):
    S = cx.S
    pk = cx.banks[pk_i]
    S.op("act", lambda e: e.activation(sq, pk, AF.Square), reads=[cx.t_bank[pk_i]], writes=[t_sq])
    S.op("pe", lambda e: e.matmul(cx.banks[pms_i], cx.bd64, sq, start=True, stop=True),
         reads=[t_sq, cx.t_const], writes=[cx.t_bank[pms_i]])
    S.op("act", lambda e: e.activation(rk, cx.banks[pms_i], AF.Sqrt, bias=cx.eps_t, scale=1.0),
         reads=[cx.t_bank[pms_i], cx.t_const], writes=[t_rk])
    S.op("dve", lambda e: e.reciprocal(rk, rk), reads=[t_rk], writes=[t_rk])
    S.op("dve", lambda e: e.scalar_tensor_tensor(out3, pk.rearrange("p (a t) -> p a t", a=4), cx.ppt[:, w_col:w_col + 1],
                                                 rk.rearrange("p (a t) -> p a t", a=4), ALU.mult, ALU.mult),
         reads=[cx.t_bank[pk_i], t_rk, cx.t_const], writes=[t_out])


def phase_p1a(cx, ntiles=NT):
    nc, S = cx.nc, cx.S
    with ExitStack() as es:
        load_mod(cx, es)
        winb, t_w = load_win(cx, es)
        nb = NormBufs(cx, es, "a")
        xt = [_alloc(cx, es, f"xt{i}", [128, D], F32) for i in range(2)]
        t_xt = toks("xt", 2)
        hT = [_alloc(cx, es, f"hT{i}", [128, 8, 128], BF16) for i in range(2)]
        t_hT = toks("hT", 2)
        sq = _alloc(cx, es, "sq", [128, 512], BF16)
        rk = _alloc(cx, es, "rk", [128, 512], F32)
        t_sq, t_rk = Tok(), Tok()
        ktst = [_alloc(cx, es, f"ktst{i}", [128, 4, 512], BF16) for i in range(2)]
        t_ktst = toks("ktst", 2)
        ikst = [_alloc(cx, es, f"ikst{i}", [64, 512], BF16) for i in range(2)]
        t_ikst = toks("ikst", 2)
        vst = [_alloc(cx, es, f"vst{i}", [128, 8, 65], BF16) for i in range(2)]
        t_vst = toks("vst", 2)
        t_KT, t_VP, t_IKT = cx.t_KT, cx.t_VP, cx.t_IKT
        for b in range(2):
            S.op("pool", (lambda b=b: (lambda e: e.memset(vst[b], 1.0)))(), writes=[t_vst[b]])
        KTd = cx.KT.rearrange("a p t -> p a t")
        def stage_a(i):
            b = i % 2
            S.dma("sp", (lambda i=i, b=b: (lambda e: e.dma_start(out=xt[b], in_=cx.xseq[i * 128:(i + 1) * 128, :])))(),
                  writes=[t_xt[b]])
            norm_mod(cx, nb, xt[b], t_xt[b], 128, 0)
            transpose8(cx, nb, 128, 0, hT[b], t_hT[b])

        def stage_b(i):
            b = i % 2
            g4, s4 = (i // 4) % 2, i % 4
            pk = cx.banks[1].rearrange("p (a t) -> p a t", a=4)
            for p in range(4):
                for k in range(8):
                    S.op("pe", (lambda p=p, k=k, b=b: (lambda e: e.matmul(
                        pk[:, p, :], winb[:, k, C_K + p * 128:C_K + (p + 1) * 128], hT[b][:, k, :],
                        start=(k == 0), stop=(k == 7))))(), reads=[t_w, t_hT[b]], writes=[cx.t_bank[1]])
            qk_norm(cx, 1, 0, ktst[g4][:, :, s4 * 128:(s4 + 1) * 128], t_ktst[g4], sq, t_sq, rk, t_rk, 2)
            for k in range(8):
                S.op("pe", (lambda k=k, b=b: (lambda e: e.matmul(cx.banks[3], hT[b][:, k, :], winb[:, k, C_V:C_V + 512],
                                                                  start=(k == 0), stop=(k == 7))))(),
                     reads=[t_w, t_hT[b]], writes=[cx.t_bank[3]])
            S.op("act", (lambda b=b: (lambda e: e.activation(vst[b][:, :, 0:64], cx.banks[3].rearrange("p (h d) -> p h d", h=8),
                                                              AF.Copy)))(), reads=[cx.t_bank[3]], writes=[t_vst[b]])
            S.dma("act", (lambda i=i, b=b: (lambda e: e.dma_start(out=cx.VP[i], in_=vst[b].rearrange("p h d -> p (h d)"))))(),
                  reads=[t_vst[b]], writes=[t_VP[i]])
            for k in range(8):
                S.op("pe", (lambda k=k, b=b: (lambda e: e.matmul(cx.banks[4][0:64, 0:128], winb[:, k, C_IK:C_IK + 64], hT[b][:, k, :],
                                                                  start=(k == 0), stop=(k == 7))))(),
                     reads=[t_w, t_hT[b]], writes=[cx.t_bank[4]])
            S.op("dve", (lambda g4=g4, s4=s4: (lambda e: e.tensor_copy(ikst[g4][:, s4 * 128:(s4 + 1) * 128], cx.banks[4][0:64, 0:128])))(),
                 reads=[cx.t_bank[4]], writes=[t_ikst[g4]])
            if s4 == 3:
                i0 = i - 3
                S.dma("act", (lambda i0=i0, g4=g4: (lambda e: e.dma_start(out=KTd[:, :, i0 * 128:(i0 + 4) * 128], in_=ktst[g4])))(),
                      reads=[t_ktst[g4]], writes=t_KT[i0:i0 + 4])
                S.dma("act", (lambda i0=i0, g4=g4: (lambda e: e.dma_start(out=cx.IKT[:, i0 * 128:(i0 + 4) * 128], in_=ikst[g4])))(),
                      reads=[t_ikst[g4]], writes=t_IKT[i0:i0 + 4])

        stage_a(0)
        for i in range(ntiles):
            if i + 1 < ntiles:
                stage_a(i + 1)
            stage_b(i)
        phase_end(cx)


def phase_p1b(cx, nblocks=NB):
    nc, S = cx.nc, cx.S
    with ExitStack() as es:
        load_mod(cx, es)
        winb, t_w = load_win(cx, es)
        nb = NormBufs(cx, es, "b")
        xt = [_alloc(cx, es, f"xt{i}", [128, D], F32) for i in range(2)]
        t_xt = toks("xt", 2)
        hT = [_alloc(cx, es, f"hT{i}", [128, 8, 128], BF16) for i in range(2)]
        t_hT = toks("hT", 2)
        sq = _alloc(cx, es, "sq", [128, 512], BF16)
        rk = _alloc(cx, es, "rk", [128, 512], F32)
        t_sq, t_rk = Tok(), Tok()
        uh = _alloc(cx, es, "uh", [128, 4, 64], F32)
        cxh = _alloc(cx, es, "cxh", [128, 4, 64], F32)
        t_uh, t_cxh = Tok(), Tok()
        qst = [_alloc(cx, es, f"qst{i}", [128, 4, 128], BF16) for i in range(2)]
        t_qst = toks("qst", 2)
        iqst = [_alloc(cx, es, f"iqst{i}", [64, 8, 128], BF16) for i in range(2)]
        t_iqst = toks("iqst", 2)
        iwst = [_alloc(cx, es, f"iwst{i}", [128, 8], F32) for i in range(2)]
        t_iwst = toks("iwst", 2)
        cxs = _alloc(cx, es, "cxs", [128, 4, 128], F32)
        u = _alloc(cx, es, "u", [128, 4, 130], F32)
        acc = _alloc(cx, es, "acc", [128, 4, 128], F32)
        y = _alloc(cx, es, "y", [128, 4, 128], F32)
        ysq = _alloc(cx, es, "ysq", [128, 4, 128], BF16)
        rs = _alloc(cx, es, "rs", [128, 128], F32)
        ycst = [_alloc(cx, es, f"ycst{i}", [128, 4, 128], BF16) for i in range(2)]
        t_ycst = toks("ycst", 2)
        t_cxs, t_u, t_acc, t_y, t_ysq, t_rs = Tok(), Tok(), Tok(), Tok(), Tok(), Tok()
        ppt = cx.ppt

        S.dma("sp", lambda e: e.dma_start(out=xt[0][0:64, :], in_=cx.xhalo), writes=[t_xt[0]])
        norm_mod(cx, nb, xt[0], t_xt[0], 64, 0)
        transpose8(cx, nb, 64, 0, hT[0], t_hT[0])
        hpcc = cx.banks[1].rearrange("p (a t) -> p a t", a=4)
        hpcx = cx.banks[2].rearrange("p (a t) -> p a t", a=4)
        for (pp_, col, bi) in ((hpcc, C_CC, 1), (hpcx, C_CX, 2)):
            for c in range(4):
                for k in range(8):
                    S.op("pe", (lambda pp_=pp_, col=col, c=c, k=k: (lambda e: e.matmul(
                        pp_[:, c, 0:64], winb[:, k, col + c * 128:col + (c + 1) * 128], hT[0][:, k, 0:64],
                        start=(k == 0), stop=(k == 7))))(), reads=[t_w, t_hT[0]], writes=[cx.t_bank[bi]])
        S.op("act", lambda e: e.activation(cxh, hpcx[:, :, 0:64], AF.Copy), reads=[cx.t_bank[2]], writes=[t_cxh])
        S.op("dve", lambda e: e.tensor_tensor(uh, hpcc[:, :, 0:64], cxh, ALU.mult), reads=[cx.t_bank[1], t_cxh], writes=[t_uh])
        hm = _alloc(cx, es, "hm", [128, 64], F32)
        t_hm = Tok()
        S.dma("sp", lambda e: e.dma_start(out=hm, in_=cx.hmask), writes=[t_hm])
        S.op("dve", lambda e: e.tensor_tensor(uh, uh, hm.unsqueeze(1).to_broadcast([128, 4, 64]), ALU.mult),
             reads=[t_uh, t_hm], writes=[t_uh])

        def stage_a(j):
            b = j % 2
            S.dma("sp", (lambda j=j, b=b: (lambda e: e.dma_start(out=xt[b], in_=cx.xown[j * 128:(j + 1) * 128, :])))(),
                  writes=[t_xt[b]])
            norm_mod(cx, nb, xt[b], t_xt[b], 128, 0)
            transpose8(cx, nb, 128, 0, hT[b], t_hT[b])

        def stage_b(j):
            b = j % 2
            pq = cx.banks[1].rearrange("p (a t) -> p a t", a=4)
            for p in range(4):
                for k in range(8):
                    S.op("pe", (lambda p=p, k=k, b=b: (lambda e: e.matmul(
                        pq[:, p, :], winb[:, k, C_Q + p * 128:C_Q + (p + 1) * 128], hT[b][:, k, :],
                        start=(k == 0), stop=(k == 7))))(), reads=[t_w, t_hT[b]], writes=[cx.t_bank[1]])
            qk_norm(cx, 1, 1, qst[b], t_qst[b], sq, t_sq, rk, t_rk, 2)
            S.dma("act", (lambda j=j, b=b: (lambda e: e.dma_start(out=cx.QT[j], in_=qst[b])))(), reads=[t_qst[b]], writes=[cx.t_QT[j]])
            for h in range(8):
                bi = 3 + h // 4
                pi = cx.banks[bi].rearrange("p (a t) -> p a t", a=4)
                for k in range(8):
                    S.op("pe", (lambda h=h, k=k, b=b, pi=pi: (lambda e: e.matmul(
                        pi[0:64, h % 4, :], winb[:, k, C_IQ + h * 64:C_IQ + (h + 1) * 64], hT[b][:, k, :],
                        start=(k == 0), stop=(k == 7))))(), reads=[t_w, t_hT[b]], writes=[cx.t_bank[bi]])
            for hh in range(2):
                pi = cx.banks[3 + hh].rearrange("p (a t) -> p a t", a=4)
                S.op("act", (lambda hh=hh, b=b, pi=pi: (lambda e: e.activation(iqst[b][:, hh * 4:(hh + 1) * 4, :], pi[0:64], AF.Copy)))(),
                     reads=[cx.t_bank[3 + hh]], writes=[t_iqst[b]])
            S.dma("act", (lambda j=j, b=b: (lambda e: e.dma_start(out=cx.IQT[j], in_=iqst[b])))(), reads=[t_iqst[b]], writes=[cx.t_IQT[j]])
            for k in range(8):
                S.op("pe", (lambda k=k, b=b: (lambda e: e.matmul(cx.banks[5][:, 0:8], hT[b][:, k, :], winb[:, k, C_IW:C_IW + 8],
                                                                  start=(k == 0), stop=(k == 7))))(),
                     reads=[t_w, t_hT[b]], writes=[cx.t_bank[5]])
            S.op("dve", (lambda b=b: (lambda e: e.tensor_copy(iwst[b], cx.banks[5][:, 0:8])))(), reads=[cx.t_bank[5]], writes=[t_iwst[b]])
            S.dma("act", (lambda j=j, b=b: (lambda e: e.dma_start(out=cx.IW[j], in_=iwst[b])))(), reads=[t_iwst[b]], writes=[cx.t_IW[j]])
            pcb = cx.banks[6].rearrange("p (a t) -> p a t", a=4)
            pcc = cx.banks[7].rearrange("p (a t) -> p a t", a=4)
            pcx = cx.banks[2].rearrange("p (a t) -> p a t", a=4)
            for (pp_, col, bi) in ((pcb, C_CB, 6), (pcc, C_CC, 7), (pcx, C_CX, 2)):
                for c in range(4):
                    for k in range(8):
                        S.op("pe", (lambda pp_=pp_, col=col, c=c, k=k, b=b: (lambda e: e.matmul(
                            pp_[:, c, :], winb[:, k, col + c * 128:col + (c + 1) * 128], hT[b][:, k, :],
                            start=(k == 0), stop=(k == 7))))(), reads=[t_w, t_hT[b]], writes=[cx.t_bank[bi]])
            S.op("act", lambda e: e.activation(cxs, pcx, AF.Copy), reads=[cx.t_bank[2]], writes=[t_cxs])
            S.op("dve", lambda e: e.tensor_tensor(u[:, :, 2:130], pcc, cxs, ALU.mult), reads=[cx.t_bank[7], t_cxs], writes=[t_u])
            S.op("pool", (lambda j=j: (lambda e: e.tensor_copy(u[:, :, 0:2], uh[:, :, 2 * j:2 * j + 2])))(), reads=[t_uh], writes=[t_u])
            for c in range(4):
                S.op("dve", (lambda c=c: (lambda e: e.tensor_scalar(acc[:, c, :], u[:, c, 2:130], ppt[:, 2 + c * 3 + 2:2 + c * 3 + 3],
                                                                     ppt[:, 14 + c:15 + c], ALU.mult, ALU.add)))(),
                     reads=[t_u, cx.t_const], writes=[t_acc])
                S.op("dve", (lambda c=c: (lambda e: e.scalar_tensor_tensor(acc[:, c, :], u[:, c, 1:129], ppt[:, 2 + c * 3 + 1:2 + c * 3 + 2],
                                                                            acc[:, c, :], ALU.mult, ALU.add)))(),
                     reads=[t_u, t_acc, cx.t_const], writes=[t_acc])
                S.op("dve", (lambda c=c: (lambda e: e.scalar_tensor_tensor(acc[:, c, :], u[:, c, 0:128], ppt[:, 2 + c * 3:2 + c * 3 + 1],
                                                                            acc[:, c, :], ALU.mult, ALU.add)))(),
                     reads=[t_u, t_acc, cx.t_const], writes=[t_acc])
            S.op("dve", lambda e: e.tensor_tensor(y, acc, pcb, ALU.mult), reads=[t_acc, cx.t_bank[6]], writes=[t_y])
            S.op("act", lambda e: e.activation(ysq, y, AF.Square), reads=[t_y], writes=[t_ysq])
            for c in range(4):
                S.op("pe", (lambda c=c: (lambda e: e.matmul(cx.banks[5][:, 128:256], cx.ones512, ysq[:, c, :], start=(c == 0), stop=(c == 3))))(),
                     reads=[t_ysq, cx.t_const], writes=[cx.t_bank[5]])
            S.op("act", lambda e: e.activation(rs, cx.banks[5][:, 128:256], AF.Sqrt, bias=cx.eps_t, scale=1.0),
                 reads=[cx.t_bank[5], cx.t_const], writes=[t_rs])
            S.op("dve", lambda e: e.reciprocal(rs, rs), reads=[t_rs], writes=[t_rs])
            for c in range(4):
                S.op("dve", (lambda c=c, b=b: (lambda e: e.scalar_tensor_tensor(ycst[b][:, c, :], y[:, c, :], ppt[:, 18 + c:19 + c], rs,
                                                                                 ALU.mult, ALU.mult)))(),
                     reads=[t_y, t_rs, cx.t_const], writes=[t_ycst[b]])
            S.dma("act", (lambda j=j, b=b: (lambda e: e.dma_start(out=cx.YCT[j], in_=ycst[b])))(), reads=[t_ycst[b]], writes=[cx.t_YCT[j]])

        stage_a(0)
        for j in range(nblocks):
            if j + 1 < nblocks:
                stage_a(j + 1)
            stage_b(j)
        phase_end(cx)


def prep_shared(inp):
    f = lambda a: np.ascontiguousarray(np.asarray(a, dtype=np.float32))
    sh = {}
    sh["w_ada"] = f(inp["w_ada"])
    sh["b_ada"] = f(inp["b_ada"]).reshape(1, -1)
    sh["norm1_w"] = f(inp["norm1_w"]).reshape(1, -1)
    sh["norm2_w"] = f(inp["norm2_w"]).reshape(1, -1)
    sh["w_in"] = f(inp["w_in"])
    pp = np.zeros((128, 32), np.float32)
    pp[:, 0] = np.tile(f(inp["k_norm_w"]), 2)
    pp[:, 1] = np.tile(f(inp["q_norm_w"]), 2)
    cw = f(inp["conv_w"])
    for c in range(4):
        for j in range(3):
            pp[:, 2 + c * 3 + j] = cw[j, c * 128:(c + 1) * 128]
        pp[:, 14 + c] = f(inp["conv_b"])[c * 128:(c + 1) * 128]
        pp[:, 18 + c] = f(inp["conv_out_norm_w"])[c * 128:(c + 1) * 128]
    sh["pp"] = pp
    sh["aonw"] = f(inp["attn_out_norm_w"]).reshape(1, -1)
    sh["w_out"] = f(inp["w_out"])
    sh["peer_wq"] = f(inp["peer_wq"])
    sk = f(inp["peer_subkeys"])
    sh["skT"] = np.ascontiguousarray(sk.transpose(0, 1, 3, 2).reshape(16, 128, 128))
    U = f(inp["peer_u"]).reshape(128, 128, D)
    sh["UT"] = np.ascontiguousarray(U.transpose(2, 1, 0).reshape(D, 16384))
    V = f(inp["peer_v"]).reshape(128, 128, D)
    sh["VH"] = np.ascontiguousarray(V.transpose(1, 0, 2).reshape(16384, D))
    sh["ident"] = np.eye(128, dtype=np.float32)
    sh["iota"] = np.ascontiguousarray(np.tile(np.arange(128, dtype=np.float32)[None, :], (128, 1)))
    return sh


def prep_core(inp, sh, core):
    b, half = core // 2, core % 2
    x = np.asarray(inp["x"], dtype=np.float32)[b]
    blocks = own_blocks(half)
    m = dict(sh)
    m["xseq"] = np.ascontiguousarray(x)
    m["xown"] = np.ascontiguousarray(np.concatenate([x[g * 128:(g + 1) * 128] for g in blocks], 0))
    halo = np.zeros((64, D), np.float32)
    for j, g in enumerate(blocks):
        if g > 0:
            halo[2 * j:2 * j + 2] = x[g * 128 - 2:g * 128]
    m["xhalo"] = halo
    hm = np.ones((128, 64), np.float32)
    for j, g in enumerate(blocks):
        if g == 0:
            hm[:, 2 * j:2 * j + 2] = 0.0
    m["hmask"] = hm
    m["cT"] = np.ascontiguousarray(np.asarray(inp["c"], dtype=np.float32)[b].reshape(8, 128).T)
    cm = np.zeros((128, 2, 256), np.float32)
    for par in range(2):
        j = par
        g = blocks[j]
        n = nchunks(j)
        kpos = (n - 2) * 128 + np.arange(256)[None, :]
        qpos = g * 128 + np.arange(128)[:, None]
        cm[:, par, :] = (kpos <= qpos)
    m["cmask"] = cm
    return m


def build_program(phases, dbg_out=(), dbg_in=(), **kw):
    nc = bass.Bass("TRN2", target_bir_lowering=False)
    cx = declare(nc, dbg_out, dbg_in)
    setup_persistent(cx)
    for ph in phases:
        PHASES[ph](cx, **kw.get(ph, {}))
    cx.S.final_wait("sp")
    cx.S.final_wait("act")
    cx.S.final_wait("pool")
    cx.S.emit()
    return nc, cx


PHASES = {"ada": phase_ada, "p1a": phase_p1a, "p1b": phase_p1b}


def phase_p2(cx, nblocks=NB):
    nc, S = cx.nc, cx.S
    with ExitStack() as es:
        ikt = _alloc(cx, es, "ikt", [64, SEQ], BF16)
        t_ikt = Tok()
        cm = _alloc(cx, es, "cm", [128, 2, 256], F32)
        nbm = _alloc(cx, es, "nbm", [128, 2, 256], F32)
        t_cm = Tok()
        sc = [_alloc(cx, es, f"sc{i}", [128, SEQ], F32) for i in range(4)]
        t_sc = toks("sc", 4)
        rl = [_alloc(cx, es, f"rl{i}", [128, 512], BF16) for i in range(6)]
        aw = [_alloc(cx, es, f"aw{i}", [128, 8], F32) for i in range(2)]
        sg = [_alloc(cx, es, f"sg{i}", [128, 8], F32) for i in range(2)]
        dg = [_alloc(cx, es, f"dg{i}", [128, 8, 128], BF16) for i in range(2)]
        t_aw, t_sg, t_dg = toks("aw", 2), toks("sg", 2), toks("dg", 2)
        t_rl = toks("rl", 6)
        junkD = _alloc(cx, es, "junkD", [128, 8], BF16)
        junkA = _alloc(cx, es, "junkA", [128, 8], BF16)
        t_junkD, t_junkA = Tok(), Tok()
        mbs = [_alloc(cx, es, f"mbs{i}", [128, SEQ], BF16) for i in range(2)]
        t_mbs = toks("mbs", 2)
        t_mbs2 = toks("mbs2", 2)
        iq = [_alloc(cx, es, f"iq{i}", [64, 8, 128], BF16) for i in range(2)]
        t_iq = toks("iq", 2)
        iw = [_alloc(cx, es, f"iw{i}", [128, 8], F32) for i in range(2)]
        t_iw = toks("iw", 2)
        p2c = _alloc(cx, es, "p2c", [128, NIT + 2], F32)
        t_p2c = Tok()
        STs = [_alloc(cx, es, f"ST{i}", [128, NIT + 2], F32) for i in range(4)]
        sms = [_alloc(cx, es, f"sm{i}", [128, 10], F32) for i in range(4)]
        tk = [{n: Tok() for n in ("ST", "mm", "mn", "W", "mid", "cnt", "ps", "lo", "ssum", "tot")} for _ in range(4)]
        for q4 in range(4):
            lo_, hi_ = q4 * (SEQ // 4), (q4 + 1) * (SEQ // 4)
            S.dma("sp", (lambda lo_=lo_, hi_=hi_: (lambda e: e.dma_start(out=ikt[:, lo_:hi_], in_=cx.IKT[:, lo_:hi_])))(),
                  reads=cx.t_IKT[q4 * 16:(q4 + 1) * 16], writes=[t_ikt])
        S.dma("sp", lambda e: e.dma_start(out=cm, in_=cx.cmask), writes=[t_cm])
        S.op("dve", lambda e: e.tensor_scalar(nbm, cm, -1.0, 1e30, ALU.add, ALU.mult), reads=[t_cm], writes=[t_cm])
        for k in range(NIT + 2):
            S.op("pool", (lambda k=k: (lambda e: e.memset(p2c[:, k:k + 1], 2.0 ** (-k))))(), writes=[t_p2c])
        cnts = {"rl": 0, "bk": 0}

        def accumulate(j):
            b = j % 2
            b4 = j % 4
            par = j % 2
            nk = 128 * nchunks(j)
            sm, ST, T = sms[b4], STs[b4], tk[b4]
            S.dma("sp", lambda e: e.dma_start(out=iq[b], in_=cx.IQT[j]), reads=[cx.t_IQT[j]], writes=[t_iq[b]])
            S.dma("sp", lambda e: e.dma_start(out=iw[b], in_=cx.IW[j]), reads=[cx.t_IW[j]], writes=[t_iw[b]])
            S.op("act", lambda e: e.activation(sg[b], iw[b], AF.Sign), reads=[t_iw[b]], writes=[t_sg[b]])
            S.op("dve", lambda e: e.tensor_tensor(aw[b], iw[b], sg[b], ALU.mult), reads=[t_iw[b], t_sg[b]], writes=[t_aw[b]])
            for h in range(8):
                S.op("dve", (lambda h=h: (lambda e: e.tensor_scalar(dg[b][:, h, :], cx.identb, sg[b][:, h:h + 1], None, ALU.mult)))(),
                     reads=[t_sg[b], cx.t_const], writes=[t_dg[b]])
            ngr = (nk + 511) // 512
            items = [(kg, h) for kg in range(ngr) for h in range(8)]
            LAGY = 2
            slot = {}
            for s_i in range(len(items) + LAGY):
                if s_i < len(items):
                    kg, h = items[s_i]
                    k0 = kg * 512
                    w = min(512, nk - k0)
                    bi = cnts["bk"] % 4
                    cnts["bk"] += 1
                    r = cnts["rl"] % 6
                    cnts["rl"] += 1
                    slot[s_i] = r
                    S.op("pe", (lambda bi=bi, h=h, k0=k0, w=w: (lambda e: e.matmul(
                        cx.banks[bi][:, :w], iq[b][:, h, :], ikt[:, k0:k0 + w], start=True, stop=True)))(),
                        reads=[t_iq[b], t_ikt], writes=[cx.t_bank[bi]])
                    if h % 4 == 3:
                        S.op("dve", (lambda bi=bi, r=r, w=w, h=h: (lambda e: e.tensor_scalar(rl[r][:, :w], cx.banks[bi][:, :w], 0.0,
                                                                                            aw[b][:, h:h + 1], ALU.max, ALU.mult)))(),
                             reads=[cx.t_bank[bi], t_aw[b]], writes=[t_rl[r]])
                    else:
                        S.op("act", (lambda bi=bi, r=r, w=w, h=h: (lambda e: e.activation(rl[r][:, :w], cx.banks[bi][:, :w], AF.Relu,
                                                                                          scale=aw[b][:, h:h + 1])))(),
                             reads=[cx.t_bank[bi], t_aw[b]], writes=[t_rl[r]])
                if s_i - LAGY >= 0:
                    kg, h = items[s_i - LAGY]
                    k0 = kg * 512
                    w = min(512, nk - k0)
                    r = slot[s_i - LAGY]
                    ab = 4 + kg % 2
                    S.op("pe", (lambda ab=ab, h=h, r=r, w=w: (lambda e: e.matmul(
                        cx.banks[ab][:, :w], dg[b][:, h, :], rl[r][:, :w], start=(h == 0), stop=(h == 7))))(),
                        reads=[t_dg[b], t_rl[r]], writes=[cx.t_bank[ab]])
                    if h == 7:
                        if kg % 2 == 0:
                            S.op("act", (lambda ab=ab, k0=k0, w=w: (lambda e: e.activation(sc[b4][:, k0:k0 + w], cx.banks[ab][:, :w], AF.Copy)))(),
                                 reads=[cx.t_bank[ab]], writes=[t_sc[b4]])
                        else:
                            S.op("dve", (lambda ab=ab, k0=k0, w=w: (lambda e: e.tensor_copy(sc[b4][:, k0:k0 + w], cx.banks[ab][:, :w])))(),
                                 reads=[cx.t_bank[ab]], writes=[t_sc[b4]])
                yield 1

        def acc_tail(j):
            b4 = j % 4
            par = j % 2
            nk = 128 * nchunks(j)
            sm, ST, T = sms[b4], STs[b4], tk[b4]
            scv = sc[b4][:, :nk]
            S.op("dve", lambda e: e.tensor_reduce(sm[:, 0:1], scv, AX.X, ALU.max), reads=[t_sc[b4]], writes=[T["mm"]])
            S.op("dve", lambda e: e.tensor_reduce(sm[:, 1:2], scv, AX.X, ALU.min), reads=[t_sc[b4]], writes=[T["mn"]])
            tail = sc[b4][:, nk - 256:nk]
            S.op("dve", lambda e: e.tensor_tensor(tail, tail, cm[:, par, :], ALU.mult), reads=[t_sc[b4], t_cm], writes=[t_sc[b4]])
            S.op("dve", lambda e: e.tensor_tensor(tail, tail, nbm[:, par, :], ALU.add), reads=[t_sc[b4], t_cm], writes=[t_sc[b4]])
            S.op("dve", lambda e: e.tensor_scalar(sm[:, 2:3], sm[:, 0:1], sm[:, 1:2], 2.0, ALU.subtract, ALU.add),
                 reads=[T["mm"], T["mn"]], writes=[T["W"]])
            S.op("dve", lambda e: e.tensor_scalar(ST, p2c, sm[:, 2:3], None, ALU.mult), reads=[T["W"], t_p2c], writes=[T["ST"]])
            S.op("dve", lambda e: e.tensor_scalar(sm[:, 3:4], sm[:, 1:2], -1.0, ST[:, 1:2], ALU.add, ALU.add),
                 reads=[T["mn"], T["ST"]], writes=[T["mid"]])

        def bis_iter(j, k):
            b = j % 2
            b4 = j % 4
            nk = 128 * nchunks(j)
            sm, ST, T = sms[b4], STs[b4], tk[b4]
            nA = max(128, (int(nk * 0.52) // 128) * 128)
            nB = nk - nA
            thr = TOPK - nB / 2.0
            S.op("dve", lambda e: e.tensor_scalar(junkD[:, 0:1].to_broadcast([128, nA]), sc[b4][:, :nA], sm[:, 3:4], None, ALU.is_ge, ALU.add, accum_out=sm[:, 4:5]),
                 reads=[t_sc[b4], T["mid"]], writes=[t_junkD, T["cnt"]])
            S.op("act", lambda e: e.activation(junkA[:, 0:1].to_broadcast([128, nk - nA]), sc[b4][:, nA:nk], AF.Sign, bias=sm[:, 3:4], scale=-1.0, accum_out=sm[:, 7:8]),
                 reads=[t_sc[b4], T["mid"]], writes=[t_junkA, T["ssum"]])
            S.op("dve", lambda e: e.scalar_tensor_tensor(sm[:, 8:9], sm[:, 7:8], -0.5, sm[:, 4:5], ALU.mult, ALU.add),
                 reads=[T["ssum"], T["cnt"]], writes=[T["tot"]])
            S.op("dve", lambda e: e.tensor_scalar(sm[:, 5:6], sm[:, 8:9], thr, ST[:, k:k + 1], ALU.is_ge, ALU.mult),
                 reads=[T["tot"], T["ST"]], writes=[T["ps"]])
            S.op("dve", lambda e: e.scalar_tensor_tensor(sm[:, 3:4], sm[:, 3:4], ST[:, k + 1:k + 2], sm[:, 5:6], ALU.subtract, ALU.add),
                 reads=[T["mid"], T["ST"], T["ps"]], writes=[T["mid"]])

        def finalize(j):
            b = j % 2
            b4 = j % 4
            nk = 128 * nchunks(j)
            sm, ST, T = sms[b4], STs[b4], tk[b4]
            S.op("dve", lambda e: e.tensor_scalar(sm[:, 6:7], sm[:, 3:4], ST[:, NIT + 1:NIT + 2], None, ALU.subtract),
                 reads=[T["mid"], T["ST"]], writes=[T["lo"]])
            S.op("dve", lambda e: e.tensor_scalar(mbs[b][:, :nk], sc[b4][:, :nk], sm[:, 6:7], NEG, ALU.is_lt, ALU.mult),
                 reads=[t_sc[b4], T["lo"]], writes=[t_mbs[b]])
            S.dma("sp", lambda e: e.dma_start(out=cx.MB[j][:, :nk], in_=mbs[b][:, :nk]),
                  reads=[t_mbs[b]], writes=[cx.t_MB[j]])

        import itertools
        npairs = nblocks // 2

        def acc_pair_gen(p):
            for jj in (2 * p, 2 * p + 1):
                yield from accumulate(jj)

        for _ in acc_pair_gen(0):
            pass
        acc_tail(0)
        acc_tail(1)
        for p in range(npairs):
            jA, jB = 2 * p, 2 * p + 1
            nxt = acc_pair_gen(p + 1) if p + 1 < npairs else iter(())
            nsteps = 0
            if p + 1 < npairs:
                for jj in (2 * p + 2, 2 * p + 3):
                    nsteps += 8 * ((128 * nchunks(jj) + 511) // 512) + 2
            per = -(-nsteps // (2 * NIT)) if nsteps else 0
            for k in range(1, NIT + 1):
                bis_iter(jA, k)
                for _ in itertools.islice(nxt, per):
                    pass
                bis_iter(jB, k)
                for _ in itertools.islice(nxt, per):
                    pass
            for _ in nxt:
                pass
            finalize(jA)
            finalize(jB)
            if p + 1 < npairs:
                acc_tail(2 * p + 2)
                acc_tail(2 * p + 3)
        phase_end(cx)


PHASES["p2"] = phase_p2


def phase_p3(cx, nblocks=NB, nkt=NT):
    nc, S = cx.nc, cx.S
    with ExitStack() as es:
        load_mod(cx, es)
        kt = _alloc(cx, es, "kt", [128, 4, SEQ], BF16)
        vp = _alloc(cx, es, "vp", [128, NT, 520], BF16)
        t_kt, t_vp = Tok(), Tok()
        woutb = _alloc(cx, es, "woutb", [128, 8, D], BF16)
        aon = _alloc(cx, es, "aon", [128, 512], F32)
        t_wo, t_aon = Tok(), Tok()
        qt = [_alloc(cx, es, f"qt{i}", [128, 4, 128], BF16) for i in range(2)]
        qp = [_alloc(cx, es, f"qp{i}", [128, 8, 128], BF16) for i in range(2)]
        t_qt, t_qp = toks("qt", 2), toks("qp", 2)
        mbg = [_alloc(cx, es, f"mbg{i}", [128, 512], BF16) for i in range(3)]
        t_mbg = toks("mbg", 3)
        pt = [_alloc(cx, es, f"pt{i}", [128, 4, 128], BF16) for i in range(4)]
        t_pt = toks("pt", 4)
        o = _alloc(cx, es, "o", [128, 8, 65], F32)
        rden = _alloc(cx, es, "rden", [128, 8], F32)
        ya = _alloc(cx, es, "ya", [128, 8, 64], F32)
        junk = _alloc(cx, es, "junk3", [128, 512], BF16)
        st3 = _alloc(cx, es, "st3", [128, 4], F32)
        yan = _alloc(cx, es, "yan", [128, 512], BF16)
        t_o, t_rden, t_ya, t_junk, t_ss, t_rt, t_rstd, t_yan = Tok(), Tok(), Tok(), Tok(), Tok(), Tok(), Tok(), Tok()
        ymT = [_alloc(cx, es, f"ymT{i}", [128, 8, 128], BF16) for i in range(2)]
        t_ymA, t_ymC = toks("ymA", 2), toks("ymC", 2)
        xt = [_alloc(cx, es, f"xt{i}", [128, D], F32) for i in range(2)]
        tmp = _alloc(cx, es, "tmp3", [128, 512], F32)
        t_xt, t_tmp = toks("xt", 2), Tok()
        G1 = cx.modbc[:, 2 * D:3 * D]

        KTd = cx.KT.rearrange("a p t -> p a t")
        VPd = cx.VP.rearrange("n p f -> p n f")
        step = 8
        for i0 in range(0, nkt, step):
            i1 = min(nkt, i0 + step)
            S.dma("sp", (lambda i0=i0, i1=i1: (lambda e: e.dma_start(out=kt[:, :, i0 * 128:i1 * 128], in_=KTd[:, :, i0 * 128:i1 * 128])))(),
                  reads=cx.t_KT[i0:i1], writes=[t_kt])
            S.dma("act", (lambda i0=i0, i1=i1: (lambda e: e.dma_start(out=vp[:, i0:i1, :], in_=VPd[:, i0:i1, :])))(),
                  reads=cx.t_VP[i0:i1], writes=[t_vp])
        wsrc = cx.w_out.rearrange("(k p) n -> p k n", p=128)
        for k in range(8):
            S.dma("pool", (lambda k=k: (lambda e: e.dma_start(out=woutb[:, k, :], in_=wsrc[:, k, :])))(), writes=[t_wo])
        S.dma("sp", lambda e: e.dma_start(out=aon, in_=cx.aonw.partition_broadcast(128).rearrange("p a b -> p (a b)")), writes=[t_aon])
        for b in range(2):
            S.op("pool", (lambda b=b: (lambda e: e.memset(qp[b], 0.0)))(), writes=[t_qp[b]])
        ident4 = _alloc(cx, es, "ident4", [128, 4, 128], BF16)
        t_id4 = Tok()
        S.op("pool", lambda e: e.tensor_copy(ident4, cx.identb.unsqueeze(1).to_broadcast([128, 4, 128])), reads=[cx.t_const], writes=[t_id4])
        id4f = ident4.rearrange("p a t -> p (a t)")
        PSB = (0, 1, 7)
        LAG = 2
        itc = [0]
        mgc = [0]

        def load_mask(j, c, nk):
            mr = mgc[0] % 3
            mgc[0] += 1
            w = min(512, nk - c * 128)
            S.dma("sp", lambda e: e.dma_start(out=mbg[mr][:, :w], in_=cx.MB[j][:, c * 128:c * 128 + w]),
                  reads=[cx.t_MB[j]], writes=[t_mbg[mr]])
            return mr

        def stage_a(b, c, hg, it, mr):
            bi = PSB[it % 3]
            pr = it % 4
            qpf = qp[b].rearrange("p a t -> p (a t)")
            for pp in range(2):
                pair = 2 * hg + pp
                S.op("pe", (lambda pp=pp, pair=pair: (lambda e: e.matmul(
                    cx.banks[bi][:, pp * 256:(pp + 1) * 256], kt[:, pair, c * 128:(c + 1) * 128], qpf[:, pair * 256:(pair + 1) * 256],
                    start=(pp == 0), stop=False, skip_group_check=True)))(),
                    reads=[t_kt, t_qp[b]], writes=[cx.t_bank[bi]])
            S.op("pe", lambda e: e.matmul(cx.banks[bi], mbg[mr][:, (c % 4) * 128:(c % 4 + 1) * 128], id4f,
                                          start=False, stop=True, skip_group_check=True),
                 reads=[t_mbg[mr], t_id4], writes=[cx.t_bank[bi]])
            S.op("act", lambda e: e.activation(pt[pr].rearrange("p a t -> p (a t)"), cx.banks[bi], AF.Exp, scale=0.125),
                 reads=[cx.t_bank[bi]], writes=[t_pt[pr]])

        def stage_c(c, hg, it, nck):
            pr = it % 4
            po = cx.banks[2 + hg][:, 0:260].rearrange("p (a d) -> p a d", a=4)
            for hh in range(4):
                h = hg * 4 + hh
                S.op("pe", (lambda hh=hh, h=h: (lambda e: e.matmul(
                    po[:, hh, :], pt[pr][:, hh, :], vp[:, c, h * 65:(h + 1) * 65], start=(c == 0 and hh == 0), stop=(c == nck - 1),
                    skip_group_check=True)))(),
                    reads=[t_pt[pr], t_vp], writes=[cx.t_bank[2 + hg]])

        for j in range(nblocks):
            b = j % 2
            nck = nchunks(j)
            nk = nck * 128
            S.dma("sp", (lambda j=j, b=b: (lambda e: e.dma_start(out=qt[b], in_=cx.QT[j])))(), reads=[cx.t_QT[j]], writes=[t_qt[b]])
            S.dma("sp", (lambda j=j, b=b: (lambda e: e.dma_start(out=xt[b], in_=cx.xown[j * 128:(j + 1) * 128, :])))(), writes=[t_xt[b]])
            S.dma("sp", (lambda j=j, b=b: (lambda e: e.dma_start(out=ymT[b][:, 4:8, :], in_=cx.YCT[j])))(), reads=[cx.t_YCT[j]], writes=[t_ymC[b]])
            qp4 = qp[b].rearrange("p (a two) t -> p a two t", two=2)
            S.op("pool", (lambda b=b, qp4=qp4: (lambda e: e.tensor_copy(qp4[0:64, :, 0, :], qt[b][0:64, :, :])))(), reads=[t_qt[b]], writes=[t_qp[b]])
            S.op("pool", (lambda b=b, qp4=qp4: (lambda e: e.tensor_copy(qp4[64:128, :, 1, :], qt[b][64:128, :, :])))(), reads=[t_qt[b]], writes=[t_qp[b]])
            items = [(c, hg) for c in range(nck) for hg in range(2)]
            mrs = {}
            mrs[0] = load_mask(j, 0, nk)
            its = {}
            for s_i in range(len(items) + LAG):
                if s_i < len(items):
                    c, hg = items[s_i]
                    if hg == 0 and c % 4 == 0 and c + 4 < nck:
                        mrs[c + 4] = load_mask(j, c + 4, nk)
                    its[s_i] = itc[0]
                    itc[0] += 1
                    stage_a(b, c, hg, its[s_i], mrs[(c // 4) * 4])
                if s_i - LAG >= 0:
                    c, hg = items[s_i - LAG]
                    stage_c(c, hg, its[s_i - LAG], nck)
            for hg in range(2):
                S.op("dve", (lambda hg=hg: (lambda e: e.tensor_copy(o[:, hg * 4:(hg + 1) * 4, :],
                                                                    cx.banks[2 + hg][:, 0:260].rearrange("p (a d) -> p a d", a=4))))(),
                     reads=[cx.t_bank[2 + hg]], writes=[t_o])
            S.op("dve", lambda e: e.reciprocal(rden, o[:, :, 64]), reads=[t_o], writes=[t_rden])
            S.op("dve", lambda e: e.tensor_tensor(ya, o[:, :, 0:64], rden.unsqueeze(2).to_broadcast([128, 8, 64]), ALU.mult),
                 reads=[t_o, t_rden], writes=[t_ya])
            yaf = ya.rearrange("p h d -> p (h d)")
            S.op("act", (lambda yaf=yaf: (lambda e: e.activation(junk, yaf, AF.Square, accum_out=st3[:, 0:1])))(), reads=[t_ya], writes=[t_junk, t_ss])
            S.op("act", lambda e: e.activation(st3[:, 1:2], st3[:, 0:1], AF.Sqrt, bias=cx.eps_t, scale=1.0 / 512),
                 reads=[t_ss, cx.t_const], writes=[t_rt])
            S.op("dve", lambda e: e.reciprocal(st3[:, 2:3], st3[:, 1:2]), reads=[t_rt], writes=[t_rstd])
            S.op("dve", (lambda yaf=yaf: (lambda e: e.scalar_tensor_tensor(yan, yaf, st3[:, 2:3], aon, ALU.mult, ALU.mult)))(),
                 reads=[t_ya, t_rstd, t_aon], writes=[t_yan])
            pb = cx.banks[4].bitcast(BF16).rearrange("p (k t) -> p k t", k=8)
            for c4 in range(4):
                S.op("pe", (lambda c4=c4, pb=pb: (lambda e: e.transpose(pb[:, c4, :], yan[:, c4 * 128:(c4 + 1) * 128], cx.identb)))(),
                     reads=[t_yan, cx.t_const], writes=[cx.t_bank[4]])
            S.op("act", (lambda b=b, pb=pb: (lambda e: e.activation(ymT[b][:, 0:4, :], pb[:, 0:4, :], AF.Copy)))(),
                 reads=[cx.t_bank[4]], writes=[t_ymA[b]])
            for half in range(2):
                for k in range(8):
                    S.op("pe", (lambda half=half, k=k, b=b: (lambda e: e.matmul(
                        cx.banks[5 + half], ymT[b][:, k, :], woutb[:, k, half * 512:(half + 1) * 512], start=(k == 0), stop=(k == 7))))(),
                        reads=[t_ymA[b], t_ymC[b], t_wo], writes=[cx.t_bank[5 + half]])
                S.op("dve", (lambda half=half: (lambda e: e.tensor_tensor(tmp, cx.banks[5 + half],
                                                                          G1[:, half * 512:(half + 1) * 512], ALU.mult)))(),
                     reads=[cx.t_bank[5 + half], cx.t_mod], writes=[t_tmp])
                S.op("pool", (lambda b=b, half=half: (lambda e: e.tensor_tensor(xt[b][:, half * 512:(half + 1) * 512], tmp,
                                                                                xt[b][:, half * 512:(half + 1) * 512], ALU.add)))(),
                     reads=[t_tmp, t_xt[b]], writes=[t_xt[b]])
            S.dma("pool", (lambda j=j, b=b: (lambda e: e.dma_start(out=cx.X1[j * 128:(j + 1) * 128, :], in_=xt[b])))(),
                  reads=[t_xt[b]], writes=[cx.t_X1[j]])
        phase_end(cx)


PHASES["p3"] = phase_p3


def _declare_peer(cx):
    if hasattr(cx, "SS"):
        return
    nc = cx.nc
    cx.SS = nc.dram_tensor("SS", [NB, 128, 2048], F32).ap()
    cx.MXS = nc.dram_tensor("MXS", [NB, 128, 512], F32).ap()
    cx.t_SS = toks("SS", NB)
    cx.t_UTb2 = [[Tok() for _ in range(8)] for _ in range(8)]
    cx.t_Vb2 = toks("Vb", 32)


def phase_pcast(cx):
    nc, S = cx.nc, cx.S
    _declare_peer(cx)
    with ExitStack() as es:
        st = [_alloc(cx, es, f"cst{i}", [128, 4096], BF16) for i in range(4)]
        t_st = toks("cst", 4)
        r = 0
        for k in range(8):
            for cb in range(8):
                b = r % 4
                r += 1
                S.dma("pool", (lambda k=k, cb=cb, b=b: (lambda e: e.dma_start(
                    out=st[b][:, 0:2048], in_=cx.UT[k * 128:(k + 1) * 128, cb * 2048:(cb + 1) * 2048])))(), writes=[t_st[b]])
                S.dma("act", (lambda k=k, cb=cb, b=b: (lambda e: e.dma_start(
                    out=cx.UTb[k][:, cb * 2048:(cb + 1) * 2048], in_=st[b][:, 0:2048])))(), reads=[t_st[b]], writes=[cx.t_UTb2[k][cb]])
        VHs = cx.VH.rearrange("(j i) d -> i j d", i=128)
        Vbd = cx.Vb.rearrange("j i d -> i j d")
        for jb in range(32):
            b = r % 4
            r += 1
            stv = st[b].rearrange("p (j d) -> p j d", j=4)
            S.dma("pool", (lambda jb=jb, stv=stv: (lambda e: e.dma_start(out=stv, in_=VHs[:, jb * 4:(jb + 1) * 4, :])))(), writes=[t_st[b]])
            S.dma("sp", (lambda jb=jb, stv=stv: (lambda e: e.dma_start(out=Vbd[:, jb * 4:(jb + 1) * 4, :], in_=stv)))(),
                  reads=[t_st[b]], writes=[cx.t_Vb2[jb]])
        phase_end(cx)


def phase_p4q(cx, nblocks=NB):
    nc, S = cx.nc, cx.S
    _declare_peer(cx)
    with ExitStack() as es:
        load_mod(cx, es)
        wqb = _alloc(cx, es, "wqb", [128, 8, 2048], BF16)
        skb = _alloc(cx, es, "skb", [128, 16, 128], BF16)
        t_wq, t_sk = Tok(), Tok()
        nb = NormBufs(cx, es, "q")
        xt = [_alloc(cx, es, f"xt{i}", [128, D], F32) for i in range(2)]
        t_xt = toks("xt", 2)
        hT = [_alloc(cx, es, f"hT{i}", [128, 8, 128], BF16) for i in range(2)]
        t_hT = toks("hT", 2)
        qT = _alloc(cx, es, "qT", [128, 16, 128], BF16)
        t_qT = toks("qT", 4)
        sfs = [_alloc(cx, es, f"sfs{i}", [128, 16, 128], F32) for i in range(2)]
        t_sfs = toks("sfs", 2)
        mxix = [_alloc(cx, es, f"mxix{i}", [128, 768], F32) for i in range(2)]
        t_mxix = toks("mxix", 2)
        ixu = _alloc(cx, es, "ixu", [128, 16, 16], U32)
        tm = [_alloc(cx, es, f"tmq{i}", [128, 128], F32) for i in range(16)]
        t_tm, t_mxg, t_ixg = toks("tmq", 16), toks("mxgq", 16), toks("ixgq", 16)
        wsrc = cx.peer_wq.rearrange("(k p) n -> p k n", p=128)
        for k in range(8):
            S.dma("pool", (lambda k=k: (lambda e: e.dma_start(out=wqb[:, k, :], in_=wsrc[:, k, :])))(), writes=[t_wq])
        S.dma("pool", lambda e: e.dma_start(out=skb, in_=cx.skT.rearrange("g d n -> d g n")), writes=[t_sk])
        def stage_a(j):
            b = j % 2
            S.dma("sp", (lambda j=j, b=b: (lambda e: e.dma_start(out=xt[b], in_=cx.X1[j * 128:(j + 1) * 128, :])))(),
                  reads=[cx.t_X1[j]], writes=[t_xt[b]])
            norm_mod(cx, nb, xt[b], t_xt[b], 128, 1)
            transpose8(cx, nb, 128, 0, hT[b], t_hT[b])
            S.dma("act", (lambda j=j, b=b: (lambda e: e.dma_start(out=cx.H2T[j], in_=hT[b])))(), reads=[t_hT[b]], writes=[cx.t_H2T[j]])
            for g4 in range(4):
                bi = 1 + g4 % 2
                pq = cx.banks[bi].rearrange("p (a t) -> p a t", a=4)
                for gg in range(4):
                    gq = g4 * 4 + gg
                    for k in range(8):
                        S.op("pe", (lambda pq=pq, gg=gg, gq=gq, k=k, b=b: (lambda e: e.matmul(
                            pq[:, gg, :], wqb[:, k, gq * 128:(gq + 1) * 128], hT[b][:, k, :], start=(k == 0), stop=(k == 7))))(),
                            reads=[t_wq, t_hT[b]], writes=[cx.t_bank[bi]])
                S.op("act", (lambda pq=pq, g4=g4: (lambda e: e.activation(qT[:, g4 * 4:(g4 + 1) * 4, :], pq, AF.Copy)))(),
                     reads=[cx.t_bank[bi]], writes=[t_qT[g4]])
            for g4 in range(4):
                bi = 3 + g4 % 2
                pk = cx.banks[bi].rearrange("p (a t) -> p a t", a=4)
                for gg in range(4):
                    gq = g4 * 4 + gg
                    S.op("pe", (lambda pk=pk, gg=gg, gq=gq: (lambda e: e.matmul(
                        pk[:, gg, :], qT[:, gq, :], skb[:, gq, :], start=True, stop=True)))(),
                        reads=[t_qT[g4], t_sk], writes=[cx.t_bank[bi]])
                S.op("act", (lambda pk=pk, g4=g4, b=b: (lambda e: e.activation(sfs[b][:, g4 * 4:(g4 + 1) * 4, :], pk, AF.Copy)))(),
                     reads=[cx.t_bank[bi]], writes=[t_sfs[b]])

        def stage_b(j):
            b = j % 2
            mx = mxix[b][:, 0:256].rearrange("p (g r) -> p g r", g=16)
            ixf = mxix[b][:, 256:768].rearrange("p (g r) -> p g r", g=16)
            srcs = [sfs[b][:, gq, :] for gq in range(16)]
            for gq in range(16):
                S.op("dve", (lambda gq=gq, mx=mx, srcs=srcs: (lambda e: e.max(mx[:, gq, 0:8], srcs[gq])))(), reads=[t_sfs[b]], writes=[t_mxg[gq]])
            for gq in range(16):
                S.op("dve", (lambda gq=gq, mx=mx, srcs=srcs: (lambda e: e.match_replace(tm[gq], mx[:, gq, 0:8], srcs[gq], -BIGF)))(),
                     reads=[t_sfs[b], t_mxg[gq]], writes=[t_tm[gq]])
            for gq in range(16):
                S.op("dve", (lambda gq=gq, mx=mx, srcs=srcs: (lambda e: e.max_index(ixu[:, gq, 0:8], mx[:, gq, 0:8], srcs[gq])))(),
                     reads=[t_sfs[b], t_mxg[gq]], writes=[t_ixg[gq]])
            for gq in range(16):
                S.op("dve", (lambda gq=gq, mx=mx: (lambda e: e.max(mx[:, gq, 8:16], tm[gq])))(), reads=[t_tm[gq]], writes=[t_mxg[gq]])
            for gq in range(16):
                S.op("dve", (lambda gq=gq, mx=mx: (lambda e: e.max_index(ixu[:, gq, 8:16], mx[:, gq, 8:16], tm[gq])))(),
                     reads=[t_tm[gq], t_mxg[gq]], writes=[t_ixg[gq]])
            S.op("pool", (lambda b=b: (lambda e: e.tensor_copy(mxix[b][:, 256:512], ixu.rearrange("p g r -> p (g r)"))))(),
                 reads=t_ixg, writes=[t_mxix[b]])
            S.dma("pool", (lambda j=j, b=b: (lambda e: e.dma_start(out=cx.MXS[j], in_=mxix[b][:, 0:512])))(),
                  reads=t_mxg + [t_mxix[b]], writes=[cx.t_SS[j]])

        stage_a(0)
        for j in range(nblocks):
            if j + 1 < nblocks:
                stage_a(j + 1)
            stage_b(j)
        phase_end(cx)


PHASES["pcast"] = phase_pcast
PHASES["p4q"] = phase_p4q


BIGF = 1.0e30
PHI_MARGIN = 2.0e-6


def phase_p4a_old(cx, nblocks=NB):
    nc, S = cx.nc, cx.S
    _declare_peer(cx)
    with ExitStack() as es:
        A_ = lambda name, shape, dt: _alloc(cx, es, name, shape, dt)
        iota = A_("iota", [128, 128], F32)
        t_iota = Tok()
        sf = [A_(f"sf{i}", [128, 16, 128], F32) for i in range(2)]
        t_sf = toks("sf", 2)
        tm = [A_(f"tm{i}", [128, 128], F32) for i in range(4)]
        t_tm = toks("tm", 4)
        mx = A_("mx", [128, 16, 16], F32)
        ix = A_("ix", [128, 8, 16], U32)
        ixf = A_("ixf", [128, 8, 16], F32)
        combo = A_("combo", [128, 8, 16, 16], F32)
        ctmp = [A_(f"ctmp{i}", [128, 256], F32) for i in range(2)]
        t_ctmp = toks("ctmp", 2)
        tops = A_("tops", [128, 8, 16], F32)
        sml = A_("sml", [128, 8, 8], F32)
        etop = A_("etop", [128, 8, 16], F32)
        Sel = A_("Sel", [128, 8, 16, 16], F32)
        wa = A_("wa", [128, 8, 16, 16], F32)
        wb = A_("wb", [128, 8, 16, 16], F32)
        phi = A_("phi", [128, 128], F32)
        Aw = A_("Aw", [128, 8, 16], F32)
        Bw = A_("Bw", [128, 8, 128], F32)
        tT = A_("tT", [128, 256], F32)
        L = A_("L", [128, 128, 128], BF16)
        R = A_("R", [128, 128, 128], BF16)
        MTt = A_("MTt", [128, 128, 128], BF16)
        s2x = [A_(f"s2x{i}", [128, 4, 8, 16], F32) for i in range(3)]
        abc = [A_(f"abc{i}", [128, 4, 8, 16], BF16) for i in range(3)]
        ind = [A_(f"ind{i}", [128, 128, 4], BF16) for i in range(2)]
        t_s2x, t_abc, t_ind = toks("s2x", 3), toks("abc", 3), toks("ind", 2)
        (t_mx, t_ix, t_ixf, t_combo, t_tops, t_sml, t_etop, t_Sel, t_wa, t_wb, t_phi, t_Aw, t_Bw, t_tT, t_L, t_R, t_MTt) = [Tok() for _ in range(17)]
        S.dma("sp", lambda e: e.dma_start(out=iota, in_=cx.iota), writes=[t_iota])
        mxv = mx.rearrange("p (h two) r -> p h two r", two=2)
        s1top, s2top = mxv[:, :, 0, :], mxv[:, :, 1, :]
        tmi = 0
        ri3 = 0
        ri2 = 0
        for j in range(nblocks):
            b = j % 2
            S.dma("sp", (lambda j=j, b=b: (lambda e: e.dma_start(out=sf[b].rearrange("p g n -> p (g n)"), in_=cx.SS[j])))(),
                  reads=[cx.t_SS[j]], writes=[t_sf[b]])
            sfv = sf[b].rearrange("p (h two) n -> p h two n", two=2)
            for gq in range(16):
                r = tmi % 4
                tmi += 1
                src = sf[b][:, gq, :]
                S.op("dve", (lambda gq=gq, src=src: (lambda e: e.max(mx[:, gq, 0:8], src)))(), reads=[t_sf[b]], writes=[t_mx])
                S.op("dve", (lambda gq=gq, src=src, r=r: (lambda e: e.match_replace(tm[r], mx[:, gq, 0:8], src, -BIGF)))(),
                     reads=[t_sf[b], t_mx], writes=[t_tm[r]])
                S.op("dve", (lambda gq=gq, r=r: (lambda e: e.max(mx[:, gq, 8:16], tm[r])))(), reads=[t_tm[r]], writes=[t_mx])
                if gq % 2 == 0:
                    h = gq // 2
                    S.op("dve", (lambda gq=gq, h=h, src=src: (lambda e: e.max_index(ix[:, h, 0:8], mx[:, gq, 0:8], src)))(),
                         reads=[t_sf[b], t_mx], writes=[t_ix])
                    S.op("dve", (lambda gq=gq, h=h, r=r: (lambda e: e.max_index(ix[:, h, 8:16], mx[:, gq, 8:16], tm[r])))(),
                         reads=[t_tm[r], t_mx], writes=[t_ix])
            S.op("dve", lambda e: e.tensor_copy(ixf, ix), reads=[t_ix], writes=[t_ixf])
            S.op("pool", lambda e: e.tensor_tensor(combo, s1top.unsqueeze(3).to_broadcast([128, 8, 16, 16]),
                                                    s2top.unsqueeze(2).to_broadcast([128, 8, 16, 16]), ALU.add),
                 reads=[t_mx], writes=[t_combo])
            for h in range(8):
                r = h % 2
                ch = combo[:, h, :, :].rearrange("p a b -> p (a b)")
                S.op("dve", (lambda h=h, ch=ch: (lambda e: e.max(tops[:, h, 0:8], ch)))(), reads=[t_combo], writes=[t_tops])
                S.op("dve", (lambda h=h, ch=ch, r=r: (lambda e: e.match_replace(ctmp[r], tops[:, h, 0:8], ch, -BIGF)))(),
                     reads=[t_combo, t_tops], writes=[t_ctmp[r]])
                S.op("dve", (lambda h=h, r=r: (lambda e: e.max(tops[:, h, 8:16], ctmp[r])))(), reads=[t_ctmp[r]], writes=[t_tops])
            S.op("dve", lambda e: e.tensor_scalar(sml[:, :, 0], tops[:, :, 0], -1.0, None, ALU.mult), reads=[t_tops], writes=[t_sml])
            S.op("dve", lambda e: e.tensor_scalar(sml[:, :, 3:5], mxv[:, :, :, 0], -1.0, None, ALU.mult), reads=[t_mx], writes=[t_sml])
            for h in range(8):
                S.op("act", (lambda h=h: (lambda e: e.activation(etop[:, h, :], tops[:, h, :], AF.Exp, bias=sml[:, h, 0:1],
                                                                 accum_out=sml[:, h, 1:2])))(),
                     reads=[t_tops, t_sml], writes=[t_etop, t_sml])
            S.op("dve", lambda e: e.reciprocal(sml[:, :, 2], sml[:, :, 1]), reads=[t_sml], writes=[t_sml])
            for h in range(8):
                S.op("act", (lambda h=h: (lambda e: e.activation(Aw[:, h, :], s1top[:, h, :], AF.Exp, bias=sml[:, h, 3:4])))(),
                     reads=[t_mx, t_sml], writes=[t_Aw])
                S.op("act", (lambda h=h, sfv=sfv: (lambda e: e.activation(Bw[:, h, :], sfv[:, h, 1, :], AF.Exp, bias=sml[:, h, 4:5])))(),
                     reads=[t_sf[b], t_sml], writes=[t_Bw])
            S.op("dve", lambda e: e.tensor_tensor(Aw, Aw, sml[:, :, 2:3].to_broadcast([128, 8, 16]), ALU.mult),
                 reads=[t_Aw, t_sml], writes=[t_Aw])
            S.op("dve", lambda e: e.tensor_tensor(Sel, combo, tops[:, :, 15:16].unsqueeze(3).to_broadcast([128, 8, 16, 16]), ALU.is_ge),
                 reads=[t_combo, t_tops], writes=[t_Sel])
            S.op("pool", lambda e: e.tensor_tensor(wa, Sel, s2top.unsqueeze(2).to_broadcast([128, 8, 16, 16]), ALU.mult),
                 reads=[t_Sel, t_mx], writes=[t_wa])
            S.op("dve", lambda e: e.tensor_scalar(wb, Sel, -BIGF, BIGF, ALU.mult, ALU.add), reads=[t_Sel], writes=[t_wb])
            S.op("dve", lambda e: e.tensor_tensor(wa, wa, wb, ALU.add), reads=[t_wa, t_wb], writes=[t_wa])
            S.op("dve", lambda e: e.tensor_reduce(phi, wa.rearrange("p h a b -> p (h a) b"), AX.X, ALU.min), reads=[t_wa], writes=[t_phi])
            S.op("dve", lambda e: e.tensor_scalar(phi, phi, -PHI_MARGIN, None, ALU.add), reads=[t_phi], writes=[t_phi])
            S.op("pe", lambda e: e.transpose(cx.banks[5][:, 0:128], ixf.rearrange("p h r -> p (h r)"), cx.identf),
                 reads=[t_ixf, cx.t_const], writes=[cx.t_bank[5]])
            S.op("pe", lambda e: e.transpose(cx.banks[5][:, 128:256], phi, cx.identf), reads=[t_phi, cx.t_const], writes=[cx.t_bank[5]])
            S.op("act", lambda e: e.activation(tT, cx.banks[5][:, 0:256], AF.Copy), reads=[cx.t_bank[5]], writes=[t_tT])
            S.op("dve", lambda e: e.tensor_tensor(L, iota.unsqueeze(1).to_broadcast([128, 128, 128]),
                                                  tT[:, 0:128].unsqueeze(2).to_broadcast([128, 128, 128]), ALU.is_equal),
                 reads=[t_iota, t_tT], writes=[t_L])
            Rj = R.rearrange("p t j -> p j t")
            for jc in range(32):
                j0 = jc * 4
                r3 = ri3 % 3
                ri3 += 1
                r2 = ri2 % 2
                ri2 += 1
                ba, bb = 1 + jc % 2, 3 + jc % 2
                srcS = sfv[:, :, 1, j0:j0 + 4].rearrange("p h j -> p j h").unsqueeze(3).to_broadcast([128, 4, 8, 16])
                srcB = Bw[:, :, j0:j0 + 4].rearrange("p h j -> p j h").unsqueeze(3).to_broadcast([128, 4, 8, 16])
                srcA = Aw.unsqueeze(1).to_broadcast([128, 4, 8, 16])
                S.op("act", (lambda r3=r3, srcS=srcS: (lambda e: e.activation(s2x[r3], srcS, AF.Copy)))(), reads=[t_sf[b]], writes=[t_s2x[r3]])
                S.op("pool", (lambda r3=r3, srcB=srcB, srcA=srcA: (lambda e: e.tensor_tensor(abc[r3], srcB, srcA, ALU.mult)))(),
                     reads=[t_Bw, t_Aw], writes=[t_abc[r3]])
                for jj in range(4):
                    S.op("pe", (lambda ba=ba, jj=jj, r3=r3: (lambda e: e.matmul(
                        cx.banks[ba][:, jj * 128:(jj + 1) * 128], s2x[r3][:, jj, :, :].rearrange("p h r -> p (h r)"), cx.identf,
                        start=True, stop=True)))(), reads=[t_s2x[r3], cx.t_const], writes=[cx.t_bank[ba]])
                for jj in range(4):
                    S.op("pe", (lambda bb=bb, jj=jj, r3=r3: (lambda e: e.matmul(
                        cx.banks[bb][:, jj * 128:(jj + 1) * 128], abc[r3][:, jj, :, :].rearrange("p h r -> p (h r)"), cx.identb,
                        start=True, stop=True)))(), reads=[t_abc[r3], cx.t_const], writes=[cx.t_bank[bb]])
                S.op("dve", (lambda ba=ba, r2=r2: (lambda e: e.tensor_tensor(
                    ind[r2], cx.banks[ba].rearrange("p (j t) -> p t j", j=4), tT[:, 128:256].unsqueeze(2).to_broadcast([128, 128, 4]), ALU.is_ge)))(),
                    reads=[cx.t_bank[ba], t_tT], writes=[t_ind[r2]])
                S.op("dve", (lambda bb=bb, r2=r2, j0=j0: (lambda e: e.tensor_tensor(
                    R[:, :, j0:j0 + 4], ind[r2], cx.banks[bb].rearrange("p (j t) -> p t j", j=4), ALU.mult)))(),
                    reads=[cx.t_bank[bb], t_ind[r2]], writes=[t_R])
            MTv = MTt.rearrange("p j t -> p t j")
            for t4 in range(32):
                bm = 6 + t4 % 2
                for tt in range(4):
                    t = t4 * 4 + tt
                    S.op("pe", (lambda bm=bm, tt=tt, t=t: (lambda e: e.matmul(
                        cx.banks[bm][:, tt * 128:(tt + 1) * 128], L[:, t, :], R[:, t, :], start=True, stop=True)))(),
                        reads=[t_L, t_R], writes=[cx.t_bank[bm]])
                S.op("act", (lambda bm=bm, t4=t4: (lambda e: e.activation(
                    MTt[:, :, t4 * 4:(t4 + 1) * 4], cx.banks[bm].rearrange("p (t j) -> p j t", t=4), AF.Copy)))(),
                    reads=[cx.t_bank[bm]], writes=[t_MTt])
            S.dma("sp", (lambda j=j: (lambda e: e.dma_start(out=cx.MT[j], in_=MTt)))(), reads=[t_MTt], writes=[cx.t_MT[j]])
        phase_end(cx)


def phase_p4a(cx, nblocks=NB):
    nc, S = cx.nc, cx.S
    _declare_peer(cx)
    with ExitStack() as es:
        A_ = lambda name, shape, dt: _alloc(cx, es, name, shape, dt)
        iota = A_("iota", [128, 128], F32)
        t_iota = Tok()
        mxl = [A_(f"mxl{i}", [128, 512], F32) for i in range(2)]
        t_mxl = toks("mxl", 2)
        tm = [A_(f"tm{i}", [128, 128], F32) for i in range(16)]
        t_tm = toks("tm", 16)
        t_mxg = toks("mxg", 16)
        t_ixg = toks("ixg", 16)
        t_topg = toks("topg", 8)
        t_posg = toks("posg", 8)
        mx = A_("mx", [128, 16, 16], F32)
        ix = A_("ix", [128, 16, 16], U32)
        ixf = A_("ixf", [128, 16, 16], F32)
        combo = A_("combo", [128, 8, 16, 16], F32)
        ctmp = [A_(f"ctmp{i}", [128, 256], F32) for i in range(8)]
        t_ctmp = toks("ctmp", 8)
        tops = A_("tops", [128, 8, 16], F32)
        pos = A_("pos", [128, 8, 16], U32)
        pab = A_("pab", [128, 2, 8, 16], U32)
        pabf = A_("pabf", [128, 2, 8, 16], F32)
        sml = A_("sml", [128, 8, 4], F32)
        E1 = A_("E1", [128, 8, 16, 16], F32)
        E2 = A_("E2", [128, 8, 16, 16], F32)
        IJg = A_("IJg", [128, 3, 8, 16], F32)
        tTs = [A_(f"tT{i}", [128, 3, 128], F32) for i in range(2)]
        L = A_("L", [128, 128, 128], BF16)
        R = A_("R", [128, 128, 128], BF16)
        MTt = [A_(f"MTt{i}", [128, 128, 128], BF16) for i in range(2)]
        t_MTt = toks("MTt", 2)
        (t_mx, t_ix, t_ixf, t_combo, t_tops, t_pos, t_pab, t_pabf, t_sml, t_E1, t_E2, t_I, t_J, t_g, t_tT0, t_L, t_R) = [Tok() for _ in range(17)]
        t_tTs = [t_tT0, Tok()]
        S.dma("sp", lambda e: e.dma_start(out=iota, in_=cx.iota), writes=[t_iota])
        mxv = mx.rearrange("p (h two) r -> p h two r", two=2)
        s1top, s2top = mxv[:, :, 0, :], mxv[:, :, 1, :]
        ixv = ixf.rearrange("p (h two) r -> p h two r", two=2)
        idx1f, idx2f = ixv[:, :, 0, :], ixv[:, :, 1, :]
        io16 = iota[:, 0:16]
        tmi = 0
        mxl3 = [A_(f"mxl3{i}", [128, 512], F32) for i in range(3)]
        t_mxl3 = toks("mxl3", 3)
        tops2 = [A_(f"tops2{i}", [128, 8, 16], F32) for i in range(2)]
        pos2 = [A_(f"pos2{i}", [128, 8, 16], U32) for i in range(2)]
        negm2 = [A_(f"negm2{i}", [128, 8], F32) for i in range(2)]
        Z2 = [A_(f"Z2{i}", [128, 8], F32) for i in range(2)]
        rZ = A_("rZ", [128, 8], F32)
        IJg2 = [A_(f"IJg2{i}", [128, 3, 8, 16], F32) for i in range(2)]
        t_topg2 = [toks("topg2a", 8), toks("topg2b", 8)]
        t_posg2 = [toks("posg2a", 8), toks("posg2b", 8)]
        t_negm2, t_Z2, t_e2 = toks("negm2", 2), toks("Z2", 2), toks("e2", 2)
        t_rZ = Tok()

        def front_a(j):
            b3, b2 = j % 3, j % 2
            tops, pos, negm, Zt, IJg = tops2[b2], pos2[b2], negm2[b2], Z2[b2], IJg2[b2]
            t_topg, t_posg = t_topg2[b2], t_posg2[b2]
            S.dma("sp", lambda e: e.dma_start(out=mxl3[b3], in_=cx.MXS[j]), reads=[cx.t_SS[j]], writes=[t_mxl3[b3]])
            mx = mxl3[b3][:, 0:256].rearrange("p (g r) -> p g r", g=16)
            mxv = mx.rearrange("p (h two) r -> p h two r", two=2)
            s1top, s2top = mxv[:, :, 0, :], mxv[:, :, 1, :]
            S.op("dve", lambda e: e.tensor_tensor(combo, s1top.unsqueeze(3).to_broadcast([128, 8, 16, 16]),
                                                  s2top.unsqueeze(2).to_broadcast([128, 8, 16, 16]), ALU.add),
                 reads=[t_mxl3[b3]], writes=[t_combo])
            chs = [combo[:, h, :, :].rearrange("p a b -> p (a b)") for h in range(8)]
            for h in range(8):
                S.op("dve", (lambda h=h: (lambda e: e.max(tops[:, h, 0:8], chs[h])))(), reads=[t_combo], writes=[t_topg[h]])
            for h in range(8):
                S.op("dve", (lambda h=h: (lambda e: e.match_replace(ctmp[h], tops[:, h, 0:8], chs[h], -BIGF)))(),
                     reads=[t_combo, t_topg[h]], writes=[t_ctmp[h]])
            for h in range(8):
                S.op("dve", (lambda h=h: (lambda e: e.max_index(pos[:, h, 0:8], tops[:, h, 0:8], chs[h])))(),
                     reads=[t_combo, t_topg[h]], writes=[t_posg[h]])
            for h in range(8):
                S.op("dve", (lambda h=h: (lambda e: e.max(tops[:, h, 8:16], ctmp[h])))(), reads=[t_ctmp[h]], writes=[t_topg[h]])
            for h in range(8):
                S.op("dve", (lambda h=h: (lambda e: e.max_index(pos[:, h, 8:16], tops[:, h, 8:16], ctmp[h])))(),
                     reads=[t_ctmp[h], t_topg[h]], writes=[t_posg[h]])
            S.op("dve", lambda e: e.tensor_scalar(negm, tops[:, :, 0], -1.0, None, ALU.mult), reads=t_topg, writes=[t_negm2[b2]])
            for h in range(8):
                S.op("act", (lambda h=h: (lambda e: e.activation(IJg[:, 2, h, :], tops[:, h, :], AF.Exp, bias=negm[:, h:h + 1],
                                                                 accum_out=Zt[:, h:h + 1])))(),
                     reads=t_topg + [t_negm2[b2]], writes=[t_e2[b2], t_Z2[b2]])

        def front_b(j):
            b3, b2 = j % 3, j % 2
            pos, Zt, IJg = pos2[b2], Z2[b2], IJg2[b2]
            t_posg = t_posg2[b2]
            tT, t_tT = tTs[j % 2], t_tTs[j % 2]
            ixf = mxl3[b3][:, 256:512].rearrange("p (g r) -> p g r", g=16)
            ixv = ixf.rearrange("p (h two) r -> p h two r", two=2)
            idx1f, idx2f = ixv[:, :, 0, :], ixv[:, :, 1, :]
            S.op("dve", lambda e: e.reciprocal(rZ, Zt), reads=[t_Z2[b2]], writes=[t_rZ])
            S.op("dve", lambda e: e.tensor_tensor(IJg[:, 2, :, :], IJg[:, 2, :, :], rZ.unsqueeze(2).to_broadcast([128, 8, 16]), ALU.mult),
                 reads=[t_e2[b2], t_rZ], writes=[t_g])
            S.op("dve", lambda e: e.tensor_scalar(pab[:, 0, :, :], pos, 4, None, ALU.logical_shift_right), reads=t_posg, writes=[t_pab])
            S.op("dve", lambda e: e.tensor_scalar(pab[:, 1, :, :], pos, 15, None, ALU.bitwise_and), reads=t_posg, writes=[t_pab])
            S.op("pool", lambda e: e.tensor_copy(pabf, pab), reads=[t_pab], writes=[t_pabf])
            io4 = io16.unsqueeze(1).unsqueeze(1).to_broadcast([128, 8, 16, 16])
            S.op("dve", lambda e: e.tensor_tensor(E1, io4, pabf[:, 0, :, :].unsqueeze(3).to_broadcast([128, 8, 16, 16]), ALU.is_equal),
                 reads=[t_iota, t_pabf], writes=[t_E1])
            S.op("dve", lambda e: e.tensor_tensor(E1, E1, idx1f.unsqueeze(2).to_broadcast([128, 8, 16, 16]), ALU.mult),
                 reads=[t_E1, t_mxl3[b3]], writes=[t_E1])
            S.op("dve", lambda e: e.tensor_tensor(E2, io4, pabf[:, 1, :, :].unsqueeze(3).to_broadcast([128, 8, 16, 16]), ALU.is_equal),
                 reads=[t_iota, t_pabf], writes=[t_E2])
            S.op("dve", lambda e: e.tensor_tensor(E2, E2, idx2f.unsqueeze(2).to_broadcast([128, 8, 16, 16]), ALU.mult),
                 reads=[t_E2, t_mxl3[b3]], writes=[t_E2])
            S.op("dve", lambda e: e.tensor_reduce(IJg[:, 0, :, :].rearrange("p h k -> p (h k)"), E1.rearrange("p h k a -> p (h k) a"), AX.X, ALU.add),
                 reads=[t_E1], writes=[t_I])
            S.op("dve", lambda e: e.tensor_reduce(IJg[:, 1, :, :].rearrange("p h k -> p (h k)"), E2.rearrange("p h k a -> p (h k) a"), AX.X, ALU.add),
                 reads=[t_E2], writes=[t_J])
            for q3, tk in ((0, t_I), (1, t_J), (2, t_g)):
                S.op("pe", (lambda q3=q3: (lambda e: e.transpose(cx.banks[5][:, q3 * 128:(q3 + 1) * 128],
                                                                  IJg[:, q3, :, :].rearrange("p h k -> p (h k)"), cx.identf)))(),
                     reads=[tk, cx.t_const], writes=[cx.t_bank[5]])
            S.op("dve", lambda e: e.tensor_copy(tT.rearrange("p a t -> p (a t)"), cx.banks[5][:, 0:384]), reads=[cx.t_bank[5]], writes=[t_tT])

        def back(j):
            tT, t_tT = tTs[j % 2], t_tTs[j % 2]
            io3 = iota.unsqueeze(1).to_broadcast([128, 128, 128])
            Lf = L.rearrange("p t i -> p (t i)")
            S.op("dve", lambda e: e.tensor_tensor(L, io3, tT[:, 0, :].unsqueeze(2).to_broadcast([128, 128, 128]), ALU.is_equal),
                 reads=[t_iota, t_tT], writes=[t_L])
            S.op("dve", lambda e: e.tensor_tensor(R, io3, tT[:, 1, :].unsqueeze(2).to_broadcast([128, 128, 128]), ALU.is_equal),
                 reads=[t_iota, t_tT], writes=[t_R])
            S.op("pool", lambda e: e.tensor_tensor(R, R, tT[:, 2, :].unsqueeze(2).to_broadcast([128, 128, 128]), ALU.mult),
                 reads=[t_R, t_tT], writes=[t_R])
            mb_ = j % 2
            for t4 in range(32):
                bm = 6 + t4 % 2
                for tt in range(4):
                    t = t4 * 4 + tt
                    S.op("pe", (lambda bm=bm, tt=tt, t=t: (lambda e: e.matmul(
                        cx.banks[bm][:, tt * 128:(tt + 1) * 128], L[:, t, :], R[:, t, :], start=True, stop=True)))(),
                        reads=[t_L, t_R], writes=[cx.t_bank[bm]])
                S.op("act", (lambda bm=bm, t4=t4, mb_=mb_: (lambda e: e.activation(
                    MTt[mb_][:, :, t4 * 4:(t4 + 1) * 4], cx.banks[bm].rearrange("p (t j) -> p j t", t=4), AF.Copy)))(),
                    reads=[cx.t_bank[bm]], writes=[t_MTt[mb_]])
            S.dma("act", (lambda j=j, mb_=mb_: (lambda e: e.dma_start(out=cx.MT[j], in_=MTt[mb_])))(), reads=[t_MTt[mb_]], writes=[cx.t_MT[j]])

        front_a(0)
        if nblocks > 1:
            front_a(1)
        front_b(0)
        for j in range(nblocks):
            if j + 2 < nblocks:
                front_a(j + 2)
            if j + 1 < nblocks:
                front_b(j + 1)
            back(j)
        phase_end(cx)


def phase_p4b(cx, nblocks=NB):
    nc, S = cx.nc, cx.S
    _declare_peer(cx)
    LAG = 2
    with ExitStack() as es:
        load_mod(cx, es)
        A_ = lambda name, shape, dt: _alloc(cx, es, name, shape, dt)
        utb = [A_(f"utb{i}", [128, 8, 1024], BF16) for i in range(3)]
        vb = [A_(f"vb{i}", [128, 8, 1024], BF16) for i in range(3)]
        mtb = [A_(f"mtb{i}", [128, 2, 8, 128], BF16) for i in range(3)]
        t_utb, t_vb, t_mtb = toks("utb", 3), toks("vb", 3), toks("mtb", 3)
        h2g = [A_(f"h2g{i}", [128, 8, 256], BF16) for i in range(2)]
        t_h2g = toks("h2g", 2)
        ga = [A_(f"ga{i}", [128, 256], F32) for i in range(3)]
        ptl = [A_(f"ptl{i}", [128, 256], BF16) for i in range(4)]
        t_ga, t_ptl = toks("ga", 3), toks("ptl", 4)
        x1t = [A_(f"x1t{i}", [128, D], F32) for i in range(2)]
        tmp = A_("tmp4", [128, D], F32)
        t_x1t, t_tmp = toks("x1t", 2), Tok()
        G2 = cx.modbc[:, 5 * D:6 * D]
        UTd = cx.UTb.rearrange("k p c -> p k c")
        Vbd = cx.Vb.rearrange("j i d -> i j d")
        ngroups = nblocks // 2
        PA = (4, 5, 6)

        def load_block(g, jb):
            b = (g * 16 + jb) % 3
            S.dma("sp", lambda e: e.dma_start(out=utb[b], in_=UTd[:, :, jb * 1024:(jb + 1) * 1024]),
                  reads=[cx.t_UTb2[k][jb // 2] for k in range(8)], writes=[t_utb[b]])
            S.dma("act", lambda e: e.dma_start(out=vb[b], in_=Vbd[:, jb * 8:(jb + 1) * 8, :]),
                  reads=[cx.t_Vb2[jb * 2], cx.t_Vb2[jb * 2 + 1]], writes=[t_vb[b]])
            for tt in range(2):
                S.dma("sp", (lambda tt=tt: (lambda e: e.dma_start(out=mtb[b][:, tt, :, :], in_=cx.MT[2 * g + tt][:, jb * 8:(jb + 1) * 8, :])))(),
                      reads=[cx.t_MT[2 * g + tt]], writes=[t_mtb[b]])

        def load_h2(g):
            for tt in range(2):
                S.dma("sp", (lambda tt=tt: (lambda e: e.dma_start(out=h2g[g % 2][:, :, tt * 128:(tt + 1) * 128], in_=cx.H2T[2 * g + tt])))(),
                      reads=[cx.t_H2T[2 * g + tt]], writes=[t_h2g[g % 2]])

        def stage_u(g, j):
            jb, jj = j // 8, j % 8
            b = (g * 16 + jb) % 3
            it = g * 128 + j
            ba = PA[it % 3]
            gr = it % 3
            pr = it % 4
            for k in range(8):
                S.op("pe", (lambda k=k: (lambda e: e.matmul(
                    cx.banks[ba][:, 0:256], utb[b][:, k, jj * 128:(jj + 1) * 128], h2g[g % 2][:, k, :], start=(k == 0), stop=(k == 7))))(),
                    reads=[t_utb[b], t_h2g[g % 2]], writes=[cx.t_bank[ba]])
            S.op("act", lambda e: e.activation(ga[gr], cx.banks[ba][:, 0:256], GELU_FUNC), reads=[cx.t_bank[ba]], writes=[t_ga[gr]])
            S.op("dve", lambda e: e.tensor_tensor(ptl[pr].rearrange("p (a t) -> p a t", a=2), ga[gr].rearrange("p (a t) -> p a t", a=2),
                                                  mtb[b][:, :, jj, :], ALU.mult),
                 reads=[t_ga[gr], t_mtb[b]], writes=[t_ptl[pr]])

        def stage_v(g, j):
            jb, jj = j // 8, j % 8
            b = (g * 16 + jb) % 3
            it = g * 128 + j
            pr = it % 4
            for tt in range(2):
                for half in range(2):
                    bo = tt * 2 + half
                    S.op("pe", (lambda tt=tt, half=half, bo=bo: (lambda e: e.matmul(
                        cx.banks[bo], ptl[pr][:, tt * 128:(tt + 1) * 128], vb[b][:, jj, half * 512:(half + 1) * 512],
                        start=(j == 0), stop=(j == 127))))(),
                        reads=[t_ptl[pr], t_vb[b]], writes=[cx.t_bank[bo]])

        def epilogue(g):
            for tt in range(2):
                jt = 2 * g + tt
                xb = jt % 2
                S.dma("sp", (lambda jt=jt, xb=xb: (lambda e: e.dma_start(out=x1t[xb], in_=cx.X1[jt * 128:(jt + 1) * 128, :])))(),
                      reads=[cx.t_X1[jt]], writes=[t_x1t[xb]])
                for half in range(2):
                    bo = tt * 2 + half
                    S.op("dve", (lambda bo=bo, half=half: (lambda e: e.tensor_tensor(
                        tmp[:, half * 512:(half + 1) * 512], cx.banks[bo], G2[:, half * 512:(half + 1) * 512], ALU.mult)))(),
                        reads=[cx.t_bank[bo], cx.t_mod], writes=[t_tmp])
                S.op("pool", (lambda xb=xb: (lambda e: e.tensor_tensor(x1t[xb], tmp, x1t[xb], ALU.add)))(),
                     reads=[t_tmp, t_x1t[xb]], writes=[t_x1t[xb]])
                S.dma("pool", (lambda jt=jt, xb=xb: (lambda e: e.dma_start(out=cx.out[jt * 128:(jt + 1) * 128, :], in_=x1t[xb])))(),
                      reads=[t_x1t[xb]], writes=[cx.t_outs[jt]])

        seq = [(g, j) for g in range(ngroups) for j in range(128)]
        blocks = [(g, jb) for g in range(ngroups) for jb in range(16)]
        load_h2(0)
        for bi_ in range(min(3, len(blocks))):
            load_block(*blocks[bi_])
        for s_i in range(len(seq) + LAG):
            if s_i < len(seq):
                g, j = seq[s_i]
                if j == 64 and g + 1 < ngroups:
                    load_h2(g + 1)
                stage_u(g, j)
            if s_i - LAG >= 0:
                g, j = seq[s_i - LAG]
                stage_v(g, j)
                if j % 8 == 7:
                    nb_i = (g * 16 + j // 8) + 3
                    if nb_i < len(blocks):
                        load_block(*blocks[nb_i])
                if j == 127:
                    epilogue(g)
        phase_end(cx)


GELU_FUNC = AF.Gelu
PHASES["p4a"] = phase_p4a
PHASES["p4b"] = phase_p4b


ALL_PHASES = ["ada", "p1a", "p1b", "p2", "p3", "pcast", "p4q", "p4a", "p4b"]


def kernel(**inputs):
    sh = prep_shared(inputs)
    nc, cx = build_program(ALL_PHASES)
    in_maps = [prep_core(inputs, sh, c) for c in range(8)]
    res = run_bass_kernel_spmd(nc, in_maps, core_ids=list(range(8)))
    out = np.empty((4, SEQ, D), np.float32)
    for c in range(8):
        o = np.asarray(res.results[c]["out"])
        for j, g in enumerate(own_blocks(c % 2)):
            out[c // 2, g * 128:(g + 1) * 128, :] = o[j * 128:(j + 1) * 128, :]
    return out
```

```python
import numpy as np
import concourse.bass as bass
import concourse.mybir as mybir
from concourse.bass_utils import run_bass_kernel_spmd

F32 = mybir.dt.float32
BF16 = mybir.dt.bfloat16
ALU = mybir.AluOpType
AF = mybir.ActivationFunctionType
AX = mybir.AxisListType


class Tok:
    __slots__ = ("name", "w", "r")

    def __init__(self, name=""):
        self.name = name
        self.w = None
        self.r = []


def toks(name, n):
    return [Tok(f"{name}{i}") for i in range(n)]


class Ev:
    __slots__ = ("sem", "val", "eng", "know")

    def __init__(self, sem, val, eng, know):
        self.sem = sem
        self.val = val
        self.eng = eng
        self.know = know


class Sched:
    ENG = ("pe", "act", "dve", "pool", "sp")
    NDMA = 8

    def __init__(self, nc):
        self.nc = nc
        self.streams = {e: [] for e in self.ENG}
        self.sem = {e: nc.alloc_semaphore(f"s_{e}") for e in self.ENG}
        self.cnt = {e: 0 for e in self.ENG}
        self.know = {e: {} for e in self.ENG}
        self.dsem = {e: [nc.alloc_semaphore(f"d_{e}{i}") for i in range(self.NDMA)]
                     for e in ("sp", "act", "pool")}
        self.dcnt = {e: [0] * self.NDMA for e in self.dsem}
        self.dnext = {e: 0 for e in self.dsem}
        self.dlast = {e: [None] * self.NDMA for e in self.dsem}
        self.all_events = []
        self.last_ev = {}
        self.n_ops = 0

    def _need(self, eng, ev, waits):
        if ev is None:
            return
        k = self.know[eng]
        key = id(ev.sem)
        if k.get(key, 0) >= ev.val:
            return
        cur = waits.get(key)
        if cur is None or cur.val < ev.val:
            waits[key] = ev

    def _absorb(self, eng, ev):
        k = self.know[eng]
        for kk, vv in ev.know.items():
            if k.get(kk, 0) < vv:
                k[kk] = vv
        key = id(ev.sem)
        if k.get(key, 0) < ev.val:
            k[key] = ev.val

    def _deps(self, eng, reads, writes, same_eng_raw_only=True):
        waits = {}
        for t in reads:
            self._need(eng, t.w, waits)
        for t in writes:
            if t.w is not None and not (t.w.eng == eng and eng == "pe"):
                self._need(eng, t.w, waits)
            for ev in t.r:
                if ev.eng == eng and eng == "pe":
                    continue
                self._need(eng, ev, waits)
        return waits

    def op(self, eng, fn, reads=(), writes=()):
        waits = self._deps(eng, reads, writes)
        wl = list(waits.values())
        for ev in wl:
            self._absorb(eng, ev)
        self.cnt[eng] += 1
        ev = Ev(self.sem[eng], self.cnt[eng], eng, dict(self.know[eng]))
        self.streams[eng].append(([(w.sem, w.val) for w in wl], fn, (self.sem[eng], 1)))
        for t in reads:
            t.r.append(ev)
        for t in writes:
            t.w = ev
            t.r = []
        self.last_ev[("c", eng)] = ev
        self.n_ops += 1
        return ev

    def dma(self, q, fn, reads=(), writes=()):
        waits = self._deps(q, reads, writes)
        i = self.dnext[q]
        self.dnext[q] = (i + 1) % self.NDMA
        prev = self.dlast[q][i]
        if prev is not None:
            self._need(q, prev, waits)
        wl = list(waits.values())
        for ev in wl:
            self._absorb(q, ev)
        self.dcnt[q][i] += 16
        sem = self.dsem[q][i]
        ev = Ev(sem, self.dcnt[q][i], "dma_" + q, dict(self.know[q]))
        self.dlast[q][i] = ev
        self.streams[q].append(([(w.sem, w.val) for w in wl], fn, (sem, 16)))
        for t in reads:
            t.r.append(ev)
        for t in writes:
            t.w = ev
            t.r = []
        self.last_ev[("d", q, i)] = ev
        self.n_ops += 1
        return ev

    def barrier(self):
        evs = list(self.last_ev.values())
        for eng in self.ENG:
            waits = {}
            for ev in evs:
                self._need(eng, ev, waits)
            wl = list(waits.values())
            if not wl:
                continue
            for ev in wl:
                self._absorb(eng, ev)
            self.streams[eng].append(([(w.sem, w.val) for w in wl], None, None))

    def final_wait(self, eng="sp"):
        evs = list(self.last_ev.values())
        waits = {}
        for ev in evs:
            self._need(eng, ev, waits)
        wl = list(waits.values())
        for ev in wl:
            self._absorb(eng, ev)
        self.streams[eng].append(([(w.sem, w.val) for w in wl], None, None))

    def emit(self):
        nc = self.nc
        streams = self.streams

        def run(engine, items):
            for waits, fn, inc in items:
                for sem, val in waits:
                    engine.wait_ge(sem, val)
                if fn is not None:
                    ins = fn(engine)
                    ins.then_inc(inc[0], inc[1])

        with nc.Block() as block:
            @block.tensor
            def _(e):
                run(e, streams["pe"])

            @block.scalar
            def _(e):
                run(e, streams["act"])

            @block.vector
            def _(e):
                run(e, streams["dve"])

            @block.gpsimd
            def _(e):
                run(e, streams["pool"])

            @block.sync
            def _(e):
                run(e, streams["sp"])


from contextlib import ExitStack

D = 1024
SEQ = 8192
NT = SEQ // 128
NB = 32
C_Q, C_K, C_V, C_CB, C_CC, C_CX, C_IQ, C_IK, C_IW = 0, 512, 1024, 1536, 2048, 2560, 3072, 3584, 3648
DIN = 3656
EPS = 1e-6
NEG = -30000.0
NIT = 17
TOPK = 256.0
U32 = mybir.dt.uint32


def nchunks(j):
    g = j // 2
    return 4 * g + 2 if j % 2 == 0 else 4 * g + 4


def own_blocks(half):
    res = []
    for g in range(16):
        res += [4 * g + (0 if half == 0 else 1), 4 * g + (3 if half == 0 else 2)]
    return res


class Ctx:
    pass


_UNIQ = [0]


def _alloc(cx, es, name, shape, dtype):
    _UNIQ[0] += 1
    t = es.enter_context(cx.nc.sbuf_tensor(f"{name}_{_UNIQ[0]}", list(shape), dtype))
    return t.ap()


def declare(nc, dbg_out=(), dbg_in=()):
    cx = Ctx()
    cx.nc = nc

    def inp(name, shape, dt=F32):
        return nc.dram_tensor(name, list(shape), dt, kind="ExternalInput").ap()

    def scr(name, shape, dt):
        kind = "Internal"
        if name in dbg_out:
            kind = "ExternalOutput"
        if name in dbg_in:
            kind = "ExternalInput"
        return nc.dram_tensor(name, list(shape), dt, kind=kind).ap()

    cx.xseq = inp("xseq", [SEQ, D])
    cx.xown = inp("xown", [NB * 128, D])
    cx.xhalo = inp("xhalo", [64, D])
    cx.cT = inp("cT", [128, 8])
    cx.w_ada = inp("w_ada", [D, 6 * D])
    cx.b_ada = inp("b_ada", [1, 6 * D])
    cx.norm1_w = inp("norm1_w", [1, D])
    cx.norm2_w = inp("norm2_w", [1, D])
    cx.w_in = inp("w_in", [D, DIN])
    cx.pp = inp("pp", [128, 32])
    cx.aonw = inp("aonw", [1, 512])
    cx.w_out = inp("w_out", [D, D])
    cx.peer_wq = inp("peer_wq", [D, 2048])
    cx.skT = inp("skT", [16, 128, 128])
    cx.UT = inp("UT", [D, 16384])
    cx.VH = inp("VH", [16384, D])
    cx.ident = inp("ident", [128, 128])
    cx.hmask = inp("hmask", [128, 64])
    cx.iota = inp("iota", [128, 128])
    cx.cmask = inp("cmask", [128, 2, 256])
    cx.out = nc.dram_tensor("out", [NB * 128, D], F32, kind="ExternalOutput").ap()
    cx.KT = scr("KT", [4, 128, SEQ], BF16)
    cx.VP = scr("VP", [NT, 128, 520], BF16)
    cx.IKT = scr("IKT", [64, SEQ], BF16)
    cx.QT = scr("QT", [NB, 128, 4, 128], BF16)
    cx.IQT = scr("IQT", [NB, 64, 8, 128], BF16)
    cx.IW = scr("IW", [NB, 128, 8], F32)
    cx.YCT = scr("YCT", [NB, 128, 4, 128], BF16)
    cx.MB = scr("MB", [NB, 128, SEQ], BF16)
    cx.X1 = scr("X1", [NB * 128, D], F32)
    cx.H2T = scr("H2T", [NB, 128, 8, 128], BF16)
    cx.MT = scr("MT", [NB, 128, 128, 128], BF16)
    cx.UTb = scr("UTb", [8, 128, 16384], BF16)
    cx.Vb = scr("Vb", [128, 128, D], BF16)
    return cx


def setup_persistent(cx):
    nc = cx.nc
    S = cx.S = Sched(nc)
    A = lambda name, shape, dt: nc.alloc_sbuf_tensor(name, list(shape), dt).ap()
    cx.identb = A("identb", [128, 128], BF16)
    cx.identf = A("identf", [128, 128], F32)
    cx.eps_t = A("eps_t", [128, 1], F32)
    cx.ppt = A("ppt", [128, 32], F32)
    cx.bd64 = A("bd64", [128, 128], BF16)
    cx.ones512 = A("ones512", [128, 128], BF16)
    cx.t_mod = Tok("modbc")
    cx.MODD = nc.dram_tensor("MODD", [128, 6 * D], F32).ap()
    cx.t_MODD = Tok("MODD")
    cx.t_const = Tok("const")
    cx.banks = [nc.alloc_psum_tensor(f"bank{i}", [128, 512], F32).ap() for i in range(8)]
    cx.t_bank = toks("bank", 8)
    cx.t_KT, cx.t_VP, cx.t_IKT = toks("KT", NT), toks("VP", NT), toks("IKT", NT)
    cx.t_QT, cx.t_IQT, cx.t_IW, cx.t_YCT = toks("QT", NB), toks("IQT", NB), toks("IW", NB), toks("YCT", NB)
    cx.t_MB, cx.t_X1, cx.t_H2T, cx.t_MT = toks("MB", NB), toks("X1", NB), toks("H2T", NB), toks("MT", NB)
    cx.t_UTb, cx.t_Vb = toks("UTb", 16), toks("Vb", 16)
    cx.t_outs = toks("out", NB)
    S.dma("sp", lambda e: e.dma_start(out=cx.identf, in_=cx.ident), writes=[cx.t_const])
    S.dma("pool", lambda e: e.dma_start(out=cx.identb, in_=cx.ident), writes=[cx.t_const])
    S.dma("sp", lambda e: e.dma_start(out=cx.ppt, in_=cx.pp), writes=[cx.t_const])
    S.op("dve", lambda e: e.memset(cx.eps_t, EPS), writes=[cx.t_const])
    S.op("dve", lambda e: e.memset(cx.bd64, 0.0), writes=[cx.t_const])
    S.op("dve", lambda e: e.memset(cx.bd64[0:64, 0:64], 1.0 / 64), writes=[cx.t_const])
    S.op("dve", lambda e: e.memset(cx.bd64[64:128, 64:128], 1.0 / 64), writes=[cx.t_const])
    S.op("dve", lambda e: e.memset(cx.ones512, 1.0 / 512), writes=[cx.t_const])


def load_mod(cx, es):
    cx.modbc = _alloc(cx, es, "modbc", [128, 6 * D], F32)
    cx.t_mod = Tok("modbc")
    for q in range(6):
        cx.S.dma("sp" if q % 2 == 0 else "act", (lambda q=q: (lambda e: e.dma_start(out=cx.modbc[:, q * D:(q + 1) * D], in_=cx.MODD[:, q * D:(q + 1) * D])))(),
                 reads=[cx.t_MODD], writes=[cx.t_mod])


def phase_end(cx):
    cx.S.barrier()
    cx.S.emit()
    cx.S.streams = {e: [] for e in Sched.ENG}


def phase_ada(cx):
    nc, S = cx.nc, cx.S
    with ExitStack() as es:
        cx.modbc = _alloc(cx, es, "modbc", [128, 6 * D], F32)
        ct = _alloc(cx, es, "ct", [128, 8], F32)
        sc = _alloc(cx, es, "sc", [128, 8], F32)
        scb = _alloc(cx, es, "scb", [128, 8, 128], F32)
        bbc = _alloc(cx, es, "bbc", [128, 6 * D], F32)
        n12 = _alloc(cx, es, "n12", [128, 2, D], F32)
        wa = [_alloc(cx, es, f"wa{i}", [128, 8, 512], F32) for i in range(2)]
        t_wa = toks("wa", 2)
        t_ct, t_sc, t_scb, t_bbc, t_n = Tok(), Tok(), Tok(), Tok(), Tok()
        S.dma("sp", lambda e: e.dma_start(out=ct, in_=cx.cT), writes=[t_ct])
        S.dma("sp", lambda e: e.dma_start(out=bbc, in_=cx.b_ada.partition_broadcast(128).rearrange("p a b -> p (a b)")),
              writes=[t_bbc])
        S.dma("act", lambda e: e.dma_start(out=n12[:, 0, :], in_=cx.norm1_w.partition_broadcast(128).rearrange("p a b -> p (a b)")),
              writes=[t_n])
        S.dma("act", lambda e: e.dma_start(out=n12[:, 1, :], in_=cx.norm2_w.partition_broadcast(128).rearrange("p a b -> p (a b)")),
              writes=[t_n])
        S.op("act", lambda e: e.activation(sc, ct, AF.Silu), reads=[t_ct], writes=[t_sc])
        S.op("dve", lambda e: e.tensor_copy(scb, sc.unsqueeze(2).to_broadcast([128, 8, 128])), reads=[t_sc], writes=[t_scb])
        wsrc = cx.w_ada.rearrange("(k p) n -> p k n", p=128)
        for cg in range(12):
            b = cg % 2
            q = "sp" if cg % 2 == 0 else "act"
            S.dma(q, (lambda cg=cg, b=b: (lambda e: e.dma_start(out=wa[b], in_=wsrc[:, :, cg * 512:(cg + 1) * 512])))(),
                  writes=[t_wa[b]])
            for k in range(8):
                S.op("pe", (lambda k=k, b=b: (lambda e: e.matmul(cx.banks[b], scb[:, k, :], wa[b][:, k, :],
                                                                  start=(k == 0), stop=(k == 7))))(),
                     reads=[t_scb, t_wa[b]], writes=[cx.t_bank[b]])
            S.op("dve", (lambda cg=cg, b=b: (lambda e: e.tensor_tensor(cx.modbc[:, cg * 512:(cg + 1) * 512], cx.banks[b],
                                                                        bbc[:, cg * 512:(cg + 1) * 512], ALU.add)))(),
                 reads=[cx.t_bank[b], t_bbc], writes=[cx.t_mod])
        for (o, n) in ((1, 0), (4, 1)):
            S.op("dve", (lambda o=o, n=n: (lambda e: e.scalar_tensor_tensor(
                cx.modbc[:, o * D:(o + 1) * D], cx.modbc[:, o * D:(o + 1) * D], 1.0, n12[:, n, :], ALU.add, ALU.mult)))(),
                 reads=[cx.t_mod, t_n], writes=[cx.t_mod])
        S.dma("sp", lambda e: e.dma_start(out=cx.MODD, in_=cx.modbc), reads=[cx.t_mod], writes=[cx.t_MODD])
        phase_end(cx)


class NormBufs:
    def __init__(self, cx, es, tag):
        self.junk = _alloc(cx, es, f"nj{tag}", [128, D], BF16)
        self.ss = _alloc(cx, es, f"nss{tag}", [128, 1], F32)
        self.rt = _alloc(cx, es, f"nrt{tag}", [128, 1], F32)
        self.rstd = _alloc(cx, es, f"nrs{tag}", [128, 1], F32)
        self.t1 = _alloc(cx, es, f"nt1{tag}", [128, D], F32)
        self.hb = _alloc(cx, es, f"nhb{tag}", [128, D], BF16)
        self.t_junk, self.t_ss, self.t_rt, self.t_rstd, self.t_t1, self.t_hb = Tok(), Tok(), Tok(), Tok(), Tok(), Tok()


def norm_mod(cx, nb, x, t_x, n, which):
    S = cx.S
    A = cx.modbc[:, (1 + 3 * which) * D:(2 + 3 * which) * D]
    Sh = cx.modbc[:, (3 * which) * D:(3 * which + 1) * D]
    S.op("act", lambda e: e.activation(nb.junk[:n], x[:n], AF.Square, accum_out=nb.ss[:n, 0:1]),
         reads=[t_x], writes=[nb.t_junk, nb.t_ss])
    S.op("act", lambda e: e.activation(nb.rt[:n], nb.ss[:n], AF.Sqrt, bias=cx.eps_t[:n], scale=1.0 / D),
         reads=[nb.t_ss, cx.t_const], writes=[nb.t_rt])
    S.op("dve", lambda e: e.reciprocal(nb.rstd[:n], nb.rt[:n]), reads=[nb.t_rt], writes=[nb.t_rstd])
    S.op("dve", lambda e: e.scalar_tensor_tensor(nb.t1[:n], x[:n], nb.rstd[:n, 0:1], A[:n], ALU.mult, ALU.mult),
         reads=[t_x, nb.t_rstd, cx.t_mod], writes=[nb.t_t1])
    S.op("pool", lambda e: e.tensor_tensor(nb.hb[:n], nb.t1[:n], Sh[:n], ALU.add),
         reads=[nb.t_t1, cx.t_mod], writes=[nb.t_hb])


def transpose8(cx, nb, n, bank_i, hT, t_hT, evac="act"):
    S = cx.S
    pb = cx.banks[bank_i].bitcast(BF16).rearrange("p (k t) -> p k t", k=8)
    for k in range(8):
        S.op("pe", (lambda k=k: (lambda e: e.transpose(pb[:, k, :n], nb.hb[:n, k * 128:(k + 1) * 128], cx.identb[:n, :n])))(),
             reads=[nb.t_hb, cx.t_const], writes=[cx.t_bank[bank_i]])
    if evac == "act":
        S.op("act", lambda e: e.activation(hT[:, :, :n], pb[:, :, :n], AF.Copy), reads=[cx.t_bank[bank_i]], writes=[t_hT])
    else:
        S.op("dve", lambda e: e.tensor_copy(hT[:, :, :n], pb[:, :, :n]), reads=[cx.t_bank[bank_i]], writes=[t_hT])


def load_win(cx, es):
    S = cx.S
    winb = _alloc(cx, es, "winb", [128, 8, DIN], BF16)
    t_w = Tok("winb")
    src = cx.w_in.rearrange("(k p) n -> p k n", p=128)
    for k in range(8):
        S.dma("pool", (lambda k=k: (lambda e: e.dma_start(out=winb[:, k, :], in_=src[:, k, :])))(), writes=[t_w])
    return winb, t_w


def qk_norm(cx, pk_i, w_col, out3, t_out, sq, t_sq, rk, t_rk, pms_i):
    S = cx.S
    pk = cx.banks[pk_i]
    S.op("act", lambda e: e.activation(sq, pk, AF.Square), reads=[cx.t_bank[pk_i]], writes=[t_sq])
    S.op("pe", lambda e: e.matmul(cx.banks[pms_i], cx.bd64, sq, start=True, stop=True),
         reads=[t_sq, cx.t_const], writes=[cx.t_bank[pms_i]])
    S.op("act", lambda e: e.activation(rk, cx.banks[pms_i], AF.Sqrt, bias=cx.eps_t, scale=1.0),
         reads=[cx.t_bank[pms_i], cx.t_const], writes=[t_rk])
    S.op("dve", lambda e: e.reciprocal(rk, rk), reads=[t_rk], writes=[t_rk])
    S.op("dve", lambda e: e.scalar_tensor_tensor(out3, pk.rearrange("p (a t) -> p a t", a=4), cx.ppt[:, w_col:w_col + 1],
                                                 rk.rearrange("p (a t) -> p a t", a=4), ALU.mult, ALU.mult),
         reads=[cx.t_bank[pk_i], t_rk, cx.t_const], writes=[t_out])


def phase_p1a(cx, ntiles=NT):
    nc, S = cx.nc, cx.S
    with ExitStack() as es:
        load_mod(cx, es)
        _declare_peer(cx)
        pcs = PcastStream(cx, es, PCAST_ITEMS[:48] if PCAST_OVERLAP else [])
        winb, t_w = load_win(cx, es)
        nb = NormBufs(cx, es, "a")
        xt = [_alloc(cx, es, f"xt{i}", [128, D], F32) for i in range(2)]
        t_xt = toks("xt", 2)
        hT = [_alloc(cx, es, f"hT{i}", [128, 8, 128], BF16) for i in range(2)]
        t_hT = toks("hT", 2)
        sq = _alloc(cx, es, "sq", [128, 512], BF16)
        rk = _alloc(cx, es, "rk", [128, 512], F32)
        t_sq, t_rk = Tok(), Tok()
        ktst = [_alloc(cx, es, f"ktst{i}", [128, 4, 512], BF16) for i in range(2)]
        t_ktst = toks("ktst", 2)
        ikst = [_alloc(cx, es, f"ikst{i}", [64, 512], BF16) for i in range(2)]
        t_ikst = toks("ikst", 2)
        vst = [_alloc(cx, es, f"vst{i}", [128, 8, 65], BF16) for i in range(2)]
        t_vst = toks("vst", 2)
        t_KT, t_VP, t_IKT = cx.t_KT, cx.t_VP, cx.t_IKT
        for b in range(2):
            S.op("pool", (lambda b=b: (lambda e: e.memset(vst[b], 1.0)))(), writes=[t_vst[b]])
        KTd = cx.KT.rearrange("a p t -> p a t")
        def stage_a(i):
            b = i % 2
            S.dma("sp", (lambda i=i, b=b: (lambda e: e.dma_start(out=xt[b], in_=cx.xseq[i * 128:(i + 1) * 128, :])))(),
                  writes=[t_xt[b]])
            norm_mod(cx, nb, xt[b], t_xt[b], 128, 0)
            transpose8(cx, nb, 128, 0, hT[b], t_hT[b])

        def stage_b(i):
            b = i % 2
            g4, s4 = (i // 4) % 2, i % 4
            pk = cx.banks[1].rearrange("p (a t) -> p a t", a=4)
            for p in range(4):
                for k in range(8):
                    S.op("pe", (lambda p=p, k=k, b=b: (lambda e: e.matmul(
                        pk[:, p, :], winb[:, k, C_K + p * 128:C_K + (p + 1) * 128], hT[b][:, k, :],
                        start=(k == 0), stop=(k == 7))))(), reads=[t_w, t_hT[b]], writes=[cx.t_bank[1]])
            qk_norm(cx, 1, 0, ktst[g4][:, :, s4 * 128:(s4 + 1) * 128], t_ktst[g4], sq, t_sq, rk, t_rk, 2)
            for k in range(8):
                S.op("pe", (lambda k=k, b=b: (lambda e: e.matmul(cx.banks[3], hT[b][:, k, :], winb[:, k, C_V:C_V + 512],
                                                                  start=(k == 0), stop=(k == 7))))(),
                     reads=[t_w, t_hT[b]], writes=[cx.t_bank[3]])
            S.op("act", (lambda b=b: (lambda e: e.activation(vst[b][:, :, 0:64], cx.banks[3].rearrange("p (h d) -> p h d", h=8),
                                                              AF.Copy)))(), reads=[cx.t_bank[3]], writes=[t_vst[b]])
            S.dma("act", (lambda i=i, b=b: (lambda e: e.dma_start(out=cx.VP[i], in_=vst[b].rearrange("p h d -> p (h d)"))))(),
                  reads=[t_vst[b]], writes=[t_VP[i]])
            for k in range(8):
                S.op("pe", (lambda k=k, b=b: (lambda e: e.matmul(cx.banks[4][0:64, 0:128], winb[:, k, C_IK:C_IK + 64], hT[b][:, k, :],
                                                                  start=(k == 0), stop=(k == 7))))(),
                     reads=[t_w, t_hT[b]], writes=[cx.t_bank[4]])
            S.op("dve", (lambda g4=g4, s4=s4: (lambda e: e.tensor_copy(ikst[g4][:, s4 * 128:(s4 + 1) * 128], cx.banks[4][0:64, 0:128])))(),
                 reads=[cx.t_bank[4]], writes=[t_ikst[g4]])
            if s4 == 3:
                i0 = i - 3
                S.dma("act", (lambda i0=i0, g4=g4: (lambda e: e.dma_start(out=KTd[:, :, i0 * 128:(i0 + 4) * 128], in_=ktst[g4])))(),
                      reads=[t_ktst[g4]], writes=t_KT[i0:i0 + 4])
                S.dma("act", (lambda i0=i0, g4=g4: (lambda e: e.dma_start(out=cx.IKT[:, i0 * 128:(i0 + 4) * 128], in_=ikst[g4])))(),
                      reads=[t_ikst[g4]], writes=t_IKT[i0:i0 + 4])

        stage_a(0)
        for i in range(ntiles):
            if i + 1 < ntiles:
                stage_a(i + 1)
            stage_b(i)
            pcs.issue(1)
        pcs.flush()
        phase_end(cx)


def phase_p1b(cx, nblocks=NB):
    nc, S = cx.nc, cx.S
    with ExitStack() as es:
        load_mod(cx, es)
        _declare_peer(cx)
        pcs = PcastStream(cx, es, PCAST_ITEMS[48:] if PCAST_OVERLAP else [])
        winb, t_w = load_win(cx, es)
        nb = NormBufs(cx, es, "b")
        xt = [_alloc(cx, es, f"xt{i}", [128, D], F32) for i in range(2)]
        t_xt = toks("xt", 2)
        hT = [_alloc(cx, es, f"hT{i}", [128, 8, 128], BF16) for i in range(2)]
        t_hT = toks("hT", 2)
        sq = _alloc(cx, es, "sq", [128, 512], BF16)
        rk = _alloc(cx, es, "rk", [128, 512], F32)
        t_sq, t_rk = Tok(), Tok()
        uh = _alloc(cx, es, "uh", [128, 4, 64], F32)
        cxh = _alloc(cx, es, "cxh", [128, 4, 64], F32)
        t_uh, t_cxh = Tok(), Tok()
        qst = [_alloc(cx, es, f"qst{i}", [128, 4, 128], BF16) for i in range(2)]
        t_qst = toks("qst", 2)
        iqst = [_alloc(cx, es, f"iqst{i}", [64, 8, 128], BF16) for i in range(2)]
        t_iqst = toks("iqst", 2)
        iwst = [_alloc(cx, es, f"iwst{i}", [128, 8], F32) for i in range(2)]
        t_iwst = toks("iwst", 2)
        cxs = _alloc(cx, es, "cxs", [128, 4, 128], F32)
        u = _alloc(cx, es, "u", [128, 4, 130], F32)
        acc = _alloc(cx, es, "acc", [128, 4, 128], F32)
        y = _alloc(cx, es, "y", [128, 4, 128], F32)
        ysq = _alloc(cx, es, "ysq", [128, 4, 128], BF16)
        rs = _alloc(cx, es, "rs", [128, 128], F32)
        ycst = [_alloc(cx, es, f"ycst{i}", [128, 4, 128], BF16) for i in range(2)]
        t_ycst = toks("ycst", 2)
        t_cxs, t_u, t_acc, t_y, t_ysq, t_rs = Tok(), Tok(), Tok(), Tok(), Tok(), Tok()
        ppt = cx.ppt

        S.dma("sp", lambda e: e.dma_start(out=xt[0][0:64, :], in_=cx.xhalo), writes=[t_xt[0]])
        norm_mod(cx, nb, xt[0], t_xt[0], 64, 0)
        transpose8(cx, nb, 64, 0, hT[0], t_hT[0])
        hpcc = cx.banks[1].rearrange("p (a t) -> p a t", a=4)
        hpcx = cx.banks[2].rearrange("p (a t) -> p a t", a=4)
        for (pp_, col, bi) in ((hpcc, C_CC, 1), (hpcx, C_CX, 2)):
            for c in range(4):
                for k in range(8):
                    S.op("pe", (lambda pp_=pp_, col=col, c=c, k=k: (lambda e: e.matmul(
                        pp_[:, c, 0:64], winb[:, k, col + c * 128:col + (c + 1) * 128], hT[0][:, k, 0:64],
                        start=(k == 0), stop=(k == 7))))(), reads=[t_w, t_hT[0]], writes=[cx.t_bank[bi]])
        S.op("act", lambda e: e.activation(cxh, hpcx[:, :, 0:64], AF.Copy), reads=[cx.t_bank[2]], writes=[t_cxh])
        S.op("dve", lambda e: e.tensor_tensor(uh, hpcc[:, :, 0:64], cxh, ALU.mult), reads=[cx.t_bank[1], t_cxh], writes=[t_uh])
        hm = _alloc(cx, es, "hm", [128, 64], F32)
        t_hm = Tok()
        S.dma("sp", lambda e: e.dma_start(out=hm, in_=cx.hmask), writes=[t_hm])
        S.op("dve", lambda e: e.tensor_tensor(uh, uh, hm.unsqueeze(1).to_broadcast([128, 4, 64]), ALU.mult),
             reads=[t_uh, t_hm], writes=[t_uh])

        def stage_a(j):
            b = j % 2
            S.dma("sp", (lambda j=j, b=b: (lambda e: e.dma_start(out=xt[b], in_=cx.xown[j * 128:(j + 1) * 128, :])))(),
                  writes=[t_xt[b]])
            norm_mod(cx, nb, xt[b], t_xt[b], 128, 0)
            transpose8(cx, nb, 128, 0, hT[b], t_hT[b])

        def stage_b(j):
            b = j % 2
            pq = cx.banks[1].rearrange("p (a t) -> p a t", a=4)
            for p in range(4):
                for k in range(8):
                    S.op("pe", (lambda p=p, k=k, b=b: (lambda e: e.matmul(
                        pq[:, p, :], winb[:, k, C_Q + p * 128:C_Q + (p + 1) * 128], hT[b][:, k, :],
                        start=(k == 0), stop=(k == 7))))(), reads=[t_w, t_hT[b]], writes=[cx.t_bank[1]])
            qk_norm(cx, 1, 1, qst[b], t_qst[b], sq, t_sq, rk, t_rk, 2)
            S.dma("act", (lambda j=j, b=b: (lambda e: e.dma_start(out=cx.QT[j], in_=qst[b])))(), reads=[t_qst[b]], writes=[cx.t_QT[j]])
            for h in range(8):
                bi = 3 + h // 4
                pi = cx.banks[bi].rearrange("p (a t) -> p a t", a=4)
                for k in range(8):
                    S.op("pe", (lambda h=h, k=k, b=b, pi=pi: (lambda e: e.matmul(
                        pi[0:64, h % 4, :], winb[:, k, C_IQ + h * 64:C_IQ + (h + 1) * 64], hT[b][:, k, :],
                        start=(k == 0), stop=(k == 7))))(), reads=[t_w, t_hT[b]], writes=[cx.t_bank[bi]])
            for hh in range(2):
                pi = cx.banks[3 + hh].rearrange("p (a t) -> p a t", a=4)
                S.op("act", (lambda hh=hh, b=b, pi=pi: (lambda e: e.activation(iqst[b][:, hh * 4:(hh + 1) * 4, :], pi[0:64], AF.Copy)))(),
                     reads=[cx.t_bank[3 + hh]], writes=[t_iqst[b]])
            S.dma("act", (lambda j=j, b=b: (lambda e: e.dma_start(out=cx.IQT[j], in_=iqst[b])))(), reads=[t_iqst[b]], writes=[cx.t_IQT[j]])
            for k in range(8):
                S.op("pe", (lambda k=k, b=b: (lambda e: e.matmul(cx.banks[5][:, 0:8], hT[b][:, k, :], winb[:, k, C_IW:C_IW + 8],
                                                                  start=(k == 0), stop=(k == 7))))(),
                     reads=[t_w, t_hT[b]], writes=[cx.t_bank[5]])
            S.op("dve", (lambda b=b: (lambda e: e.tensor_copy(iwst[b], cx.banks[5][:, 0:8])))(), reads=[cx.t_bank[5]], writes=[t_iwst[b]])
            S.dma("act", (lambda j=j, b=b: (lambda e: e.dma_start(out=cx.IW[j], in_=iwst[b])))(), reads=[t_iwst[b]], writes=[cx.t_IW[j]])
            pcb = cx.banks[6].rearrange("p (a t) -> p a t", a=4)
            pcc = cx.banks[7].rearrange("p (a t) -> p a t", a=4)
            pcx = cx.banks[2].rearrange("p (a t) -> p a t", a=4)
            for (pp_, col, bi) in ((pcb, C_CB, 6), (pcc, C_CC, 7), (pcx, C_CX, 2)):
                for c in range(4):
                    for k in range(8):
                        S.op("pe", (lambda pp_=pp_, col=col, c=c, k=k, b=b: (lambda e: e.matmul(
                            pp_[:, c, :], winb[:, k, col + c * 128:col + (c + 1) * 128], hT[b][:, k, :],
                            start=(k == 0), stop=(k == 7))))(), reads=[t_w, t_hT[b]], writes=[cx.t_bank[bi]])
            S.op("act", lambda e: e.activation(cxs, pcx, AF.Copy), reads=[cx.t_bank[2]], writes=[t_cxs])
            S.op("dve", lambda e: e.tensor_tensor(u[:, :, 2:130], pcc, cxs, ALU.mult), reads=[cx.t_bank[7], t_cxs], writes=[t_u])
            S.op("pool", (lambda j=j: (lambda e: e.tensor_copy(u[:, :, 0:2], uh[:, :, 2 * j:2 * j + 2])))(), reads=[t_uh], writes=[t_u])
            for c in range(4):
                S.op("dve", (lambda c=c: (lambda e: e.tensor_scalar(acc[:, c, :], u[:, c, 2:130], ppt[:, 2 + c * 3 + 2:2 + c * 3 + 3],
                                                                     ppt[:, 14 + c:15 + c], ALU.mult, ALU.add)))(),
                     reads=[t_u, cx.t_const], writes=[t_acc])
                S.op("dve", (lambda c=c: (lambda e: e.scalar_tensor_tensor(acc[:, c, :], u[:, c, 1:129], ppt[:, 2 + c * 3 + 1:2 + c * 3 + 2],
                                                                            acc[:, c, :], ALU.mult, ALU.add)))(),
                     reads=[t_u, t_acc, cx.t_const], writes=[t_acc])
                S.op("dve", (lambda c=c: (lambda e: e.scalar_tensor_tensor(acc[:, c, :], u[:, c, 0:128], ppt[:, 2 + c * 3:2 + c * 3 + 1],
                                                                            acc[:, c, :], ALU.mult, ALU.add)))(),
                     reads=[t_u, t_acc, cx.t_const], writes=[t_acc])
            S.op("dve", lambda e: e.tensor_tensor(y, acc, pcb, ALU.mult), reads=[t_acc, cx.t_bank[6]], writes=[t_y])
            S.op("act", lambda e: e.activation(ysq, y, AF.Square), reads=[t_y], writes=[t_ysq])
            for c in range(4):
                S.op("pe", (lambda c=c: (lambda e: e.matmul(cx.banks[5][:, 128:256], cx.ones512, ysq[:, c, :], start=(c == 0), stop=(c == 3))))(),
                     reads=[t_ysq, cx.t_const], writes=[cx.t_bank[5]])
            S.op("act", lambda e: e.activation(rs, cx.banks[5][:, 128:256], AF.Sqrt, bias=cx.eps_t, scale=1.0),
                 reads=[cx.t_bank[5], cx.t_const], writes=[t_rs])
            S.op("dve", lambda e: e.reciprocal(rs, rs), reads=[t_rs], writes=[t_rs])
            for c in range(4):
                S.op("dve", (lambda c=c, b=b: (lambda e: e.scalar_tensor_tensor(ycst[b][:, c, :], y[:, c, :], ppt[:, 18 + c:19 + c], rs,
                                                                                 ALU.mult, ALU.mult)))(),
                     reads=[t_y, t_rs, cx.t_const], writes=[t_ycst[b]])
            S.dma("act", (lambda j=j, b=b: (lambda e: e.dma_start(out=cx.YCT[j], in_=ycst[b])))(), reads=[t_ycst[b]], writes=[cx.t_YCT[j]])

        stage_a(0)
        for j in range(nblocks):
            if j + 1 < nblocks:
                stage_a(j + 1)
            stage_b(j)
            pcs.issue(2)
        pcs.flush()
        phase_end(cx)


def prep_shared(inp):
    f = lambda a: np.ascontiguousarray(np.asarray(a, dtype=np.float32))
    sh = {}
    sh["w_ada"] = f(inp["w_ada"])
    sh["b_ada"] = f(inp["b_ada"]).reshape(1, -1)
    sh["norm1_w"] = f(inp["norm1_w"]).reshape(1, -1)
    sh["norm2_w"] = f(inp["norm2_w"]).reshape(1, -1)
    sh["w_in"] = f(inp["w_in"])
    pp = np.zeros((128, 32), np.float32)
    pp[:, 0] = np.tile(f(inp["k_norm_w"]), 2)
    pp[:, 1] = np.tile(f(inp["q_norm_w"]), 2)
    cw = f(inp["conv_w"])
    for c in range(4):
        for j in range(3):
            pp[:, 2 + c * 3 + j] = cw[j, c * 128:(c + 1) * 128]
        pp[:, 14 + c] = f(inp["conv_b"])[c * 128:(c + 1) * 128]
        pp[:, 18 + c] = f(inp["conv_out_norm_w"])[c * 128:(c + 1) * 128]
    sh["pp"] = pp
    sh["aonw"] = f(inp["attn_out_norm_w"]).reshape(1, -1)
    sh["w_out"] = f(inp["w_out"])
    sh["peer_wq"] = f(inp["peer_wq"])
    sk = f(inp["peer_subkeys"])
    sh["skT"] = np.ascontiguousarray(sk.transpose(0, 1, 3, 2).reshape(16, 128, 128))
    U = f(inp["peer_u"]).reshape(128, 128, D)
    sh["UT"] = np.ascontiguousarray(U.transpose(2, 1, 0).reshape(D, 16384))
    V = f(inp["peer_v"]).reshape(128, 128, D)
    sh["VH"] = np.ascontiguousarray(V.transpose(1, 0, 2).reshape(16384, D))
    sh["ident"] = np.eye(128, dtype=np.float32)
    sh["iota"] = np.ascontiguousarray(np.tile(np.arange(128, dtype=np.float32)[None, :], (128, 1)))
    return sh


def prep_core(inp, sh, core):
    b, half = core // 2, core % 2
    x = np.asarray(inp["x"], dtype=np.float32)[b]
    blocks = own_blocks(half)
    m = dict(sh)
    m["xseq"] = np.ascontiguousarray(x)
    m["xown"] = np.ascontiguousarray(np.concatenate([x[g * 128:(g + 1) * 128] for g in blocks], 0))
    halo = np.zeros((64, D), np.float32)
    for j, g in enumerate(blocks):
        if g > 0:
            halo[2 * j:2 * j + 2] = x[g * 128 - 2:g * 128]
    m["xhalo"] = halo
    hm = np.ones((128, 64), np.float32)
    for j, g in enumerate(blocks):
        if g == 0:
            hm[:, 2 * j:2 * j + 2] = 0.0
    m["hmask"] = hm
    m["cT"] = np.ascontiguousarray(np.asarray(inp["c"], dtype=np.float32)[b].reshape(8, 128).T)
    cm = np.zeros((128, 2, 256), np.float32)
    for par in range(2):
        j = par
        g = blocks[j]
        n = nchunks(j)
        kpos = (n - 2) * 128 + np.arange(256)[None, :]
        qpos = g * 128 + np.arange(128)[:, None]
        cm[:, par, :] = (kpos <= qpos)
    m["cmask"] = cm
    return m


def build_program(phases, dbg_out=(), dbg_in=(), **kw):
    nc = bass.Bass("TRN2", target_bir_lowering=False)
    cx = declare(nc, dbg_out, dbg_in)
    setup_persistent(cx)
    for ph in phases:
        PHASES[ph](cx, **kw.get(ph, {}))
    cx.S.final_wait("sp")
    cx.S.final_wait("act")
    cx.S.final_wait("pool")
    cx.S.emit()
    return nc, cx


PHASES = {"ada": phase_ada, "p1a": phase_p1a, "p1b": phase_p1b}


def phase_p2(cx, nblocks=NB):
    nc, S = cx.nc, cx.S
    with ExitStack() as es:
        ikt = _alloc(cx, es, "ikt", [64, SEQ], BF16)
        t_ikt = Tok()
        cm = _alloc(cx, es, "cm", [128, 2, 256], F32)
        nbm = _alloc(cx, es, "nbm", [128, 2, 256], F32)
        t_cm = Tok()
        sc = [_alloc(cx, es, f"sc{i}", [128, SEQ], F32) for i in range(4)]
        t_sc = toks("sc", 4)
        rl = [_alloc(cx, es, f"rl{i}", [128, 512], BF16) for i in range(6)]
        aw = [_alloc(cx, es, f"aw{i}", [128, 8], F32) for i in range(2)]
        sg = [_alloc(cx, es, f"sg{i}", [128, 8], F32) for i in range(2)]
        dg = [_alloc(cx, es, f"dg{i}", [128, 8, 128], BF16) for i in range(2)]
        t_aw, t_sg, t_dg = toks("aw", 2), toks("sg", 2), toks("dg", 2)
        t_rl = toks("rl", 6)
        junkD = _alloc(cx, es, "junkD", [128, 8], BF16)
        junkA = _alloc(cx, es, "junkA", [128, 8], BF16)
        t_junkD, t_junkA = Tok(), Tok()
        mbs = [_alloc(cx, es, f"mbs{i}", [128, SEQ], BF16) for i in range(2)]
        t_mbs = toks("mbs", 2)
        t_mbs2 = toks("mbs2", 2)
        iq = [_alloc(cx, es, f"iq{i}", [64, 8, 128], BF16) for i in range(2)]
        t_iq = toks("iq", 2)
        iw = [_alloc(cx, es, f"iw{i}", [128, 8], F32) for i in range(2)]
        t_iw = toks("iw", 2)
        p2c = _alloc(cx, es, "p2c", [128, NIT + 2], F32)
        t_p2c = Tok()
        STs = [_alloc(cx, es, f"ST{i}", [128, NIT + 2], F32) for i in range(4)]
        sms = [_alloc(cx, es, f"sm{i}", [128, 10], F32) for i in range(4)]
        tk = [{n: Tok() for n in ("ST", "mm", "mn", "W", "mid", "cnt", "ps", "lo", "ssum", "tot")} for _ in range(4)]
        for q4 in range(4):
            lo_, hi_ = q4 * (SEQ // 4), (q4 + 1) * (SEQ // 4)
            S.dma("sp", (lambda lo_=lo_, hi_=hi_: (lambda e: e.dma_start(out=ikt[:, lo_:hi_], in_=cx.IKT[:, lo_:hi_])))(),
                  reads=cx.t_IKT[q4 * 16:(q4 + 1) * 16], writes=[t_ikt])
        S.dma("sp", lambda e: e.dma_start(out=cm, in_=cx.cmask), writes=[t_cm])
        S.op("dve", lambda e: e.tensor_scalar(nbm, cm, -1.0, 1e30, ALU.add, ALU.mult), reads=[t_cm], writes=[t_cm])
        for k in range(NIT + 2):
            S.op("pool", (lambda k=k: (lambda e: e.memset(p2c[:, k:k + 1], 2.0 ** (-k))))(), writes=[t_p2c])
        cnts = {"rl": 0, "bk": 0}

        def accumulate(j):
            b = j % 2
            b4 = j % 4
            par = j % 2
            nk = 128 * nchunks(j)
            sm, ST, T = sms[b4], STs[b4], tk[b4]
            S.dma("sp", lambda e: e.dma_start(out=iq[b], in_=cx.IQT[j]), reads=[cx.t_IQT[j]], writes=[t_iq[b]])
            S.dma("sp", lambda e: e.dma_start(out=iw[b], in_=cx.IW[j]), reads=[cx.t_IW[j]], writes=[t_iw[b]])
            S.op("act", lambda e: e.activation(sg[b], iw[b], AF.Sign), reads=[t_iw[b]], writes=[t_sg[b]])
            S.op("dve", lambda e: e.tensor_tensor(aw[b], iw[b], sg[b], ALU.mult), reads=[t_iw[b], t_sg[b]], writes=[t_aw[b]])
            for h in range(8):
                S.op("dve", (lambda h=h: (lambda e: e.tensor_scalar(dg[b][:, h, :], cx.identb, sg[b][:, h:h + 1], None, ALU.mult)))(),
                     reads=[t_sg[b], cx.t_const], writes=[t_dg[b]])
            ngr = (nk + 511) // 512
            items = [(kg, h) for kg in range(ngr) for h in range(8)]
            LAGY = 2
            slot = {}
            for s_i in range(len(items) + LAGY):
                if s_i < len(items):
                    kg, h = items[s_i]
                    k0 = kg * 512
                    w = min(512, nk - k0)
                    bi = cnts["bk"] % 4
                    cnts["bk"] += 1
                    r = cnts["rl"] % 6
                    cnts["rl"] += 1
                    slot[s_i] = r
                    S.op("pe", (lambda bi=bi, h=h, k0=k0, w=w: (lambda e: e.matmul(
                        cx.banks[bi][:, :w], iq[b][:, h, :], ikt[:, k0:k0 + w], start=True, stop=True)))(),
                        reads=[t_iq[b], t_ikt], writes=[cx.t_bank[bi]])
                    S.op("act", (lambda bi=bi, r=r, w=w, h=h: (lambda e: e.activation(rl[r][:, :w], cx.banks[bi][:, :w], AF.Relu,
                                                                                      scale=aw[b][:, h:h + 1])))(),
                         reads=[cx.t_bank[bi], t_aw[b]], writes=[t_rl[r]])
                if s_i - LAGY >= 0:
                    kg, h = items[s_i - LAGY]
                    k0 = kg * 512
                    w = min(512, nk - k0)
                    r = slot[s_i - LAGY]
                    ab = 4 + kg % 2
                    S.op("pe", (lambda ab=ab, h=h, r=r, w=w: (lambda e: e.matmul(
                        cx.banks[ab][:, :w], dg[b][:, h, :], rl[r][:, :w], start=(h == 0), stop=(h == 7))))(),
                        reads=[t_dg[b], t_rl[r]], writes=[cx.t_bank[ab]])
                    if h == 7:
                        if kg % 2 == 0:
                            S.op("act", (lambda ab=ab, k0=k0, w=w: (lambda e: e.activation(sc[b4][:, k0:k0 + w], cx.banks[ab][:, :w], AF.Copy)))(),
                                 reads=[cx.t_bank[ab]], writes=[t_sc[b4]])
                        else:
                            S.op("dve", (lambda ab=ab, k0=k0, w=w: (lambda e: e.tensor_copy(sc[b4][:, k0:k0 + w], cx.banks[ab][:, :w])))(),
                                 reads=[cx.t_bank[ab]], writes=[t_sc[b4]])
                yield 1

        def acc_tail(j):
            b4 = j % 4
            par = j % 2
            nk = 128 * nchunks(j)
            sm, ST, T = sms[b4], STs[b4], tk[b4]
            scv = sc[b4][:, :nk]
            S.op("dve", lambda e: e.tensor_reduce(sm[:, 0:1], scv, AX.X, ALU.max), reads=[t_sc[b4]], writes=[T["mm"]])
            S.op("dve", lambda e: e.tensor_reduce(sm[:, 1:2], scv, AX.X, ALU.min), reads=[t_sc[b4]], writes=[T["mn"]])
            tail = sc[b4][:, nk - 256:nk]
            S.op("dve", lambda e: e.tensor_tensor(tail, tail, cm[:, par, :], ALU.mult), reads=[t_sc[b4], t_cm], writes=[t_sc[b4]])
            S.op("dve", lambda e: e.tensor_tensor(tail, tail, nbm[:, par, :], ALU.add), reads=[t_sc[b4], t_cm], writes=[t_sc[b4]])
            S.op("dve", lambda e: e.tensor_scalar(sm[:, 2:3], sm[:, 0:1], sm[:, 1:2], 2.0, ALU.subtract, ALU.add),
                 reads=[T["mm"], T["mn"]], writes=[T["W"]])
            S.op("dve", lambda e: e.tensor_scalar(ST, p2c, sm[:, 2:3], None, ALU.mult), reads=[T["W"], t_p2c], writes=[T["ST"]])
            S.op("dve", lambda e: e.tensor_scalar(sm[:, 3:4], sm[:, 1:2], -1.0, ST[:, 1:2], ALU.add, ALU.add),
                 reads=[T["mn"], T["ST"]], writes=[T["mid"]])

        def bis_iter(j, k):
            b = j % 2
            b4 = j % 4
            nk = 128 * nchunks(j)
            sm, ST, T = sms[b4], STs[b4], tk[b4]
            nA = max(128, (int(nk * 0.52) // 128) * 128)
            nB = nk - nA
            thr = TOPK - nB / 2.0
            S.op("dve", lambda e: e.tensor_scalar(junkD[:, 0:1].to_broadcast([128, nA]), sc[b4][:, :nA], sm[:, 3:4], None, ALU.is_ge, ALU.add, accum_out=sm[:, 4:5]),
                 reads=[t_sc[b4], T["mid"]], writes=[t_junkD, T["cnt"]])
            S.op("act", lambda e: e.activation(junkA[:, 0:1].to_broadcast([128, nk - nA]), sc[b4][:, nA:nk], AF.Sign, bias=sm[:, 3:4], scale=-1.0, accum_out=sm[:, 7:8]),
                 reads=[t_sc[b4], T["mid"]], writes=[t_junkA, T["ssum"]])
            S.op("dve", lambda e: e.scalar_tensor_tensor(sm[:, 8:9], sm[:, 7:8], -0.5, sm[:, 4:5], ALU.mult, ALU.add),
                 reads=[T["ssum"], T["cnt"]], writes=[T["tot"]])
            S.op("dve", lambda e: e.tensor_scalar(sm[:, 5:6], sm[:, 8:9], thr, ST[:, k:k + 1], ALU.is_ge, ALU.mult),
                 reads=[T["tot"], T["ST"]], writes=[T["ps"]])
            S.op("dve", lambda e: e.scalar_tensor_tensor(sm[:, 3:4], sm[:, 3:4], ST[:, k + 1:k + 2], sm[:, 5:6], ALU.subtract, ALU.add),
                 reads=[T["mid"], T["ST"], T["ps"]], writes=[T["mid"]])

        def finalize(j):
            b = j % 2
            b4 = j % 4
            nk = 128 * nchunks(j)
            sm, ST, T = sms[b4], STs[b4], tk[b4]
            S.op("dve", lambda e: e.tensor_scalar(sm[:, 6:7], sm[:, 3:4], ST[:, NIT + 1:NIT + 2], None, ALU.subtract),
                 reads=[T["mid"], T["ST"]], writes=[T["lo"]])
            S.op("dve", lambda e: e.tensor_scalar(mbs[b][:, :nk], sc[b4][:, :nk], sm[:, 6:7], NEG, ALU.is_lt, ALU.mult),
                 reads=[t_sc[b4], T["lo"]], writes=[t_mbs[b]])
            S.dma("sp", lambda e: e.dma_start(out=cx.MB[j][:, :nk], in_=mbs[b][:, :nk]),
                  reads=[t_mbs[b]], writes=[cx.t_MB[j]])

        import itertools
        npairs = nblocks // 2

        def acc_pair_gen(p):
            for jj in (2 * p, 2 * p + 1):
                yield from accumulate(jj)

        for _ in acc_pair_gen(0):
            pass
        acc_tail(0)
        acc_tail(1)
        for p in range(npairs):
            jA, jB = 2 * p, 2 * p + 1
            nxt = acc_pair_gen(p + 1) if p + 1 < npairs else iter(())
            nsteps = 0
            if p + 1 < npairs:
                for jj in (2 * p + 2, 2 * p + 3):
                    nsteps += 8 * ((128 * nchunks(jj) + 511) // 512) + 2
            per = -(-nsteps // (2 * NIT)) if nsteps else 0
            for k in range(1, NIT + 1):
                bis_iter(jA, k)
                for _ in itertools.islice(nxt, per):
                    pass
                bis_iter(jB, k)
                for _ in itertools.islice(nxt, per):
                    pass
            for _ in nxt:
                pass
            finalize(jA)
            finalize(jB)
            if p + 1 < npairs:
                acc_tail(2 * p + 2)
                acc_tail(2 * p + 3)
        phase_end(cx)


PHASES["p2"] = phase_p2


def phase_p3(cx, nblocks=NB, nkt=NT):
    nc, S = cx.nc, cx.S
    with ExitStack() as es:
        load_mod(cx, es)
        kt = _alloc(cx, es, "kt", [128, 4, SEQ], BF16)
        vp = _alloc(cx, es, "vp", [128, NT, 520], BF16)
        t_kt, t_vp = Tok(), Tok()
        woutb = _alloc(cx, es, "woutb", [128, 8, D], BF16)
        aon = _alloc(cx, es, "aon", [128, 512], F32)
        t_wo, t_aon = Tok(), Tok()
        qt = [_alloc(cx, es, f"qt{i}", [128, 4, 128], BF16) for i in range(2)]
        qp = [_alloc(cx, es, f"qp{i}", [128, 8, 128], BF16) for i in range(2)]
        t_qt, t_qp = toks("qt", 2), toks("qp", 2)
        mbg = [_alloc(cx, es, f"mbg{i}", [128, 512], BF16) for i in range(3)]
        t_mbg = toks("mbg", 3)
        pt = [_alloc(cx, es, f"pt{i}", [128, 4, 128], BF16) for i in range(4)]
        t_pt = toks("pt", 4)
        o = _alloc(cx, es, "o", [128, 8, 65], F32)
        rden = _alloc(cx, es, "rden", [128, 8], F32)
        ya = _alloc(cx, es, "ya", [128, 8, 64], F32)
        junk = _alloc(cx, es, "junk3", [128, 512], BF16)
        st3 = _alloc(cx, es, "st3", [128, 4], F32)
        yan = _alloc(cx, es, "yan", [128, 512], BF16)
        t_o, t_rden, t_ya, t_junk, t_ss, t_rt, t_rstd, t_yan = Tok(), Tok(), Tok(), Tok(), Tok(), Tok(), Tok(), Tok()
        ymT = [_alloc(cx, es, f"ymT{i}", [128, 8, 128], BF16) for i in range(2)]
        t_ymA, t_ymC = toks("ymA", 2), toks("ymC", 2)
        xt = [_alloc(cx, es, f"xt{i}", [128, D], F32) for i in range(2)]
        tmp = _alloc(cx, es, "tmp3", [128, 512], F32)
        t_xt, t_tmp = toks("xt", 2), Tok()
        G1 = cx.modbc[:, 2 * D:3 * D]

        KTd = cx.KT.rearrange("a p t -> p a t")
        VPd = cx.VP.rearrange("n p f -> p n f")
        step = 8
        for i0 in range(0, nkt, step):
            i1 = min(nkt, i0 + step)
            S.dma("sp", (lambda i0=i0, i1=i1: (lambda e: e.dma_start(out=kt[:, :, i0 * 128:i1 * 128], in_=KTd[:, :, i0 * 128:i1 * 128])))(),
                  reads=cx.t_KT[i0:i1], writes=[t_kt])
            S.dma("act", (lambda i0=i0, i1=i1: (lambda e: e.dma_start(out=vp[:, i0:i1, :], in_=VPd[:, i0:i1, :])))(),
                  reads=cx.t_VP[i0:i1], writes=[t_vp])
        wsrc = cx.w_out.rearrange("(k p) n -> p k n", p=128)
        for k in range(8):
            S.dma("pool", (lambda k=k: (lambda e: e.dma_start(out=woutb[:, k, :], in_=wsrc[:, k, :])))(), writes=[t_wo])
        S.dma("sp", lambda e: e.dma_start(out=aon, in_=cx.aonw.partition_broadcast(128).rearrange("p a b -> p (a b)")), writes=[t_aon])
        for b in range(2):
            S.op("pool", (lambda b=b: (lambda e: e.memset(qp[b], 0.0)))(), writes=[t_qp[b]])
        ident4 = _alloc(cx, es, "ident4", [128, 4, 128], BF16)
        t_id4 = Tok()
        S.op("pool", lambda e: e.tensor_copy(ident4, cx.identb.unsqueeze(1).to_broadcast([128, 4, 128])), reads=[cx.t_const], writes=[t_id4])
        id4f = ident4.rearrange("p a t -> p (a t)")
        PSB = (0, 1, 7)
        LAG = 2
        itc = [0]
        mgc = [0]

        def load_mask(j, c, nk):
            mr = mgc[0] % 3
            mgc[0] += 1
            w = min(512, nk - c * 128)
            S.dma("sp", lambda e: e.dma_start(out=mbg[mr][:, :w], in_=cx.MB[j][:, c * 128:c * 128 + w]),
                  reads=[cx.t_MB[j]], writes=[t_mbg[mr]])
            return mr

        def stage_a(b, c, hg, it, mr):
            bi = PSB[it % 3]
            pr = it % 4
            qpf = qp[b].rearrange("p a t -> p (a t)")
            for pp in range(2):
                pair = 2 * hg + pp
                S.op("pe", (lambda pp=pp, pair=pair: (lambda e: e.matmul(
                    cx.banks[bi][:, pp * 256:(pp + 1) * 256], kt[:, pair, c * 128:(c + 1) * 128], qpf[:, pair * 256:(pair + 1) * 256],
                    start=(pp == 0), stop=False, skip_group_check=True)))(),
                    reads=[t_kt, t_qp[b]], writes=[cx.t_bank[bi]])
            S.op("pe", lambda e: e.matmul(cx.banks[bi], mbg[mr][:, (c % 4) * 128:(c % 4 + 1) * 128], id4f,
                                          start=False, stop=True, skip_group_check=True),
                 reads=[t_mbg[mr], t_id4], writes=[cx.t_bank[bi]])
            S.op("act", lambda e: e.activation(pt[pr].rearrange("p a t -> p (a t)"), cx.banks[bi], AF.Exp, scale=0.125),
                 reads=[cx.t_bank[bi]], writes=[t_pt[pr]])

        def stage_c(c, hg, it, nck):
            pr = it % 4
            po = cx.banks[2 + hg][:, 0:260].rearrange("p (a d) -> p a d", a=4)
            for hh in range(4):
                h = hg * 4 + hh
                S.op("pe", (lambda hh=hh, h=h: (lambda e: e.matmul(
                    po[:, hh, :], pt[pr][:, hh, :], vp[:, c, h * 65:(h + 1) * 65], start=(c == 0 and hh == 0), stop=(c == nck - 1),
                    skip_group_check=True)))(),
                    reads=[t_pt[pr], t_vp], writes=[cx.t_bank[2 + hg]])

        for j in range(nblocks):
            b = j % 2
            nck = nchunks(j)
            nk = nck * 128
            S.dma("sp", (lambda j=j, b=b: (lambda e: e.dma_start(out=qt[b], in_=cx.QT[j])))(), reads=[cx.t_QT[j]], writes=[t_qt[b]])
            S.dma("sp", (lambda j=j, b=b: (lambda e: e.dma_start(out=xt[b], in_=cx.xown[j * 128:(j + 1) * 128, :])))(), writes=[t_xt[b]])
            S.dma("sp", (lambda j=j, b=b: (lambda e: e.dma_start(out=ymT[b][:, 4:8, :], in_=cx.YCT[j])))(), reads=[cx.t_YCT[j]], writes=[t_ymC[b]])
            qp4 = qp[b].rearrange("p (a two) t -> p a two t", two=2)
            S.op("pool", (lambda b=b, qp4=qp4: (lambda e: e.tensor_copy(qp4[0:64, :, 0, :], qt[b][0:64, :, :])))(), reads=[t_qt[b]], writes=[t_qp[b]])
            S.op("pool", (lambda b=b, qp4=qp4: (lambda e: e.tensor_copy(qp4[64:128, :, 1, :], qt[b][64:128, :, :])))(), reads=[t_qt[b]], writes=[t_qp[b]])
            items = [(c, hg) for c in range(nck) for hg in range(2)]
            mrs = {}
            mrs[0] = load_mask(j, 0, nk)
            its = {}
            for s_i in range(len(items) + LAG):
                if s_i < len(items):
                    c, hg = items[s_i]
                    if hg == 0 and c % 4 == 0 and c + 4 < nck:
                        mrs[c + 4] = load_mask(j, c + 4, nk)
                    its[s_i] = itc[0]
                    itc[0] += 1
                    stage_a(b, c, hg, its[s_i], mrs[(c // 4) * 4])
                if s_i - LAG >= 0:
                    c, hg = items[s_i - LAG]
                    stage_c(c, hg, its[s_i - LAG], nck)
            for hg in range(2):
                S.op("dve", (lambda hg=hg: (lambda e: e.tensor_copy(o[:, hg * 4:(hg + 1) * 4, :],
                                                                    cx.banks[2 + hg][:, 0:260].rearrange("p (a d) -> p a d", a=4))))(),
                     reads=[cx.t_bank[2 + hg]], writes=[t_o])
            S.op("dve", lambda e: e.reciprocal(rden, o[:, :, 64]), reads=[t_o], writes=[t_rden])
            S.op("dve", lambda e: e.tensor_tensor(ya, o[:, :, 0:64], rden.unsqueeze(2).to_broadcast([128, 8, 64]), ALU.mult),
                 reads=[t_o, t_rden], writes=[t_ya])
            yaf = ya.rearrange("p h d -> p (h d)")
            S.op("act", (lambda yaf=yaf: (lambda e: e.activation(junk, yaf, AF.Square, accum_out=st3[:, 0:1])))(), reads=[t_ya], writes=[t_junk, t_ss])
            S.op("act", lambda e: e.activation(st3[:, 1:2], st3[:, 0:1], AF.Sqrt, bias=cx.eps_t, scale=1.0 / 512),
                 reads=[t_ss, cx.t_const], writes=[t_rt])
            S.op("dve", lambda e: e.reciprocal(st3[:, 2:3], st3[:, 1:2]), reads=[t_rt], writes=[t_rstd])
            S.op("dve", (lambda yaf=yaf: (lambda e: e.scalar_tensor_tensor(yan, yaf, st3[:, 2:3], aon, ALU.mult, ALU.mult)))(),
                 reads=[t_ya, t_rstd, t_aon], writes=[t_yan])
            pb = cx.banks[4].bitcast(BF16).rearrange("p (k t) -> p k t", k=8)
            for c4 in range(4):
                S.op("pe", (lambda c4=c4, pb=pb: (lambda e: e.transpose(pb[:, c4, :], yan[:, c4 * 128:(c4 + 1) * 128], cx.identb)))(),
                     reads=[t_yan, cx.t_const], writes=[cx.t_bank[4]])
            S.op("act", (lambda b=b, pb=pb: (lambda e: e.activation(ymT[b][:, 0:4, :], pb[:, 0:4, :], AF.Copy)))(),
                 reads=[cx.t_bank[4]], writes=[t_ymA[b]])
            for half in range(2):
                for k in range(8):
                    S.op("pe", (lambda half=half, k=k, b=b: (lambda e: e.matmul(
                        cx.banks[5 + half], ymT[b][:, k, :], woutb[:, k, half * 512:(half + 1) * 512], start=(k == 0), stop=(k == 7))))(),
                        reads=[t_ymA[b], t_ymC[b], t_wo], writes=[cx.t_bank[5 + half]])
                S.op("dve", (lambda half=half: (lambda e: e.tensor_tensor(tmp, cx.banks[5 + half],
                                                                          G1[:, half * 512:(half + 1) * 512], ALU.mult)))(),
                     reads=[cx.t_bank[5 + half], cx.t_mod], writes=[t_tmp])
                S.op("pool", (lambda b=b, half=half: (lambda e: e.tensor_tensor(xt[b][:, half * 512:(half + 1) * 512], tmp,
                                                                                xt[b][:, half * 512:(half + 1) * 512], ALU.add)))(),
                     reads=[t_tmp, t_xt[b]], writes=[t_xt[b]])
            S.dma("pool", (lambda j=j, b=b: (lambda e: e.dma_start(out=cx.X1[j * 128:(j + 1) * 128, :], in_=xt[b])))(),
                  reads=[t_xt[b]], writes=[cx.t_X1[j]])
        phase_end(cx)


PHASES["p3"] = phase_p3


def _declare_peer(cx):
    if hasattr(cx, "SS"):
        return
    nc = cx.nc
    cx.SS = nc.dram_tensor("SS", [NB, 128, 2048], F32).ap()
    cx.MXS = nc.dram_tensor("MXS", [NB, 128, 512], F32).ap()
    cx.t_SS = toks("SS", NB)
    cx.t_UTb2 = [[Tok() for _ in range(8)] for _ in range(8)]
    cx.t_Vb2 = toks("Vb", 32)


class PcastStream:
    def __init__(self, cx, es, items, nring=8):
        self.cx, self.items, self.pos = cx, list(items), 0
        self.st = [_alloc(cx, es, f"pcs{i}", [128, 4096], BF16) for i in range(nring)]
        self.t_st = toks("pcs", nring)
        self.r = 0
        self.pending = []
        self.VHs = cx.VH.rearrange("(j i) d -> i j d", i=128)
        self.Vbd = cx.Vb.rearrange("j i d -> i j d")

    def issue(self, n):
        cx, S = self.cx, self.cx.S
        ready, self.pending = self.pending, []
        for _ in range(n):
            if self.pos >= len(self.items):
                break
            it = self.items[self.pos]
            self.pos += 1
            b = self.r % len(self.st)
            self.r += 1
            if it[0] == "u":
                _, k, cb = it
                S.dma("pool", lambda e, k=k, cb=cb, b=b: e.dma_start(out=self.st[b][:, 0:2048], in_=cx.UT[k * 128:(k + 1) * 128, cb * 2048:(cb + 1) * 2048]),
                      writes=[self.t_st[b]])
            else:
                _, jb = it
                stv = self.st[b].rearrange("p (j d) -> p j d", j=4)
                S.dma("pool", lambda e, jb=jb, stv=stv: e.dma_start(out=stv, in_=self.VHs[:, jb * 4:(jb + 1) * 4, :]), writes=[self.t_st[b]])
            self.pending.append((it, b))
        self._store(ready)

    def _store(self, lst):
        cx, S = self.cx, self.cx.S
        for it, b in lst:
            if it[0] == "u":
                _, k, cb = it
                S.dma("sp", lambda e, k=k, cb=cb, b=b: e.dma_start(out=cx.UTb[k][:, cb * 2048:(cb + 1) * 2048], in_=self.st[b][:, 0:2048]),
                      reads=[self.t_st[b]], writes=[cx.t_UTb2[k][cb]])
            else:
                _, jb = it
                stv = self.st[b].rearrange("p (j d) -> p j d", j=4)
                S.dma("sp", lambda e, jb=jb, stv=stv: e.dma_start(out=self.Vbd[:, jb * 4:(jb + 1) * 4, :], in_=stv),
                      reads=[self.t_st[b]], writes=[cx.t_Vb2[jb]])

    def flush(self):
        while self.pos < len(self.items):
            self.issue(4)
        self._store(self.pending)
        self.pending = []


PCAST_ITEMS = [("u", k, cb) for k in range(8) for cb in range(8)] + [("v", jb) for jb in range(32)]


def phase_pcast(cx):
    nc, S = cx.nc, cx.S
    _declare_peer(cx)
    with ExitStack() as es:
        st = [_alloc(cx, es, f"cst{i}", [128, 4096], BF16) for i in range(4)]
        t_st = toks("cst", 4)
        r = 0
        for k in range(8):
            for cb in range(8):
                b = r % 4
                r += 1
                S.dma("pool", (lambda k=k, cb=cb, b=b: (lambda e: e.dma_start(
                    out=st[b][:, 0:2048], in_=cx.UT[k * 128:(k + 1) * 128, cb * 2048:(cb + 1) * 2048])))(), writes=[t_st[b]])
                S.dma("act", (lambda k=k, cb=cb, b=b: (lambda e: e.dma_start(
                    out=cx.UTb[k][:, cb * 2048:(cb + 1) * 2048], in_=st[b][:, 0:2048])))(), reads=[t_st[b]], writes=[cx.t_UTb2[k][cb]])
        VHs = cx.VH.rearrange("(j i) d -> i j d", i=128)
        Vbd = cx.Vb.rearrange("j i d -> i j d")
        for jb in range(32):
            b = r % 4
            r += 1
            stv = st[b].rearrange("p (j d) -> p j d", j=4)
            S.dma("pool", (lambda jb=jb, stv=stv: (lambda e: e.dma_start(out=stv, in_=VHs[:, jb * 4:(jb + 1) * 4, :])))(), writes=[t_st[b]])
            S.dma("sp", (lambda jb=jb, stv=stv: (lambda e: e.dma_start(out=Vbd[:, jb * 4:(jb + 1) * 4, :], in_=stv)))(),
                  reads=[t_st[b]], writes=[cx.t_Vb2[jb]])
        phase_end(cx)


def phase_p4q(cx, nblocks=NB):
    nc, S = cx.nc, cx.S
    _declare_peer(cx)
    with ExitStack() as es:
        load_mod(cx, es)
        wqb = _alloc(cx, es, "wqb", [128, 8, 2048], BF16)
        skb = _alloc(cx, es, "skb", [128, 16, 128], BF16)
        t_wq, t_sk = Tok(), Tok()
        nb = NormBufs(cx, es, "q")
        xt = [_alloc(cx, es, f"xt{i}", [128, D], F32) for i in range(2)]
        t_xt = toks("xt", 2)
        hT = [_alloc(cx, es, f"hT{i}", [128, 8, 128], BF16) for i in range(2)]
        t_hT = toks("hT", 2)
        qT = _alloc(cx, es, "qT", [128, 16, 128], BF16)
        t_qT = toks("qT", 4)
        sfs = [_alloc(cx, es, f"sfs{i}", [128, 16, 128], F32) for i in range(2)]
        t_sfs = toks("sfs", 2)
        mxix = [_alloc(cx, es, f"mxix{i}", [128, 768], F32) for i in range(2)]
        t_mxix = toks("mxix", 2)
        ixu = _alloc(cx, es, "ixu", [128, 16, 16], U32)
        tm = [_alloc(cx, es, f"tmq{i}", [128, 128], F32) for i in range(16)]
        t_tm, t_mxg, t_ixg = toks("tmq", 16), toks("mxgq", 16), toks("ixgq", 16)
        wsrc = cx.peer_wq.rearrange("(k p) n -> p k n", p=128)
        for k in range(8):
            S.dma("pool", (lambda k=k: (lambda e: e.dma_start(out=wqb[:, k, :], in_=wsrc[:, k, :])))(), writes=[t_wq])
        S.dma("pool", lambda e: e.dma_start(out=skb, in_=cx.skT.rearrange("g d n -> d g n")), writes=[t_sk])
        def stage_a(j):
            b = j % 2
            S.dma("sp", (lambda j=j, b=b: (lambda e: e.dma_start(out=xt[b], in_=cx.X1[j * 128:(j + 1) * 128, :])))(),
                  reads=[cx.t_X1[j]], writes=[t_xt[b]])
            norm_mod(cx, nb, xt[b], t_xt[b], 128, 1)
            transpose8(cx, nb, 128, 0, hT[b], t_hT[b])
            S.dma("act", (lambda j=j, b=b: (lambda e: e.dma_start(out=cx.H2T[j], in_=hT[b])))(), reads=[t_hT[b]], writes=[cx.t_H2T[j]])
            for g4 in range(4):
                bi = 1 + g4 % 2
                pq = cx.banks[bi].rearrange("p (a t) -> p a t", a=4)
                for gg in range(4):
                    gq = g4 * 4 + gg
                    for k in range(8):
                        S.op("pe", (lambda pq=pq, gg=gg, gq=gq, k=k, b=b: (lambda e: e.matmul(
                            pq[:, gg, :], wqb[:, k, gq * 128:(gq + 1) * 128], hT[b][:, k, :], start=(k == 0), stop=(k == 7))))(),
                            reads=[t_wq, t_hT[b]], writes=[cx.t_bank[bi]])
                S.op("act", (lambda pq=pq, g4=g4: (lambda e: e.activation(qT[:, g4 * 4:(g4 + 1) * 4, :], pq, AF.Copy)))(),
                     reads=[cx.t_bank[bi]], writes=[t_qT[g4]])
            for g4 in range(4):
                bi = 3 + g4 % 2
                pk = cx.banks[bi].rearrange("p (a t) -> p a t", a=4)
                for gg in range(4):
                    gq = g4 * 4 + gg
                    S.op("pe", (lambda pk=pk, gg=gg, gq=gq: (lambda e: e.matmul(
                        pk[:, gg, :], qT[:, gq, :], skb[:, gq, :], start=True, stop=True)))(),
                        reads=[t_qT[g4], t_sk], writes=[cx.t_bank[bi]])
                S.op("act", (lambda pk=pk, g4=g4, b=b: (lambda e: e.activation(sfs[b][:, g4 * 4:(g4 + 1) * 4, :], pk, AF.Copy)))(),
                     reads=[cx.t_bank[bi]], writes=[t_sfs[b]])

        def stage_b(j):
            b = j % 2
            mx = mxix[b][:, 0:256].rearrange("p (g r) -> p g r", g=16)
            ixf = mxix[b][:, 256:768].rearrange("p (g r) -> p g r", g=16)
            srcs = [sfs[b][:, gq, :] for gq in range(16)]
            for gq in range(16):
                S.op("dve", (lambda gq=gq, mx=mx, srcs=srcs: (lambda e: e.max(mx[:, gq, 0:8], srcs[gq])))(), reads=[t_sfs[b]], writes=[t_mxg[gq]])
            for gq in range(16):
                S.op("dve", (lambda gq=gq, mx=mx, srcs=srcs: (lambda e: e.match_replace(tm[gq], mx[:, gq, 0:8], srcs[gq], -BIGF)))(),
                     reads=[t_sfs[b], t_mxg[gq]], writes=[t_tm[gq]])
            for gq in range(16):
                S.op("dve", (lambda gq=gq, mx=mx, srcs=srcs: (lambda e: e.max_index(ixu[:, gq, 0:8], mx[:, gq, 0:8], srcs[gq])))(),
                     reads=[t_sfs[b], t_mxg[gq]], writes=[t_ixg[gq]])
            for gq in range(16):
                S.op("dve", (lambda gq=gq, mx=mx: (lambda e: e.max(mx[:, gq, 8:16], tm[gq])))(), reads=[t_tm[gq]], writes=[t_mxg[gq]])
            for gq in range(16):
                S.op("dve", (lambda gq=gq, mx=mx: (lambda e: e.max_index(ixu[:, gq, 8:16], mx[:, gq, 8:16], tm[gq])))(),
                     reads=[t_tm[gq], t_mxg[gq]], writes=[t_ixg[gq]])
            S.op("pool", (lambda b=b: (lambda e: e.tensor_copy(mxix[b][:, 256:512], ixu.rearrange("p g r -> p (g r)"))))(),
                 reads=t_ixg, writes=[t_mxix[b]])
            S.dma("pool", (lambda j=j, b=b: (lambda e: e.dma_start(out=cx.MXS[j], in_=mxix[b][:, 0:512])))(),
                  reads=t_mxg + [t_mxix[b]], writes=[cx.t_SS[j]])

        stage_a(0)
        for j in range(nblocks):
            if j + 1 < nblocks:
                stage_a(j + 1)
            stage_b(j)
        phase_end(cx)


PHASES["pcast"] = phase_pcast
PHASES["p4q"] = phase_p4q


BIGF = 1.0e30
PHI_MARGIN = 2.0e-6


def phase_p4a_old(cx, nblocks=NB):
    nc, S = cx.nc, cx.S
    _declare_peer(cx)
    with ExitStack() as es:
        A_ = lambda name, shape, dt: _alloc(cx, es, name, shape, dt)
        iota = A_("iota", [128, 128], F32)
        t_iota = Tok()
        sf = [A_(f"sf{i}", [128, 16, 128], F32) for i in range(2)]
        t_sf = toks("sf", 2)
        tm = [A_(f"tm{i}", [128, 128], F32) for i in range(4)]
        t_tm = toks("tm", 4)
        mx = A_("mx", [128, 16, 16], F32)
        ix = A_("ix", [128, 8, 16], U32)
        ixf = A_("ixf", [128, 8, 16], F32)
        combo = A_("combo", [128, 8, 16, 16], F32)
        ctmp = [A_(f"ctmp{i}", [128, 256], F32) for i in range(2)]
        t_ctmp = toks("ctmp", 2)
        tops = A_("tops", [128, 8, 16], F32)
        sml = A_("sml", [128, 8, 8], F32)
        etop = A_("etop", [128, 8, 16], F32)
        Sel = A_("Sel", [128, 8, 16, 16], F32)
        wa = A_("wa", [128, 8, 16, 16], F32)
        wb = A_("wb", [128, 8, 16, 16], F32)
        phi = A_("phi", [128, 128], F32)
        Aw = A_("Aw", [128, 8, 16], F32)
        Bw = A_("Bw", [128, 8, 128], F32)
        tT = A_("tT", [128, 256], F32)
        L = A_("L", [128, 128, 128], BF16)
        R = A_("R", [128, 128, 128], BF16)
        MTt = A_("MTt", [128, 128, 128], BF16)
        s2x = [A_(f"s2x{i}", [128, 4, 8, 16], F32) for i in range(3)]
        abc = [A_(f"abc{i}", [128, 4, 8, 16], BF16) for i in range(3)]
        ind = [A_(f"ind{i}", [128, 128, 4], BF16) for i in range(2)]
        t_s2x, t_abc, t_ind = toks("s2x", 3), toks("abc", 3), toks("ind", 2)
        (t_mx, t_ix, t_ixf, t_combo, t_tops, t_sml, t_etop, t_Sel, t_wa, t_wb, t_phi, t_Aw, t_Bw, t_tT, t_L, t_R, t_MTt) = [Tok() for _ in range(17)]
        S.dma("sp", lambda e: e.dma_start(out=iota, in_=cx.iota), writes=[t_iota])
        mxv = mx.rearrange("p (h two) r -> p h two r", two=2)
        s1top, s2top = mxv[:, :, 0, :], mxv[:, :, 1, :]
        tmi = 0
        ri3 = 0
        ri2 = 0
        for j in range(nblocks):
            b = j % 2
            S.dma("sp", (lambda j=j, b=b: (lambda e: e.dma_start(out=sf[b].rearrange("p g n -> p (g n)"), in_=cx.SS[j])))(),
                  reads=[cx.t_SS[j]], writes=[t_sf[b]])
            sfv = sf[b].rearrange("p (h two) n -> p h two n", two=2)
            for gq in range(16):
                r = tmi % 4
                tmi += 1
                src = sf[b][:, gq, :]
                S.op("dve", (lambda gq=gq, src=src: (lambda e: e.max(mx[:, gq, 0:8], src)))(), reads=[t_sf[b]], writes=[t_mx])
                S.op("dve", (lambda gq=gq, src=src, r=r: (lambda e: e.match_replace(tm[r], mx[:, gq, 0:8], src, -BIGF)))(),
                     reads=[t_sf[b], t_mx], writes=[t_tm[r]])
                S.op("dve", (lambda gq=gq, r=r: (lambda e: e.max(mx[:, gq, 8:16], tm[r])))(), reads=[t_tm[r]], writes=[t_mx])
                if gq % 2 == 0:
                    h = gq // 2
                    S.op("dve", (lambda gq=gq, h=h, src=src: (lambda e: e.max_index(ix[:, h, 0:8], mx[:, gq, 0:8], src)))(),
                         reads=[t_sf[b], t_mx], writes=[t_ix])
                    S.op("dve", (lambda gq=gq, h=h, r=r: (lambda e: e.max_index(ix[:, h, 8:16], mx[:, gq, 8:16], tm[r])))(),
                         reads=[t_tm[r], t_mx], writes=[t_ix])
            S.op("dve", lambda e: e.tensor_copy(ixf, ix), reads=[t_ix], writes=[t_ixf])
            S.op("pool", lambda e: e.tensor_tensor(combo, s1top.unsqueeze(3).to_broadcast([128, 8, 16, 16]),
                                                    s2top.unsqueeze(2).to_broadcast([128, 8, 16, 16]), ALU.add),
                 reads=[t_mx], writes=[t_combo])
            for h in range(8):
                r = h % 2
                ch = combo[:, h, :, :].rearrange("p a b -> p (a b)")
                S.op("dve", (lambda h=h, ch=ch: (lambda e: e.max(tops[:, h, 0:8], ch)))(), reads=[t_combo], writes=[t_tops])
                S.op("dve", (lambda h=h, ch=ch, r=r: (lambda e: e.match_replace(ctmp[r], tops[:, h, 0:8], ch, -BIGF)))(),
                     reads=[t_combo, t_tops], writes=[t_ctmp[r]])
                S.op("dve", (lambda h=h, r=r: (lambda e: e.max(tops[:, h, 8:16], ctmp[r])))(), reads=[t_ctmp[r]], writes=[t_tops])
            S.op("dve", lambda e: e.tensor_scalar(sml[:, :, 0], tops[:, :, 0], -1.0, None, ALU.mult), reads=[t_tops], writes=[t_sml])
            S.op("dve", lambda e: e.tensor_scalar(sml[:, :, 3:5], mxv[:, :, :, 0], -1.0, None, ALU.mult), reads=[t_mx], writes=[t_sml])
            for h in range(8):
                S.op("act", (lambda h=h: (lambda e: e.activation(etop[:, h, :], tops[:, h, :], AF.Exp, bias=sml[:, h, 0:1],
                                                                 accum_out=sml[:, h, 1:2])))(),
                     reads=[t_tops, t_sml], writes=[t_etop, t_sml])
            S.op("dve", lambda e: e.reciprocal(sml[:, :, 2], sml[:, :, 1]), reads=[t_sml], writes=[t_sml])
            for h in range(8):
                S.op("act", (lambda h=h: (lambda e: e.activation(Aw[:, h, :], s1top[:, h, :], AF.Exp, bias=sml[:, h, 3:4])))(),
                     reads=[t_mx, t_sml], writes=[t_Aw])
                S.op("act", (lambda h=h, sfv=sfv: (lambda e: e.activation(Bw[:, h, :], sfv[:, h, 1, :], AF.Exp, bias=sml[:, h, 4:5])))(),
                     reads=[t_sf[b], t_sml], writes=[t_Bw])
            S.op("dve", lambda e: e.tensor_tensor(Aw, Aw, sml[:, :, 2:3].to_broadcast([128, 8, 16]), ALU.mult),
                 reads=[t_Aw, t_sml], writes=[t_Aw])
            S.op("dve", lambda e: e.tensor_tensor(Sel, combo, tops[:, :, 15:16].unsqueeze(3).to_broadcast([128, 8, 16, 16]), ALU.is_ge),
                 reads=[t_combo, t_tops], writes=[t_Sel])
            S.op("pool", lambda e: e.tensor_tensor(wa, Sel, s2top.unsqueeze(2).to_broadcast([128, 8, 16, 16]), ALU.mult),
                 reads=[t_Sel, t_mx], writes=[t_wa])
            S.op("dve", lambda e: e.tensor_scalar(wb, Sel, -BIGF, BIGF, ALU.mult, ALU.add), reads=[t_Sel], writes=[t_wb])
            S.op("dve", lambda e: e.tensor_tensor(wa, wa, wb, ALU.add), reads=[t_wa, t_wb], writes=[t_wa])
            S.op("dve", lambda e: e.tensor_reduce(phi, wa.rearrange("p h a b -> p (h a) b"), AX.X, ALU.min), reads=[t_wa], writes=[t_phi])
            S.op("dve", lambda e: e.tensor_scalar(phi, phi, -PHI_MARGIN, None, ALU.add), reads=[t_phi], writes=[t_phi])
            S.op("pe", lambda e: e.transpose(cx.banks[5][:, 0:128], ixf.rearrange("p h r -> p (h r)"), cx.identf),
                 reads=[t_ixf, cx.t_const], writes=[cx.t_bank[5]])
            S.op("pe", lambda e: e.transpose(cx.banks[5][:, 128:256], phi, cx.identf), reads=[t_phi, cx.t_const], writes=[cx.t_bank[5]])
            S.op("act", lambda e: e.activation(tT, cx.banks[5][:, 0:256], AF.Copy), reads=[cx.t_bank[5]], writes=[t_tT])
            S.op("dve", lambda e: e.tensor_tensor(L, iota.unsqueeze(1).to_broadcast([128, 128, 128]),
                                                  tT[:, 0:128].unsqueeze(2).to_broadcast([128, 128, 128]), ALU.is_equal),
                 reads=[t_iota, t_tT], writes=[t_L])
            Rj = R.rearrange("p t j -> p j t")
            for jc in range(32):
                j0 = jc * 4
                r3 = ri3 % 3
                ri3 += 1
                r2 = ri2 % 2
                ri2 += 1
                ba, bb = 1 + jc % 2, 3 + jc % 2
                srcS = sfv[:, :, 1, j0:j0 + 4].rearrange("p h j -> p j h").unsqueeze(3).to_broadcast([128, 4, 8, 16])
                srcB = Bw[:, :, j0:j0 + 4].rearrange("p h j -> p j h").unsqueeze(3).to_broadcast([128, 4, 8, 16])
                srcA = Aw.unsqueeze(1).to_broadcast([128, 4, 8, 16])
                S.op("act", (lambda r3=r3, srcS=srcS: (lambda e: e.activation(s2x[r3], srcS, AF.Copy)))(), reads=[t_sf[b]], writes=[t_s2x[r3]])
                S.op("pool", (lambda r3=r3, srcB=srcB, srcA=srcA: (lambda e: e.tensor_tensor(abc[r3], srcB, srcA, ALU.mult)))(),
                     reads=[t_Bw, t_Aw], writes=[t_abc[r3]])
                for jj in range(4):
                    S.op("pe", (lambda ba=ba, jj=jj, r3=r3: (lambda e: e.matmul(
                        cx.banks[ba][:, jj * 128:(jj + 1) * 128], s2x[r3][:, jj, :, :].rearrange("p h r -> p (h r)"), cx.identf,
                        start=True, stop=True)))(), reads=[t_s2x[r3], cx.t_const], writes=[cx.t_bank[ba]])
                for jj in range(4):
                    S.op("pe", (lambda bb=bb, jj=jj, r3=r3: (lambda e: e.matmul(
                        cx.banks[bb][:, jj * 128:(jj + 1) * 128], abc[r3][:, jj, :, :].rearrange("p h r -> p (h r)"), cx.identb,
                        start=True, stop=True)))(), reads=[t_abc[r3], cx.t_const], writes=[cx.t_bank[bb]])
                S.op("dve", (lambda ba=ba, r2=r2: (lambda e: e.tensor_tensor(
                    ind[r2], cx.banks[ba].rearrange("p (j t) -> p t j", j=4), tT[:, 128:256].unsqueeze(2).to_broadcast([128, 128, 4]), ALU.is_ge)))(),
                    reads=[cx.t_bank[ba], t_tT], writes=[t_ind[r2]])
                S.op("dve", (lambda bb=bb, r2=r2, j0=j0: (lambda e: e.tensor_tensor(
                    R[:, :, j0:j0 + 4], ind[r2], cx.banks[bb].rearrange("p (j t) -> p t j", j=4), ALU.mult)))(),
                    reads=[cx.t_bank[bb], t_ind[r2]], writes=[t_R])
            MTv = MTt.rearrange("p j t -> p t j")
            for t4 in range(32):
                bm = 6 + t4 % 2
                for tt in range(4):
                    t = t4 * 4 + tt
                    S.op("pe", (lambda bm=bm, tt=tt, t=t: (lambda e: e.matmul(
                        cx.banks[bm][:, tt * 128:(tt + 1) * 128], L[:, t, :], R[:, t, :], start=True, stop=True)))(),
                        reads=[t_L, t_R], writes=[cx.t_bank[bm]])
                S.op("act", (lambda bm=bm, t4=t4: (lambda e: e.activation(
                    MTt[:, :, t4 * 4:(t4 + 1) * 4], cx.banks[bm].rearrange("p (t j) -> p j t", t=4), AF.Copy)))(),
                    reads=[cx.t_bank[bm]], writes=[t_MTt])
            S.dma("sp", (lambda j=j: (lambda e: e.dma_start(out=cx.MT[j], in_=MTt)))(), reads=[t_MTt], writes=[cx.t_MT[j]])
        phase_end(cx)


def phase_p4a(cx, nblocks=NB):
    nc, S = cx.nc, cx.S
    _declare_peer(cx)
    with ExitStack() as es:
        A_ = lambda name, shape, dt: _alloc(cx, es, name, shape, dt)
        iota = A_("iota", [128, 128], F32)
        t_iota = Tok()
        mxl = [A_(f"mxl{i}", [128, 512], F32) for i in range(2)]
        t_mxl = toks("mxl", 2)
        tm = [A_(f"tm{i}", [128, 128], F32) for i in range(16)]
        t_tm = toks("tm", 16)
        t_mxg = toks("mxg", 16)
        t_ixg = toks("ixg", 16)
        t_topg = toks("topg", 8)
        t_posg = toks("posg", 8)
        mx = A_("mx", [128, 16, 16], F32)
        ix = A_("ix", [128, 16, 16], U32)
        ixf = A_("ixf", [128, 16, 16], F32)
        combo = A_("combo", [128, 8, 16, 16], F32)
        ctmp = [A_(f"ctmp{i}", [128, 256], F32) for i in range(8)]
        t_ctmp = toks("ctmp", 8)
        tops = A_("tops", [128, 8, 16], F32)
        pos = A_("pos", [128, 8, 16], U32)
        pab = A_("pab", [128, 2, 8, 16], U32)
        pabf = A_("pabf", [128, 2, 8, 16], F32)
        sml = A_("sml", [128, 8, 4], F32)
        E1 = A_("E1", [128, 8, 16, 16], F32)
        E2 = A_("E2", [128, 8, 16, 16], F32)
        IJg = A_("IJg", [128, 3, 8, 16], F32)
        tTs = [A_(f"tT{i}", [128, 3, 128], F32) for i in range(2)]
        L = A_("L", [128, 128, 128], BF16)
        R = A_("R", [128, 128, 128], BF16)
        MTt = [A_(f"MTt{i}", [128, 128, 128], BF16) for i in range(2)]
        t_MTt = toks("MTt", 2)
        (t_mx, t_ix, t_ixf, t_combo, t_tops, t_pos, t_pab, t_pabf, t_sml, t_E1, t_E2, t_I, t_J, t_g, t_tT0, t_L, t_R) = [Tok() for _ in range(17)]
        t_tTs = [t_tT0, Tok()]
        S.dma("sp", lambda e: e.dma_start(out=iota, in_=cx.iota), writes=[t_iota])
        mxv = mx.rearrange("p (h two) r -> p h two r", two=2)
        s1top, s2top = mxv[:, :, 0, :], mxv[:, :, 1, :]
        ixv = ixf.rearrange("p (h two) r -> p h two r", two=2)
        idx1f, idx2f = ixv[:, :, 0, :], ixv[:, :, 1, :]
        io16 = iota[:, 0:16]
        tmi = 0
        mxl3 = [A_(f"mxl3{i}", [128, 512], F32) for i in range(3)]
        t_mxl3 = toks("mxl3", 3)
        tops2 = [A_(f"tops2{i}", [128, 8, 16], F32) for i in range(2)]
        pos2 = [A_(f"pos2{i}", [128, 8, 16], U32) for i in range(2)]
        negm2 = [A_(f"negm2{i}", [128, 8], F32) for i in range(2)]
        Z2 = [A_(f"Z2{i}", [128, 8], F32) for i in range(2)]
        rZ = A_("rZ", [128, 8], F32)
        IJg2 = [A_(f"IJg2{i}", [128, 3, 8, 16], F32) for i in range(2)]
        t_topg2 = [toks("topg2a", 8), toks("topg2b", 8)]
        t_posg2 = [toks("posg2a", 8), toks("posg2b", 8)]
        t_negm2, t_Z2, t_e2 = toks("negm2", 2), toks("Z2", 2), toks("e2", 2)
        t_rZ = Tok()

        def front_a(j):
            b3, b2 = j % 3, j % 2
            tops, pos, negm, Zt, IJg = tops2[b2], pos2[b2], negm2[b2], Z2[b2], IJg2[b2]
            t_topg, t_posg = t_topg2[b2], t_posg2[b2]
            S.dma("sp", lambda e: e.dma_start(out=mxl3[b3], in_=cx.MXS[j]), reads=[cx.t_SS[j]], writes=[t_mxl3[b3]])
            mx = mxl3[b3][:, 0:256].rearrange("p (g r) -> p g r", g=16)
            mxv = mx.rearrange("p (h two) r -> p h two r", two=2)
            s1top, s2top = mxv[:, :, 0, :], mxv[:, :, 1, :]
            S.op("dve", lambda e: e.tensor_tensor(combo, s1top.unsqueeze(3).to_broadcast([128, 8, 16, 16]),
                                                  s2top.unsqueeze(2).to_broadcast([128, 8, 16, 16]), ALU.add),
                 reads=[t_mxl3[b3]], writes=[t_combo])
            chs = [combo[:, h, :, :].rearrange("p a b -> p (a b)") for h in range(8)]
            for h in range(8):
                S.op("dve", (lambda h=h: (lambda e: e.max(tops[:, h, 0:8], chs[h])))(), reads=[t_combo], writes=[t_topg[h]])
            for h in range(8):
                S.op("dve", (lambda h=h: (lambda e: e.match_replace(ctmp[h], tops[:, h, 0:8], chs[h], -BIGF)))(),
                     reads=[t_combo, t_topg[h]], writes=[t_ctmp[h]])
            for h in range(8):
                S.op("dve", (lambda h=h: (lambda e: e.max_index(pos[:, h, 0:8], tops[:, h, 0:8], chs[h])))(),
                     reads=[t_combo, t_topg[h]], writes=[t_posg[h]])
            for h in range(8):
                S.op("dve", (lambda h=h: (lambda e: e.max(tops[:, h, 8:16], ctmp[h])))(), reads=[t_ctmp[h]], writes=[t_topg[h]])
            for h in range(8):
                S.op("dve", (lambda h=h: (lambda e: e.max_index(pos[:, h, 8:16], tops[:, h, 8:16], ctmp[h])))(),
                     reads=[t_ctmp[h], t_topg[h]], writes=[t_posg[h]])
            S.op("dve", lambda e: e.tensor_scalar(negm, tops[:, :, 0], -1.0, None, ALU.mult), reads=t_topg, writes=[t_negm2[b2]])
            for h in range(8):
                S.op("act", (lambda h=h: (lambda e: e.activation(IJg[:, 2, h, :], tops[:, h, :], AF.Exp, bias=negm[:, h:h + 1],
                                                                 accum_out=Zt[:, h:h + 1])))(),
                     reads=t_topg + [t_negm2[b2]], writes=[t_e2[b2], t_Z2[b2]])

        def front_b(j):
            b3, b2 = j % 3, j % 2
            pos, Zt, IJg = pos2[b2], Z2[b2], IJg2[b2]
            t_posg = t_posg2[b2]
            tT, t_tT = tTs[j % 2], t_tTs[j % 2]
            ixf = mxl3[b3][:, 256:512].rearrange("p (g r) -> p g r", g=16)
            ixv = ixf.rearrange("p (h two) r -> p h two r", two=2)
            idx1f, idx2f = ixv[:, :, 0, :], ixv[:, :, 1, :]
            S.op("dve", lambda e: e.reciprocal(rZ, Zt), reads=[t_Z2[b2]], writes=[t_rZ])
            S.op("dve", lambda e: e.tensor_tensor(IJg[:, 2, :, :], IJg[:, 2, :, :], rZ.unsqueeze(2).to_broadcast([128, 8, 16]), ALU.mult),
                 reads=[t_e2[b2], t_rZ], writes=[t_g])
            S.op("dve", lambda e: e.tensor_scalar(pab[:, 0, :, :], pos, 4, None, ALU.logical_shift_right), reads=t_posg, writes=[t_pab])
            S.op("dve", lambda e: e.tensor_scalar(pab[:, 1, :, :], pos, 15, None, ALU.bitwise_and), reads=t_posg, writes=[t_pab])
            S.op("pool", lambda e: e.tensor_copy(pabf, pab), reads=[t_pab], writes=[t_pabf])
            io4 = io16.unsqueeze(1).unsqueeze(1).to_broadcast([128, 8, 16, 16])
            S.op("dve", lambda e: e.tensor_tensor(E1, io4, pabf[:, 0, :, :].unsqueeze(3).to_broadcast([128, 8, 16, 16]), ALU.is_equal),
                 reads=[t_iota, t_pabf], writes=[t_E1])
            S.op("dve", lambda e: e.tensor_tensor(E1, E1, idx1f.unsqueeze(2).to_broadcast([128, 8, 16, 16]), ALU.mult),
                 reads=[t_E1, t_mxl3[b3]], writes=[t_E1])
            S.op("dve", lambda e: e.tensor_tensor(E2, io4, pabf[:, 1, :, :].unsqueeze(3).to_broadcast([128, 8, 16, 16]), ALU.is_equal),
                 reads=[t_iota, t_pabf], writes=[t_E2])
            S.op("dve", lambda e: e.tensor_tensor(E2, E2, idx2f.unsqueeze(2).to_broadcast([128, 8, 16, 16]), ALU.mult),
                 reads=[t_E2, t_mxl3[b3]], writes=[t_E2])
            S.op("dve", lambda e: e.tensor_reduce(IJg[:, 0, :, :].rearrange("p h k -> p (h k)"), E1.rearrange("p h k a -> p (h k) a"), AX.X, ALU.add),
                 reads=[t_E1], writes=[t_I])
            S.op("dve", lambda e: e.tensor_reduce(IJg[:, 1, :, :].rearrange("p h k -> p (h k)"), E2.rearrange("p h k a -> p (h k) a"), AX.X, ALU.add),
                 reads=[t_E2], writes=[t_J])
            for q3, tk in ((0, t_I), (1, t_J), (2, t_g)):
                S.op("pe", (lambda q3=q3: (lambda e: e.transpose(cx.banks[5][:, q3 * 128:(q3 + 1) * 128],
                                                                  IJg[:, q3, :, :].rearrange("p h k -> p (h k)"), cx.identf)))(),
                     reads=[tk, cx.t_const], writes=[cx.t_bank[5]])
            S.op("dve", lambda e: e.tensor_copy(tT.rearrange("p a t -> p (a t)"), cx.banks[5][:, 0:384]), reads=[cx.t_bank[5]], writes=[t_tT])

        def back(j):
            tT, t_tT = tTs[j % 2], t_tTs[j % 2]
            io3 = iota.unsqueeze(1).to_broadcast([128, 128, 128])
            Lf = L.rearrange("p t i -> p (t i)")
            S.op("dve", lambda e: e.tensor_tensor(L, io3, tT[:, 0, :].unsqueeze(2).to_broadcast([128, 128, 128]), ALU.is_equal),
                 reads=[t_iota, t_tT], writes=[t_L])
            S.op("dve", lambda e: e.tensor_tensor(R, io3, tT[:, 1, :].unsqueeze(2).to_broadcast([128, 128, 128]), ALU.is_equal),
                 reads=[t_iota, t_tT], writes=[t_R])
            S.op("pool", lambda e: e.tensor_tensor(R, R, tT[:, 2, :].unsqueeze(2).to_broadcast([128, 128, 128]), ALU.mult),
                 reads=[t_R, t_tT], writes=[t_R])
            mb_ = j % 2
            for t4 in range(32):
                bm = 6 + t4 % 2
                for tt in range(4):
                    t = t4 * 4 + tt
                    S.op("pe", (lambda bm=bm, tt=tt, t=t: (lambda e: e.matmul(
                        cx.banks[bm][:, tt * 128:(tt + 1) * 128], L[:, t, :], R[:, t, :], start=True, stop=True)))(),
                        reads=[t_L, t_R], writes=[cx.t_bank[bm]])
                S.op("act", (lambda bm=bm, t4=t4, mb_=mb_: (lambda e: e.activation(
                    MTt[mb_][:, :, t4 * 4:(t4 + 1) * 4], cx.banks[bm].rearrange("p (t j) -> p j t", t=4), AF.Copy)))(),
                    reads=[cx.t_bank[bm]], writes=[t_MTt[mb_]])
            S.dma("act", (lambda j=j, mb_=mb_: (lambda e: e.dma_start(out=cx.MT[j], in_=MTt[mb_])))(), reads=[t_MTt[mb_]], writes=[cx.t_MT[j]])

        front_a(0)
        if nblocks > 1:
            front_a(1)
        front_b(0)
        for j in range(nblocks):
            if j + 2 < nblocks:
                front_a(j + 2)
            if j + 1 < nblocks:
                front_b(j + 1)
            back(j)
        phase_end(cx)


def phase_p4b(cx, nblocks=NB):
    nc, S = cx.nc, cx.S
    _declare_peer(cx)
    LAG = 2
    with ExitStack() as es:
        load_mod(cx, es)
        A_ = lambda name, shape, dt: _alloc(cx, es, name, shape, dt)
        utb = [A_(f"utb{i}", [128, 8, 1024], BF16) for i in range(3)]
        vb = [A_(f"vb{i}", [128, 8, 1024], BF16) for i in range(3)]
        mtb = [A_(f"mtb{i}", [128, 2, 8, 128], BF16) for i in range(3)]
        t_utb, t_vb, t_mtb = toks("utb", 3), toks("vb", 3), toks("mtb", 3)
        h2g = [A_(f"h2g{i}", [128, 8, 256], BF16) for i in range(2)]
        t_h2g = toks("h2g", 2)
        ga = [A_(f"ga{i}", [128, 256], F32) for i in range(3)]
        ptl = [A_(f"ptl{i}", [128, 256], BF16) for i in range(4)]
        t_ga, t_ptl = toks("ga", 3), toks("ptl", 4)
        x1t = [A_(f"x1t{i}", [128, D], F32) for i in range(2)]
        tmp = A_("tmp4", [128, D], F32)
        t_x1t, t_tmp = toks("x1t", 2), Tok()
        G2 = cx.modbc[:, 5 * D:6 * D]
        UTd = cx.UTb.rearrange("k p c -> p k c")
        Vbd = cx.Vb.rearrange("j i d -> i j d")
        ngroups = nblocks // 2
        PA = (4, 5, 6)

        def load_block(g, jb):
            b = (g * 16 + jb) % 3
            S.dma("sp", lambda e: e.dma_start(out=utb[b], in_=UTd[:, :, jb * 1024:(jb + 1) * 1024]),
                  reads=[cx.t_UTb2[k][jb // 2] for k in range(8)], writes=[t_utb[b]])
            S.dma("act", lambda e: e.dma_start(out=vb[b], in_=Vbd[:, jb * 8:(jb + 1) * 8, :]),
                  reads=[cx.t_Vb2[jb * 2], cx.t_Vb2[jb * 2 + 1]], writes=[t_vb[b]])
            for tt in range(2):
                S.dma("sp", (lambda tt=tt: (lambda e: e.dma_start(out=mtb[b][:, tt, :, :], in_=cx.MT[2 * g + tt][:, jb * 8:(jb + 1) * 8, :])))(),
                      reads=[cx.t_MT[2 * g + tt]], writes=[t_mtb[b]])

        def load_h2(g):
            for tt in range(2):
                S.dma("sp", (lambda tt=tt: (lambda e: e.dma_start(out=h2g[g % 2][:, :, tt * 128:(tt + 1) * 128], in_=cx.H2T[2 * g + tt])))(),
                      reads=[cx.t_H2T[2 * g + tt]], writes=[t_h2g[g % 2]])

        def stage_u(g, j):
            jb, jj = j // 8, j % 8
            b = (g * 16 + jb) % 3
            it = g * 128 + j
            ba = PA[it % 3]
            gr = it % 3
            pr = it % 4
            for k in range(8):
                S.op("pe", (lambda k=k: (lambda e: e.matmul(
                    cx.banks[ba][:, 0:256], utb[b][:, k, jj * 128:(jj + 1) * 128], h2g[g % 2][:, k, :], start=(k == 0), stop=(k == 7))))(),
                    reads=[t_utb[b], t_h2g[g % 2]], writes=[cx.t_bank[ba]])
            S.op("act", lambda e: e.activation(ga[gr], cx.banks[ba][:, 0:256], GELU_FUNC), reads=[cx.t_bank[ba]], writes=[t_ga[gr]])
            S.op("dve", lambda e: e.tensor_tensor(ptl[pr].rearrange("p (a t) -> p a t", a=2), ga[gr].rearrange("p (a t) -> p a t", a=2),
                                                  mtb[b][:, :, jj, :], ALU.mult),
                 reads=[t_ga[gr], t_mtb[b]], writes=[t_ptl[pr]])

        def stage_v(g, j):
            jb, jj = j // 8, j % 8
            b = (g * 16 + jb) % 3
            it = g * 128 + j
            pr = it % 4
            for tt in range(2):
                for half in range(2):
                    bo = tt * 2 + half
                    S.op("pe", (lambda tt=tt, half=half, bo=bo: (lambda e: e.matmul(
                        cx.banks[bo], ptl[pr][:, tt * 128:(tt + 1) * 128], vb[b][:, jj, half * 512:(half + 1) * 512],
                        start=(j == 0), stop=(j == 127))))(),
                        reads=[t_ptl[pr], t_vb[b]], writes=[cx.t_bank[bo]])

        def epilogue(g):
            for tt in range(2):
                jt = 2 * g + tt
                xb = jt % 2
                S.dma("sp", (lambda jt=jt, xb=xb: (lambda e: e.dma_start(out=x1t[xb], in_=cx.X1[jt * 128:(jt + 1) * 128, :])))(),
                      reads=[cx.t_X1[jt]], writes=[t_x1t[xb]])
                for half in range(2):
                    bo = tt * 2 + half
                    S.op("dve", (lambda bo=bo, half=half: (lambda e: e.tensor_tensor(
                        tmp[:, half * 512:(half + 1) * 512], cx.banks[bo], G2[:, half * 512:(half + 1) * 512], ALU.mult)))(),
                        reads=[cx.t_bank[bo], cx.t_mod], writes=[t_tmp])
                S.op("pool", (lambda xb=xb: (lambda e: e.tensor_tensor(x1t[xb], tmp, x1t[xb], ALU.add)))(),
                     reads=[t_tmp, t_x1t[xb]], writes=[t_x1t[xb]])
                S.dma("pool", (lambda jt=jt, xb=xb: (lambda e: e.dma_start(out=cx.out[jt * 128:(jt + 1) * 128, :], in_=x1t[xb])))(),
                      reads=[t_x1t[xb]], writes=[cx.t_outs[jt]])

        seq = [(g, j) for g in range(ngroups) for j in range(128)]
        blocks = [(g, jb) for g in range(ngroups) for jb in range(16)]
        load_h2(0)
        for bi_ in range(min(3, len(blocks))):
            load_block(*blocks[bi_])
        for s_i in range(len(seq) + LAG):
            if s_i < len(seq):
                g, j = seq[s_i]
                if j == 64 and g + 1 < ngroups:
                    load_h2(g + 1)
                stage_u(g, j)
            if s_i - LAG >= 0:
                g, j = seq[s_i - LAG]
                stage_v(g, j)
                if j % 8 == 7:
                    nb_i = (g * 16 + j // 8) + 3
                    if nb_i < len(blocks):
                        load_block(*blocks[nb_i])
                if j == 127:
                    epilogue(g)
        phase_end(cx)


GELU_FUNC = AF.Gelu
PHASES["p4a"] = phase_p4a
PHASES["p4b"] = phase_p4b


PCAST_OVERLAP = True
ALL_PHASES = ["ada", "p1a", "p1b", "p2", "p3", "p4q", "p4a", "p4b"]


def kernel(**inputs):
    sh = prep_shared(inputs)
    nc, cx = build_program(ALL_PHASES)
    in_maps = [prep_core(inputs, sh, c) for c in range(8)]
    res = run_bass_kernel_spmd(nc, in_maps, core_ids=list(range(8)))
    out = np.empty((4, SEQ, D), np.float32)
    for c in range(8):
        o = np.asarray(res.results[c]["out"])
        for j, g in enumerate(own_blocks(c % 2)):
            out[c // 2, g * 128:(g + 1) * 128, :] = o[j * 128:(j + 1) * 128, :]
    return out
```
